# Optimizing a Trainium2 kernel written in Bass

```python
import math
import jax, jax.numpy as jnp
from jax import lax
import numpy as np

D_MODEL = 1024
BATCH = 8
SEQ = 8192
DEPTH = 2

HEAD_DIM = 64
ROPE_THETA = 500000.0
ROT_DIM = HEAD_DIM // 4
Q_BLOCK = 128
NEG_INF = -1e30

DIFF_HEADS = 4
DIFF_QK_DIM = HEAD_DIM
DIFF_V_DIM = 2 * HEAD_DIM
MLA_HEADS = 8
MLA_Q_RANK = 384
MLA_KV_RANK = 256
MLA_NOPE_DIM = 64
MLA_ROPE_DIM = 32
MLA_V_DIM = 64
DIL_PATTERNS = ((128, 1), (512, 4), (2048, 16))
DIL_GROUPS = len(DIL_PATTERNS)
DIL_HEADS = 4
DIL_QK_DIM = HEAD_DIM
DIL_V_DIM = 2 * HEAD_DIM
BRANCH_A = DIFF_HEADS * DIFF_V_DIM
BRANCH_B = MLA_HEADS * MLA_V_DIM
BRANCH_C = DIL_HEADS * DIL_V_DIM
N_BRANCHES = 3
IN_SIZES = (DIFF_HEADS * 2 * DIFF_QK_DIM, DIFF_HEADS * 2 * DIFF_QK_DIM, DIFF_HEADS * DIFF_V_DIM,
            MLA_Q_RANK, MLA_KV_RANK, MLA_ROPE_DIM,
            DIL_GROUPS * DIL_HEADS * DIL_QK_DIM, DIL_GROUPS * DIL_HEADS * DIL_QK_DIM,
            DIL_GROUPS * DIL_HEADS * DIL_V_DIM,
            N_BRANCHES * D_MODEL)
IN_SPLITS = tuple(int(v) for v in np.cumsum(IN_SIZES)[:-1])
IN_WIDTH = int(sum(IN_SIZES))
N_EXPERTS = 16
N_EXPERT_GROUPS = 4
EXPERTS_PER_GROUP = N_EXPERTS // N_EXPERT_GROUPS
TOPK_GROUPS = 1
TOP_K = 2
EXPERT_HIDDEN = 256
SHARED_HIDDEN = 256
DEEPNORM_ALPHA = (2 * DEPTH) ** 0.25
DEEPNORM_BETA = (8 * DEPTH) ** -0.25

kernel_name = 'hybrid_diff_mla_dilated_moe_encoder'

f32 = jnp.float32


def layer_norm(x, g, b, eps=1e-5):
    xf = x.astype(f32)
    mu = xf.mean(-1, keepdims=True)
    var = jnp.square(xf - mu).mean(-1, keepdims=True)
    return ((xf - mu) * lax.rsqrt(var + eps) * g.astype(f32) + b.astype(f32)).astype(x.dtype)


def rms_norm(x, g, eps=1e-6):
    xf = x.astype(f32)
    return (xf * lax.rsqrt(jnp.mean(xf * xf, -1, keepdims=True) + eps) * g.astype(f32)).astype(x.dtype)


def rope_tables(positions, dim):
    inv_freq = ROPE_THETA ** (-jnp.arange(0, dim, 2, dtype=f32) / dim)
    ang = positions.astype(f32)[..., None] * inv_freq
    return jnp.cos(ang)[:, :, None, :], jnp.sin(ang)[:, :, None, :]


def apply_rope(x, cos, sin):
    x1, x2 = jnp.split(x.astype(f32), 2, axis=-1)
    return jnp.concatenate([x1 * cos - x2 * sin, x2 * cos + x1 * sin], axis=-1).astype(x.dtype)


def partial_rope(x, cos, sin):
    r = 2 * cos.shape[-1]
    return jnp.concatenate([apply_rope(x[..., :r], cos, sin), x[..., r:]], axis=-1)


def _to_blocks(t):
    b, s = t.shape[:2]
    return jnp.swapaxes(t.reshape(b, s // Q_BLOCK, Q_BLOCK, *t.shape[2:]), 0, 1)


def _from_blocks(t):
    t = jnp.swapaxes(t, 0, 1)
    return t.reshape(t.shape[0], -1, *t.shape[3:])


def diff_attention(q, k, v, lam):
    scale = q.shape[-1] ** -0.5

    def block(qb):
        s = jnp.einsum('bqhmd,bkhmd->bhmqk', qb, k).astype(f32) * scale
        p = jax.nn.softmax(s, axis=-1)
        a = p[:, :, 0] - lam * p[:, :, 1]
        return jnp.einsum('bhqk,bkhe->bqhe', a.astype(v.dtype), v)

    return _from_blocks(lax.map(block, _to_blocks(q)))


def mla_attention(q_nope, q_rope, k_nope, k_rope, v):
    scale = (q_nope.shape[-1] + q_rope.shape[-1]) ** -0.5

    def block(qs):
        qn, qr = qs
        s = (jnp.einsum('bqhd,bkhd->bhqk', qn, k_nope)
             + jnp.einsum('bqhr,bkr->bhqk', qr, k_rope)).astype(f32) * scale
        p = jax.nn.softmax(s, axis=-1)
        return jnp.einsum('bhqk,bkhe->bqhe', p.astype(v.dtype), v)

    return _from_blocks(lax.map(block, (_to_blocks(q_nope), _to_blocks(q_rope))))


def _to_sub(t, d):
    b, s = t.shape[:2]
    rest = t.shape[2:]
    return jnp.swapaxes(t.reshape(b, s // d, d, *rest), 1, 2).reshape(b * d, s // d, *rest)


def _from_sub(t, b, d):
    l = t.shape[1]
    rest = t.shape[2:]
    return jnp.swapaxes(t.reshape(b, d, l, *rest), 1, 2).reshape(b, l * d, *rest)


def dilated_window_attention(q, k, v, dilation, n_side):
    b, s, h, dk = q.shape
    L = s // dilation
    nb = -(-L // n_side)
    lp = nb * n_side
    bd = b * dilation
    qs, ks, vs = (_to_sub(t, dilation) for t in (q, k, v))
    qs = jnp.pad(qs, ((0, 0), (0, lp - L), (0, 0), (0, 0))).reshape(bd, nb, n_side, h, dk)

    def band(t):
        tp = jnp.pad(t, ((0, 0), (n_side, lp - L + n_side), (0, 0), (0, 0)))
        tp = tp.reshape(bd, nb + 2, n_side, h, t.shape[-1])
        return jnp.concatenate([tp[:, :-2], tp[:, 1:-1], tp[:, 2:]], axis=2)

    kb, vb = band(ks), band(vs)
    scores = jnp.einsum('bnqhd,bnkhd->bnhqk', qs, kb).astype(f32) * dk ** -0.5
    qi = jnp.arange(n_side)[:, None]
    kc = jnp.arange(3 * n_side)[None, :]
    in_band = jnp.abs(kc - n_side - qi) <= n_side
    kpos = jnp.arange(nb)[:, None] * n_side - n_side + kc
    valid = (kpos >= 0) & (kpos < L)
    mask = in_band[None] & valid[:, None]
    scores = jnp.where(mask[None, :, None], scores, NEG_INF)
    lse = jax.nn.logsumexp(scores, axis=-1)
    p = jnp.exp(scores - lse[..., None])
    o = jnp.einsum('bnhqk,bnkhe->bnqhe', p.astype(v.dtype), vb).reshape(bd, lp, h, -1)[:, :L]
    lse = jnp.swapaxes(lse, 2, 3).reshape(bd, lp, h)[:, :L]
    return _from_sub(o, b, dilation), _from_sub(lse, b, dilation)


def hybrid_mixer(x, layer, cos_p, sin_p, cos_m, sin_m, w_in, b_gate, lam_q1, lam_k1, lam_q2, lam_k2,
                 diff_norm_g, q_norm_g, kv_norm_g, w_qb, w_kvb, w_br_a, w_br_b, w_br_c, w_out):
    B, S, D = x.shape
    h = x @ w_in
    a_q, a_k, a_v, b_cq, b_ckv, b_kr, c_q, c_k, c_v, g = jnp.split(h, IN_SPLITS, axis=-1)

    aq = partial_rope(a_q.reshape(B, S, DIFF_HEADS * 2, DIFF_QK_DIM), cos_p, sin_p)
    ak = partial_rope(a_k.reshape(B, S, DIFF_HEADS * 2, DIFF_QK_DIM), cos_p, sin_p)
    aq = aq.reshape(B, S, DIFF_HEADS, 2, DIFF_QK_DIM)
    ak = ak.reshape(B, S, DIFF_HEADS, 2, DIFF_QK_DIM)
    av = a_v.reshape(B, S, DIFF_HEADS, DIFF_V_DIM)
    lam_init = 0.8 - 0.6 * math.exp(-0.3 * layer)
    lam = (jnp.exp(jnp.sum(lam_q1.astype(f32) * lam_k1.astype(f32)))
           - jnp.exp(jnp.sum(lam_q2.astype(f32) * lam_k2.astype(f32))) + lam_init)
    out_a = diff_attention(aq, ak, av, lam)
    out_a = (rms_norm(out_a, diff_norm_g) * (1.0 - lam_init)).reshape(B, S, BRANCH_A)

    q_full = (rms_norm(b_cq, q_norm_g) @ w_qb).reshape(B, S, MLA_HEADS, MLA_NOPE_DIM + MLA_ROPE_DIM)
    q_nope, q_rope = q_full[..., :MLA_NOPE_DIM], apply_rope(q_full[..., MLA_NOPE_DIM:], cos_m, sin_m)
    kv = (rms_norm(b_ckv, kv_norm_g) @ w_kvb).reshape(B, S, MLA_HEADS, MLA_NOPE_DIM + MLA_V_DIM)
    k_nope, v_b = kv[..., :MLA_NOPE_DIM], kv[..., MLA_NOPE_DIM:]
    k_rope = apply_rope(b_kr[:, :, None, :], cos_m, sin_m)[:, :, 0]
    out_b = mla_attention(q_nope, q_rope, k_nope, k_rope, v_b).reshape(B, S, BRANCH_B)

    cq = partial_rope(c_q.reshape(B, S, DIL_GROUPS * DIL_HEADS, DIL_QK_DIM), cos_p, sin_p)
    ck = partial_rope(c_k.reshape(B, S, DIL_GROUPS * DIL_HEADS, DIL_QK_DIM), cos_p, sin_p)
    cq = cq.reshape(B, S, DIL_GROUPS, DIL_HEADS, DIL_QK_DIM)
    ck = ck.reshape(B, S, DIL_GROUPS, DIL_HEADS, DIL_QK_DIM)
    cv = c_v.reshape(B, S, DIL_GROUPS, DIL_HEADS, DIL_V_DIM)
    outs, lses = [], []
    for gi, (window, dilation) in enumerate(DIL_PATTERNS):
        o, lse = dilated_window_attention(cq[:, :, gi], ck[:, :, gi], cv[:, :, gi],
                                          dilation, (window // 2) // dilation)
        outs.append(o)
        lses.append(lse)
    w_grp = jax.nn.softmax(jnp.stack(lses, axis=-1), axis=-1)
    out_c = jnp.einsum('bshg,bshge->bshe', w_grp, jnp.stack(outs, axis=3).astype(f32))
    out_c = out_c.astype(x.dtype).reshape(B, S, BRANCH_C)

    gates = jax.nn.sigmoid((g + b_gate).astype(f32)).astype(x.dtype).reshape(B, S, N_BRANCHES, D)
    y = (gates[:, :, 0] * (out_a @ w_br_a) + gates[:, :, 1] * (out_b @ w_br_b)
         + gates[:, :, 2] * (out_c @ w_br_c))
    return y @ w_out


def moe_ffn(x, router_w, router_bias, w_g, w_u, w_d, w_sg, w_su, w_sd):
    B, S, D = x.shape
    xt = x.reshape(-1, D)
    scores = jax.nn.sigmoid((xt @ router_w).astype(f32))
    biased = scores + router_bias.astype(f32)
    grp = biased.reshape(-1, N_EXPERT_GROUPS, EXPERTS_PER_GROUP)
    grp_score = lax.top_k(grp, 2)[0].sum(-1)
    gsel = lax.top_k(grp_score, TOPK_GROUPS)[1]
    gmask = jax.nn.one_hot(gsel, N_EXPERT_GROUPS, dtype=f32).sum(1) > 0
    emask = jnp.repeat(gmask, EXPERTS_PER_GROUP, axis=-1)
    eidx = lax.top_k(jnp.where(emask, biased, -jnp.inf), TOP_K)[1]
    wsel = jnp.take_along_axis(scores, eidx, axis=-1)
    wsel = wsel / wsel.sum(-1, keepdims=True)
    gate = (jax.nn.one_hot(eidx, N_EXPERTS, dtype=f32) * wsel[..., None]).sum(1)
    routed = jnp.zeros(xt.shape, f32)
    for e in range(N_EXPERTS):
        hid = jax.nn.silu(xt @ w_g[e]) * (xt @ w_u[e])
        routed = routed + gate[:, e:e + 1] * (hid @ w_d[e]).astype(f32)
    shared = (jax.nn.silu(xt @ w_sg) * (xt @ w_su)) @ w_sd
    return (routed.astype(x.dtype) + shared).reshape(B, S, D)


def setup_inputs(seed: int = 0) -> dict:
    key = jax.random.key(seed)
    ks = iter(jax.random.split(key, 40))

    def nrm(shape, scale):
        return scale * jax.random.normal(next(ks), shape, f32)

    def gain(shape):
        return 1.0 + nrm(shape, 0.02)

    L, D = DEPTH, D_MODEL
    x = jax.random.normal(next(ks), (BATCH, SEQ, D), f32)
    offset = jax.random.randint(next(ks), (BATCH, 1), 0, 65536)
    positions = (offset + jnp.arange(SEQ)[None, :]).astype(jnp.int32)
    return {
        'x': x,
        'positions': positions,
        'ln_in_g': gain((D,)),
        'ln_in_b': nrm((D,), 0.02),
        'w_in': nrm((L, D, IN_WIDTH), D ** -0.5),
        'b_gate': nrm((L, N_BRANCHES * D), 0.1),
        'lam_q1': nrm((L, DIFF_QK_DIM), 0.1),
        'lam_k1': nrm((L, DIFF_QK_DIM), 0.1),
        'lam_q2': nrm((L, DIFF_QK_DIM), 0.1),
        'lam_k2': nrm((L, DIFF_QK_DIM), 0.1),
        'diff_norm_g': gain((L, DIFF_V_DIM)),
        'mla_q_norm_g': gain((L, MLA_Q_RANK)),
        'mla_kv_norm_g': gain((L, MLA_KV_RANK)),
        'w_mla_qb': nrm((L, MLA_Q_RANK, MLA_HEADS * (MLA_NOPE_DIM + MLA_ROPE_DIM)), MLA_Q_RANK ** -0.5),
        'w_mla_kvb': nrm((L, MLA_KV_RANK, MLA_HEADS * (MLA_NOPE_DIM + MLA_V_DIM)), MLA_KV_RANK ** -0.5),
        'w_branch_a': nrm((L, BRANCH_A, D), BRANCH_A ** -0.5),
        'w_branch_b': nrm((L, BRANCH_B, D), BRANCH_B ** -0.5),
        'w_branch_c': nrm((L, BRANCH_C, D), BRANCH_C ** -0.5),
        'w_out': nrm((L, D, D), D ** -0.5 * DEEPNORM_BETA),
        'ln1_g': gain((L, D)),
        'ln1_b': nrm((L, D), 0.02),
        'router_w': nrm((D, N_EXPERTS), D ** -0.5),
        'router_bias': nrm((N_EXPERTS,), 0.01),
        'w_exp_gate': nrm((L, N_EXPERTS, D, EXPERT_HIDDEN), D ** -0.5),
        'w_exp_up': nrm((L, N_EXPERTS, D, EXPERT_HIDDEN), D ** -0.5),
        'w_exp_down': nrm((L, N_EXPERTS, EXPERT_HIDDEN, D), EXPERT_HIDDEN ** -0.5 * DEEPNORM_BETA),
        'w_sh_gate': nrm((L, D, SHARED_HIDDEN), D ** -0.5),
        'w_sh_up': nrm((L, D, SHARED_HIDDEN), D ** -0.5),
        'w_sh_down': nrm((L, SHARED_HIDDEN, D), SHARED_HIDDEN ** -0.5 * DEEPNORM_BETA),
        'ln2_g': gain((L, D)),
        'ln2_b': nrm((L, D), 0.02),
    }


def reference(x, positions, ln_in_g, ln_in_b, w_in, b_gate, lam_q1, lam_k1, lam_q2, lam_k2,
              diff_norm_g, mla_q_norm_g, mla_kv_norm_g, w_mla_qb, w_mla_kvb, w_branch_a, w_branch_b,
              w_branch_c, w_out, ln1_g, ln1_b, router_w, router_bias, w_exp_gate, w_exp_up,
              w_exp_down, w_sh_gate, w_sh_up, w_sh_down, ln2_g, ln2_b):
    cos_p, sin_p = rope_tables(positions, ROT_DIM)
    cos_m, sin_m = rope_tables(positions, MLA_ROPE_DIM)
    x = layer_norm(x, ln_in_g, ln_in_b)
    for l in range(DEPTH):
        mix = hybrid_mixer(x, l, cos_p, sin_p, cos_m, sin_m, w_in[l], b_gate[l],
                           lam_q1[l], lam_k1[l], lam_q2[l], lam_k2[l], diff_norm_g[l],
                           mla_q_norm_g[l], mla_kv_norm_g[l], w_mla_qb[l], w_mla_kvb[l],
                           w_branch_a[l], w_branch_b[l], w_branch_c[l], w_out[l])
        x = layer_norm(DEEPNORM_ALPHA * x + mix, ln1_g[l], ln1_b[l])
        ffn = moe_ffn(x, router_w, router_bias, w_exp_gate[l], w_exp_up[l], w_exp_down[l],
                      w_sh_gate[l], w_sh_up[l], w_sh_down[l])
        x = layer_norm(DEEPNORM_ALPHA * x + ffn, ln2_g[l], ln2_b[l])
    return x
```

```python
import contextlib
import math
import numpy as np
import concourse.bass as bass
import concourse.mybir as mybir
from concourse.bass_utils import run_bass_kernel_spmd

F32 = mybir.dt.float32
BF16 = mybir.dt.bfloat16
I32 = mybir.dt.int32
AF = mybir.ActivationFunctionType
ALU = mybir.AluOpType
AX = mybir.AxisListType

S = 8192
D = 1024
NT = S // 128
TB = 512
NTB = S // TB
DEPTH = 2
INW = 8352
ALPHA = (2 * DEPTH) ** 0.25
THETA = 500000.0
TWO_PI = 2.0 * math.pi
C1 = 6.28125
C2 = TWO_PI - C1
NEG = -30000.0

O_AQ, O_AK, O_AV, O_BCQ, O_BCKV, O_BKR, O_CQ, O_CK, O_CV, O_G = (
    0, 512, 1024, 1536, 1920, 2176, 2208, 2976, 3744, 5280)

FM_BLOCKS = []
for i in range(4):
    FM_BLOCKS.append(("aq", i, O_AQ + 128 * i, 128))
for i in range(4):
    FM_BLOCKS.append(("ak", i, O_AK + 128 * i, 128))
for i in range(3):
    FM_BLOCKS.append(("bcq", i, O_BCQ + 128 * i, 128))
for i in range(2):
    FM_BLOCKS.append(("bckv", i, O_BCKV + 128 * i, 128))
FM_BLOCKS.append(("bkr", 0, O_BKR, 32))
for i in range(6):
    FM_BLOCKS.append(("cq", i, O_CQ + 128 * i, 128))
for i in range(6):
    FM_BLOCKS.append(("ck", i, O_CK + 128 * i, 128))
for i in range(24):
    FM_BLOCKS.append(("g", i, O_G + 128 * i, 128))
NFM = len(FM_BLOCKS)
TM_BLOCKS = [("av", 0, O_AV)] + [("cv", i, O_CV + 512 * i) for i in range(3)]

C_DELTA = (1, 2, 8)
C_DIL = (1, 4, 16)
C_MASKS = [(g, dl) for g in range(3) for dl in range(-C_DELTA[g], C_DELTA[g] + 1)]
RING = 20


class Buf:
    __slots__ = ("name", "w", "r", "dsem")

    def __init__(self, name):
        self.name = name
        self.w = None
        self.r = {}
        self.dsem = None


class K:
    def __init__(self, nc, es):
        self.nc = nc
        self.eng = {"pe": nc.tensor, "act": nc.scalar, "dve": nc.vector, "pool": nc.gpsimd, "sp": nc.sync}
        self.sem = {}
        self.cnt = {}
        for e in ("pe", "act", "dve", "pool"):
            self.sem[e] = es.enter_context(nc.semaphore("e_" + e))
            self.cnt[e] = 0
        self.waited = {e: {} for e in self.eng}
        self.pool_sems = []
        self.free_hw = []
        self.free_sw = []
        for i in range(88):
            self.pool_sems.append([es.enter_context(nc.semaphore("d%d" % i)), 0])
            (self.free_hw if i < 62 else self.free_sw).append(i)
        self.semkey = {}
        self.phase_bufs = []

    def buf(self, name):
        b = Buf(name)
        self.phase_bufs.append(b)
        return b

    def _key(self, sem):
        return id(sem)

    def _wait(self, e, deps):
        for (sem, val, owner) in deps:
            k = id(sem)
            if self.waited[e].get(k, 0) >= val:
                continue
            self.eng[e].wait_ge(sem, val)
            self.waited[e][k] = val

    def _deps(self, e, reads, writes, partial=False):
        deps = []
        for b in reads:
            if b.w is not None:
                if not (b.w[2] == e and e == "pe"):
                    deps.append(b.w)
        for b in writes:
            if b.w is not None and b.w[2] != e:
                if not (partial and b.w[2] == "dma"):
                    deps.append(b.w)
            for t in b.r.values():
                if t[2] != e:
                    deps.append(t)
        return deps

    def _commit(self, tok, reads, writes):
        for b in reads:
            b.r[id(tok[0])] = tok
        for b in writes:
            b.w = tok
            b.r = {}

    def op(self, e, fn, reads=(), writes=()):
        self._wait(e, self._deps(e, reads, writes))
        ins = fn(self.eng[e])
        ins.then_inc(self.sem[e], 1)
        self.cnt[e] += 1
        tok = (self.sem[e], self.cnt[e], e)
        self._commit(tok, reads, writes)
        return tok

    def mm_group(self, fns, reads=(), writes=()):
        self._wait("pe", self._deps("pe", reads, writes))
        ins = None
        for f in fns:
            ins = f(self.nc.tensor)
        ins.then_inc(self.sem["pe"], 1)
        self.cnt["pe"] += 1
        tok = (self.sem["pe"], self.cnt["pe"], "pe")
        self._commit(tok, reads, writes)
        return tok

    def dma(self, q, pairs, reads=(), writes=(), partial=False):
        self._wait(q, self._deps("dma", reads, writes, partial=partial))
        b = writes[0]
        if b.dsem is None:
            b.dsem = (self.free_sw if q == "pool" else self.free_hw).pop()
        assert (b.dsem >= 62) == (q == "pool"), "buffer %s mixes DMA queue kinds" % b.name
        ent = self.pool_sems[b.dsem]
        for (o, i) in pairs:
            self.eng[q].dma_start(out=o, in_=i).then_inc(ent[0], 16)
            ent[1] += 16
        tok = (ent[0], ent[1], "dma")
        self._commit(tok, reads, writes)
        return tok

    def barrier(self):
        deps = [(self.sem[e], self.cnt[e], e) for e in self.sem if self.cnt[e] > 0]
        for ent in self.pool_sems:
            if ent[1] > 0:
                deps.append((ent[0], ent[1], "dma"))
        for e in self.eng:
            self._wait(e, deps)
        for b in self.phase_bufs:
            if b.dsem is not None:
                (self.free_sw if b.dsem >= 62 else self.free_hw).append(b.dsem)
                b.dsem = None
            b.w = None
            b.r = {}
        self.phase_bufs = []


def _host_consts():
    c = {}
    c["ident_f"] = np.eye(128, dtype=np.float32)
    pa = np.zeros((128, 128), np.float32)
    for hh in range(2):
        for i in range(8):
            pa[hh * 64 + i + 8, hh * 64 + i] = 1.0
            pa[hh * 64 + i, hh * 64 + i + 8] = 1.0
    pb = np.zeros((128, 128), np.float32)
    for i in range(16):
        pb[64 + i + 16, 64 + i] = 1.0
        pb[64 + i, 64 + i + 16] = 1.0
    pk = np.zeros((128, 128), np.float32)
    for i in range(16):
        pk[i + 16, i] = 1.0
        pk[i, i + 16] = 1.0
    c["perm"] = np.stack([pa, pb, pk], 0)
    fs = np.zeros((128, 6), np.float32)
    invp = (THETA ** (-np.arange(0, 16, 2, dtype=np.float32) / np.float32(16))).astype(np.float32)
    invm = (THETA ** (-np.arange(0, 32, 2, dtype=np.float32) / np.float32(32))).astype(np.float32)
    for hh in range(2):
        for i in range(8):
            fs[hh * 64 + i, 0] = invp[i]
            fs[hh * 64 + i, 1] = -1.0
            fs[hh * 64 + 8 + i, 0] = invp[i]
            fs[hh * 64 + 8 + i, 1] = 1.0
    for i in range(16):
        fs[64 + i, 2] = invm[i]
        fs[64 + i, 3] = -1.0
        fs[80 + i, 2] = invm[i]
        fs[80 + i, 3] = 1.0
        fs[i, 4] = invm[i]
        fs[i, 5] = -1.0
        fs[16 + i, 4] = invm[i]
        fs[16 + i, 5] = 1.0
    c["fs"] = fs
    kk = np.arange(128)[:, None]
    qq = np.arange(128)[None, :]
    mb = np.zeros((128, len(C_MASKS), 128), np.float32)
    for mi, (g, dl) in enumerate(C_MASKS):
        d = C_DIL[g]
        diff = dl * 128 + kk - qq
        ok = (diff % d == 0) & (np.abs(diff) <= 64 * d)
        mb[:, mi, :] = np.where(ok, 1.0, 0.0)
    c["maskb"] = mb
    sel = np.zeros((16, 16, 128), np.float32)
    for e in range(16):
        sel[e, e, :] = 1.0
    c["selc"] = sel
    return c


def build_program(depth=DEPTH, dbg=None):
    nc = bass.Bass("TRN2", target_bir_lowering=False)
    dbg = dbg or {}

    def din(name, shape, dt=F32):
        return nc.dram_tensor(name, list(shape), dt, kind="ExternalInput").ap()

    def dscr(name, shape, dt):
        kind = "ExternalOutput" if name in dbg.get("outs", ()) else "Internal"
        return nc.dram_tensor(name, list(shape), dt, kind=kind).ap()

    x_in = din("x", [S, D])
    pos_in = din("positions", [1, S], I32)
    ln_in_g = din("ln_in_g", [D])
    ln_in_b = din("ln_in_b", [D])
    w_in = din("w_in", [DEPTH, D, INW])
    b_gate = din("b_gate", [DEPTH, 3 * D])
    lam_in = din("lam_all", [DEPTH, 4, 64])
    diff_g = din("diff_norm_g", [DEPTH, 128])
    q_norm_g = din("mla_q_norm_g", [DEPTH, 384])
    kv_norm_g = din("mla_kv_norm_g", [DEPTH, 256])
    w_qb = din("w_mla_qb", [DEPTH, 384, 768])
    w_kvb = din("w_mla_kvb", [DEPTH, 256, 1024])
    w_br = din("w_branch", [DEPTH, 3, 512, D])
    w_out = din("w_out", [DEPTH, D, D])
    ln1_g = din("ln1_g", [DEPTH, D])
    ln1_b = din("ln1_b", [DEPTH, D])
    router_w = din("router_w", [D, 16])
    router_bias = din("router_bias_t", [NT * 16])
    w_eg = din("w_eg", [DEPTH, 17, D, 256])
    w_eu = din("w_eu", [DEPTH, 17, D, 256])
    w_ed = din("w_ed", [DEPTH, 17, 256, D])
    ln2_g = din("ln2_g", [DEPTH, D])
    ln2_b = din("ln2_b", [DEPTH, D])
    c_ident = din("c_ident_f", [128, 128])
    c_perm = din("c_perm", [3, 128, 128])
    c_fs = din("c_fs", [128, 6])
    c_maskb = din("c_maskb", [128, len(C_MASKS), 128])
    c_selc = din("c_selc", [16, 16, 128])
    out = nc.dram_tensor("out", [S, D], F32, kind="ExternalOutput").ap()

    xres = dscr("xres", [S, D], F32)
    x1res = dscr("x1res", [S, D], F32)
    xT = dscr("xT", [128, 8, S], BF16)
    x1T = dscr("x1T", [128, 8, S], BF16)
    tabs = dscr("tabs", [6, 128, S], F32)
    WS = []
    for l_ in range(DEPTH):
        WS.append(dict(
            w1=dscr("w1_%d" % l_, [NFM, 128, 8, 128], BF16), w1v=dscr("w1v_%d" % l_, [4, 128, 8, 512], BF16),
            wqb_s=dscr("wqb_s_%d" % l_, [128, 3, 768], BF16), wkvk_s=dscr("wkvk_s_%d" % l_, [128, 2, 512], BF16),
            wkvv_s=dscr("wkvv_s_%d" % l_, [128, 2, 512], BF16), wbr_s=dscr("wbr_s_%d" % l_, [128, 12, D], BF16),
            wout_s=dscr("wout_s_%d" % l_, [128, 8, D], BF16), weg_s=dscr("weg_s_%d" % l_, [17, 128, 8, 256], BF16),
            weu_s=dscr("weu_s_%d" % l_, [17, 128, 8, 256], BF16), wed_s=dscr("wed_s_%d" % l_, [128, 34, D], BF16)))
    qA = dscr("qA", [4, 128, S], BF16)
    kA = dscr("kA", [4, 128, S], BF16)
    vA = dscr("vA", [4, 128, NT, 129], BF16)
    qB = dscr("qB", [8, 96, S], BF16)
    kB = dscr("kB", [8, 96, S], BF16)
    vB = dscr("vB", [8, 128, NT, 65], BF16)
    qC = dscr("qC", [6, 128, S], BF16)
    kC = dscr("kC", [6, 128, S], BF16)
    vC = dscr("vC", [S, 12 * 129], BF16)
    gT = dscr("gT", [24, 128, S], BF16)
    oT = dscr("oT", [12, 128, S], BF16)
    gateT = dscr("gateT", [16, S], F32)

    stop_after = dbg.get("stop_after", None)

    with contextlib.ExitStack() as es:
        es.enter_context(nc.allow_non_contiguous_dma(reason="small strided parameter loads"))
        k = K(nc, es)
        uniq = [0]

        def sb(stack, name, shape, dt):
            uniq[0] += 1
            return stack.enter_context(nc.sbuf_tensor("%s_%d" % (name, uniq[0]), list(shape), dt))

        def ps(stack, name, shape, dt=F32):
            uniq[0] += 1
            return stack.enter_context(nc.psum_tensor("%s_%d" % (name, uniq[0]), list(shape), dt))

        ident_f = sb(es, "ident_f", [128, 128], F32)
        ident_b = sb(es, "ident_b", [128, 128], BF16)
        ones_f = sb(es, "ones_f", [128, 128], F32)
        perm_b = sb(es, "perm_b", [128, 3, 128], BF16)
        fs_t = sb(es, "fs_t", [128, 6], F32)
        eps5 = sb(es, "eps5", [128, 1], F32)
        eps6 = sb(es, "eps6", [128, 1], F32)
        pi_t = sb(es, "pi_t", [128, 1], F32)
        scores_all = sb(es, "scores_all", [128, NT, 16], F32)
        B_const = k.buf("const")
        B_scores = Buf("scores")
        perm_f = sb(es, "perm_f", [128, 3, 128], F32)
        k.dma("sp", [(ident_f[:], c_ident[:, :]), (fs_t[:], c_fs[:, :]),
                     (perm_f[:], c_perm.rearrange("a p n -> p a n"))], writes=[B_const])
        k.op("dve", lambda e: e.tensor_copy(out=ident_b[:], in_=ident_f[:]), reads=[B_const], writes=[B_const])
        k.op("dve", lambda e: e.tensor_copy(out=perm_b[:], in_=perm_f[:]), reads=[B_const], writes=[B_const])
        k.op("pool", lambda e: e.memset(ones_f[:], 1.0), writes=[B_const])
        k.op("pool", lambda e: e.memset(eps5[:], 1e-5), writes=[B_const])
        k.op("pool", lambda e: e.memset(eps6[:], 1e-6), writes=[B_const])
        k.op("pool", lambda e: e.memset(pi_t[:], math.pi), writes=[B_const])
        k.barrier()

        def finish_tile(st, pfx, rbuf, r_t, gam, bet, B_gb, tidx, res_dram, xT_stage, B_xTs, psT, B_psT,
                        router=None, final_out=None, defer_tr=False):
            ring = st["ring"][tidx % len(st["ring"])]
            stats, mv, B_st = ring
            k.op("dve", lambda e: e.bn_stats(out=stats[:, 0, :], in_=r_t[:, 0:512]), reads=[rbuf], writes=[B_st])
            k.op("dve", lambda e: e.bn_stats(out=stats[:, 1, :], in_=r_t[:, 512:1024]), reads=[rbuf], writes=[B_st])
            k.op("dve", lambda e: e.bn_aggr(out=mv[:, 0:2], in_=stats[:]), reads=[B_st], writes=[B_st])
            k.op("act", lambda e: e.activation(out=mv[:, 2:3], in_=mv[:, 1:2], func=AF.Sqrt, bias=eps5[:, 0:1], scale=1.0),
                 reads=[B_st], writes=[B_st])
            k.op("dve", lambda e: e.reciprocal(out=mv[:, 3:4], in_=mv[:, 2:3]), reads=[B_st], writes=[B_st])
            k.op("dve", lambda e: e.tensor_scalar(out=r_t[:], in0=r_t[:], scalar1=mv[:, 0:1], scalar2=mv[:, 3:4],
                                                   op0=ALU.subtract, op1=ALU.mult), reads=[B_st, rbuf], writes=[rbuf])
            k.op("pool", lambda e: e.tensor_tensor(out=r_t[:], in0=r_t[:], in1=gam[:], op=ALU.mult),
                 reads=[rbuf, B_gb], writes=[rbuf])
            k.op("pool", lambda e: e.tensor_tensor(out=r_t[:], in0=r_t[:], in1=bet[:], op=ALU.add),
                 reads=[rbuf, B_gb], writes=[rbuf])
            if final_out is not None:
                k.dma("sp", [(final_out[tidx * 128:(tidx + 1) * 128, :], r_t[:])], reads=[rbuf], writes=[st["B_out"]],
                      partial=True)
                return
            k.dma("sp", [(res_dram[tidx * 128:(tidx + 1) * 128, :], r_t[:])], reads=[rbuf], writes=[st["B_res"]],
                  partial=True)
            if defer_tr:
                return lambda: finish_tr(rbuf, r_t, tidx, xT_stage, B_xTs, psT, B_psT, router)
            finish_tr(rbuf, r_t, tidx, xT_stage, B_xTs, psT, B_psT, router)

        def finish_tr(rbuf, r_t, tidx, xT_stage, B_xTs, psT, B_psT, router):
            j = tidx % 4
            for half in range(2):
                k.mm_group([(lambda e, c=c: e.transpose(out=psT[:, (c % 4) * 128:(c % 4 + 1) * 128],
                                                         in_=r_t[:, c * 128:(c + 1) * 128], identity=ident_f[:]))
                            for c in range(half * 4, half * 4 + 4)], reads=[rbuf], writes=[B_psT])
                if router is not None:
                    xf = router["xf"][tidx % 2]
                    Bxf_ = router["B_xf"][tidx % 2]
                    k.op("dve", lambda e, half=half: e.tensor_copy(
                        out=xf[:, half * 4:half * 4 + 4, :],
                        in_=psT[:].rearrange("p (c t) -> p c t", c=4)), reads=[B_psT], writes=[Bxf_])
                    k.op("pool", lambda e, half=half: e.tensor_copy(
                        out=xT_stage[:, half * 4:half * 4 + 4, j * 128:(j + 1) * 128],
                        in_=xf[:, half * 4:half * 4 + 4, :]), reads=[Bxf_], writes=[B_xTs])
                else:
                    k.op("act", lambda e, half=half: e.activation(
                        out=xT_stage[:, half * 4:half * 4 + 4, j * 128:(j + 1) * 128],
                        in_=psT[:].rearrange("p (c t) -> p c t", c=4), func=AF.Copy), reads=[B_psT], writes=[B_xTs])
            if router is not None:
                xf = router["xf"][tidx % 2]
                Bxf_ = router["B_xf"][tidx % 2]
                k.mm_group([(lambda e, c=c: e.matmul(out=router["ps"][:, 0:16], lhsT=xf[:, c, :], rhs=router["w"][:, c, :],
                                                      start=(c == 0), stop=(c == 7))) for c in range(8)],
                           reads=[Bxf_, B_const], writes=[router["B_ps"]])
                k.op("act", lambda e: e.activation(out=scores_all[:, tidx, :], in_=router["ps"][:, 0:16], func=AF.Sigmoid),
                     reads=[router["B_ps"]], writes=[B_scores])

        def ln_setup(stack, pfx):
            st = {}
            st["ring"] = [(sb(stack, pfx + "stats%d" % i, [128, 2, 6], F32), sb(stack, pfx + "mv%d" % i, [128, 4], F32),
                           k.buf(pfx + "st%d" % i)) for i in range(3)]
            st["B_res"] = k.buf(pfx + "res")
            st["B_out"] = k.buf(pfx + "out")
            return st

        def load_gb(stack, pfx, g_ap, b_ap):
            gam = sb(stack, pfx + "gam", [128, D], F32)
            bet = sb(stack, pfx + "bet", [128, D], F32)
            B_gb = k.buf(pfx + "gb")
            k.dma("sp", [(gam[:], g_ap.partition_broadcast(128)), (bet[:], b_ap.partition_broadcast(128))], writes=[B_gb])
            return gam, bet, B_gb

        def phase_tables():
            with contextlib.ExitStack() as ph:
                posi = sb(ph, "posi", [128, 1024], I32)
                posf = sb(ph, "posf", [128, 1024], F32)
                t_ang = sb(ph, "t_ang", [128, 1024], F32)
                t_k = sb(ph, "t_k", [128, 1024], F32)
                t_n = sb(ph, "t_n", [128, 1024], F32)
                t_ni = sb(ph, "t_ni", [128, 1024], I32)
                t_r = sb(ph, "t_r", [128, 1024], F32)
                t_a = sb(ph, "t_a", [128, 1024], F32)
                t_o = sb(ph, "t_o", [128, 2, 1024], F32)
                B_pos = k.buf("pos"); B_w = k.buf("tw"); B_o = k.buf("to"); B_tab = k.buf("tabs")
                early0, late0 = wprep_jobs(0, ph)
                late_jobs[0] = late0
                for job in early0:
                    job()
                for tb in range(8):
                    sl = slice(tb * 1024, (tb + 1) * 1024)
                    k.dma("sp", [(posi[:], pos_in[0, sl].partition_broadcast(128))], writes=[B_pos])
                    k.op("dve", lambda e: e.tensor_copy(out=posf[:], in_=posi[:]), reads=[B_pos], writes=[B_w])
                    for s in range(3):
                        k.op("dve", lambda e: e.tensor_scalar(out=t_ang[:], in0=posf[:], scalar1=fs_t[:, 2 * s:2 * s + 1],
                                                               scalar2=None, op0=ALU.mult), reads=[B_w], writes=[B_w])
                        k.op("dve", lambda e: e.tensor_scalar(out=t_k[:], in0=t_ang[:], scalar1=1.0 / TWO_PI, scalar2=None,
                                                               op0=ALU.mult), reads=[B_w], writes=[B_w])
                        k.op("dve", lambda e: e.tensor_copy(out=t_ni[:], in_=t_k[:]), reads=[B_w], writes=[B_w])
                        k.op("dve", lambda e: e.tensor_copy(out=t_n[:], in_=t_ni[:]), reads=[B_w], writes=[B_w])
                        k.op("dve", lambda e: e.scalar_tensor_tensor(out=t_r[:], in0=t_n[:], scalar=-C1, in1=t_ang[:],
                                                                      op0=ALU.mult, op1=ALU.add), reads=[B_w], writes=[B_w])
                        k.op("dve", lambda e: e.scalar_tensor_tensor(out=t_r[:], in0=t_n[:], scalar=-C2, in1=t_r[:],
                                                                      op0=ALU.mult, op1=ALU.add), reads=[B_w], writes=[B_w])
                        k.op("dve", lambda e: e.tensor_scalar(out=t_a[:], in0=t_r[:], scalar1=0.0, scalar2=None,
                                                               op0=ALU.is_lt), reads=[B_w], writes=[B_w])
                        k.op("dve", lambda e: e.scalar_tensor_tensor(out=t_r[:], in0=t_a[:], scalar=TWO_PI, in1=t_r[:],
                                                                      op0=ALU.mult, op1=ALU.add), reads=[B_w], writes=[B_w])
                        k.op("dve", lambda e: e.tensor_scalar(out=t_n[:], in0=t_r[:], scalar1=math.pi / 2, scalar2=None,
                                                               op0=ALU.add), reads=[B_w], writes=[B_w])
                        k.op("dve", lambda e: e.tensor_scalar(out=t_a[:], in0=t_n[:], scalar1=TWO_PI, scalar2=None,
                                                               op0=ALU.is_ge), reads=[B_w], writes=[B_w])
                        k.op("dve", lambda e: e.scalar_tensor_tensor(out=t_a[:], in0=t_a[:], scalar=-TWO_PI, in1=t_n[:],
                                                                      op0=ALU.mult, op1=ALU.add), reads=[B_w], writes=[B_w])
                        k.op("dve", lambda e: e.tensor_scalar(out=t_a[:], in0=t_a[:], scalar1=-1.0, scalar2=math.pi,
                                                               op0=ALU.mult, op1=ALU.add), reads=[B_w], writes=[B_w])
                        k.op("dve", lambda e: e.tensor_scalar(out=t_a[:], in0=t_a[:], scalar1=math.pi, scalar2=-math.pi,
                                                               op0=ALU.min, op1=ALU.max), reads=[B_w], writes=[B_w])
                        k.op("act", lambda e: e.activation(out=t_o[:, 0, :], in_=t_a[:], func=AF.Sin),
                             reads=[B_w], writes=[B_o])
                        k.op("dve", lambda e: e.tensor_scalar(out=t_k[:], in0=t_r[:], scalar1=-1.0, scalar2=math.pi,
                                                               op0=ALU.mult, op1=ALU.add), reads=[B_w], writes=[B_w])
                        k.op("dve", lambda e: e.tensor_scalar(out=t_k[:], in0=t_k[:], scalar1=math.pi, scalar2=-math.pi,
                                                               op0=ALU.min, op1=ALU.max), reads=[B_w], writes=[B_w])
                        k.op("act", lambda e: e.activation(out=t_o[:, 1, :], in_=t_k[:], func=AF.Sin),
                             reads=[B_w, B_o], writes=[B_o])
                        k.op("dve", lambda e: e.tensor_scalar(out=t_o[:, 1, :], in0=t_o[:, 1, :],
                                                               scalar1=fs_t[:, 2 * s + 1:2 * s + 2], scalar2=None, op0=ALU.mult),
                             reads=[B_o], writes=[B_o])
                        k.dma("sp", [(tabs[2 * s:2 * s + 2, :, sl].rearrange("a p t -> p a t"), t_o[:])],
                              reads=[B_o], writes=[B_tab], partial=True)
                k.barrier()

        def phase_ln_in():
            with contextlib.ExitStack() as ph:
                st = ln_setup(ph, "l0")
                gam, bet, B_gb = load_gb(ph, "l0", ln_in_g, ln_in_b)
                rt = [sb(ph, "l0r%d" % i, [128, D], F32) for i in range(3)]
                Br = [k.buf("l0r%d" % i) for i in range(3)]
                xs = [sb(ph, "l0xs%d" % i, [128, 8, 512], BF16) for i in range(2)]
                Bxs = [k.buf("l0xs%d" % i) for i in range(2)]
                psT = ps(ph, "l0psT", [128, 512])
                B_psT = k.buf("l0psT")
                B_xT = k.buf("xT")
                for t in range(NT):
                    i = t % 3
                    k.dma("sp", [(rt[i][:], x_in[t * 128:(t + 1) * 128, :])], writes=[Br[i]])
                    sidx = (t // 4) % 2
                    finish_tile(st, "l0", Br[i], rt[i], gam, bet, B_gb, t, xres, xs[sidx], Bxs[sidx], psT, B_psT)
                    if t % 4 == 3:
                        k.dma("pool", [(xT[:, :, (t // 4) * 512:(t // 4 + 1) * 512], xs[sidx][:])], reads=[Bxs[sidx]],
                              writes=[B_xT], partial=True)
                k.barrier()

        B_wprep = [Buf("wprep%d" % i) for i in range(DEPTH)]
        late_jobs = {}

        def wprep_jobs(l, ph):
            W = WS[l]
            B_w = B_wprep[l]
            jobs = []
            pairs = []
            for bi, (kind, idx, col0, ncol) in enumerate(FM_BLOCKS):
                for c in range(8):
                    pairs.append((W["w1"][bi, :, c, 0:ncol], w_in[l, c * 128:(c + 1) * 128, col0:col0 + ncol]))
            for bi, (kind, idx, col0) in enumerate(TM_BLOCKS):
                for c in range(8):
                    pairs.append((W["w1v"][bi, :, c, :], w_in[l, c * 128:(c + 1) * 128, col0:col0 + 512]))
            for br in range(3):
                for c in range(4):
                    pairs.append((W["wbr_s"][:, br * 4 + c, :], w_br[l, br, c * 128:(c + 1) * 128, :]))
            for c in range(8):
                pairs.append((W["wout_s"][:, c, :], w_out[l, c * 128:(c + 1) * 128, :]))
            for e_ in range(17):
                for c in range(8):
                    pairs.append((W["weg_s"][e_, :, c, :], w_eg[l, e_, c * 128:(c + 1) * 128, :]))
                    pairs.append((W["weu_s"][e_, :, c, :], w_eu[l, e_, c * 128:(c + 1) * 128, :]))
                for hc in range(2):
                    pairs.append((W["wed_s"][:, e_ * 2 + hc, :], w_ed[l, e_, hc * 128:(hc + 1) * 128, :]))
            n_early = (NFM + 4) * 8
            early, late = [], []
            for i in range(0, len(pairs), 8):
                (early if i < n_early else late).append(
                    lambda i=i: k.dma("pool", pairs[i:i + 8], writes=[B_w], partial=True))

            def mla_job():
                wq_f = sb(ph, "wq_f", [128, 3, 768], F32)
                wk_f = sb(ph, "wk_f", [128, 2, 1024], F32)
                gq = sb(ph, "gq", [128, 3], F32)
                gk = sb(ph, "gk", [128, 2], F32)
                wq_b = sb(ph, "wq_b", [128, 3, 768], BF16)
                wkk_b = sb(ph, "wkk_b", [128, 2, 512], BF16)
                wkv_b = sb(ph, "wkv_b", [128, 2, 512], BF16)
                B_l = k.buf("wp_l"); B_o = k.buf("wp_o")
                k.dma("sp", [(wq_f[:], w_qb[l].rearrange("(c p) n -> p c n", p=128)),
                             (wk_f[:], w_kvb[l].rearrange("(c p) n -> p c n", p=128)),
                             (gq[:], q_norm_g[l].rearrange("(c p) -> p c", p=128)),
                             (gk[:], kv_norm_g[l].rearrange("(c p) -> p c", p=128))], writes=[B_l])
                for c in range(3):
                    k.op("dve", lambda e, c=c: e.tensor_scalar(out=wq_b[:, c, :], in0=wq_f[:, c, :], scalar1=gq[:, c:c + 1],
                                                               scalar2=None, op0=ALU.mult), reads=[B_l], writes=[B_o])
                for c in range(2):
                    src = wk_f[:, c, :].rearrange("p (h t e) -> p h t e", h=8, t=2)
                    k.op("dve", lambda e, c=c, src=src: e.tensor_scalar(
                        out=wkk_b[:, c, :].rearrange("p (h e) -> p h e", h=8), in0=src[:, :, 0, :],
                        scalar1=gk[:, c:c + 1], scalar2=None, op0=ALU.mult), reads=[B_l], writes=[B_o])
                    k.op("dve", lambda e, c=c, src=src: e.tensor_scalar(
                        out=wkv_b[:, c, :].rearrange("p (h e) -> p h e", h=8), in0=src[:, :, 1, :],
                        scalar1=gk[:, c:c + 1], scalar2=None, op0=ALU.mult), reads=[B_l], writes=[B_o])
                k.dma("pool", [(W["wqb_s"][:, :, :], wq_b[:]), (W["wkvk_s"][:, :, :], wkk_b[:]),
                             (W["wkvv_s"][:, :, :], wkv_b[:])], reads=[B_o], writes=[B_w], partial=True)

            early.append(mla_job)
            return early, late

        def phase_proj(l):
            with contextlib.ExitStack() as ph:
                wqb_t = sb(ph, "p1wqb", [128, 3, 768], BF16)
                wkk_t = sb(ph, "p1wkk", [128, 2, 512], BF16)
                wkv_t = sb(ph, "p1wkv", [128, 2, 512], BF16)
                bg_t = sb(ph, "p1bg", [128, 24], F32)
                B_res = k.buf("p1res")
                W = WS[l]
                k.dma("sp", [(wqb_t[:], W["wqb_s"][:, :, :]), (wkk_t[:], W["wkvk_s"][:, :, :]), (wkv_t[:], W["wkvv_s"][:, :, :]),
                             (bg_t[:], b_gate[l].rearrange("(j p) -> p j", p=128))], writes=[B_res])
                xb = [sb(ph, "p1xb%d" % i, [128, 8, TB], BF16) for i in range(2)]
                Bxb = [k.buf("p1xb%d" % i) for i in range(2)]
                tb_t = [sb(ph, "p1tab%d" % i, [128, 6, TB], F32) for i in range(2)]
                Btb = [k.buf("p1tab%d" % i) for i in range(2)]
                NW = 4
                wt = [sb(ph, "p1w%d" % i, [128, 8, 128], BF16) for i in range(NW)]
                Bwt = [k.buf("p1w%d" % i) for i in range(NW)]
                wv = [sb(ph, "p1wv%d" % i, [128, 8, 512], BF16) for i in range(2)]
                Bwv = [k.buf("p1wv%d" % i) for i in range(2)]
                NP = 4
                pp = [ps(ph, "p1ps%d" % i, [128, 512]) for i in range(NP)]
                Bpp = [k.buf("p1ps%d" % i) for i in range(NP)]
                pr = [ps(ph, "p1pr%d" % i, [128, 512]) for i in range(2)]
                Bpr = [k.buf("p1pr%d" % i) for i in range(2)]
                pq = ps(ph, "p1pq", [128, 512]); Bpq = k.buf("p1pq")
                pz = ps(ph, "p1pz", [128, 512]); Bpz = k.buf("p1pz")
                cT = sb(ph, "p1cT", [128, 5, TB], BF16); BcT = k.buf("p1cT")
                sq = [sb(ph, "p1sq%d" % i, [128, TB], F32) for i in range(2)]
                Bsq = [k.buf("p1sq%d" % i) for i in range(2)]
                rstd = sb(ph, "p1rstd", [128, 2, TB], F32); Brstd = k.buf("p1rstd")
                rtm = sb(ph, "p1rtm", [128, 4], F32); Brtm = k.buf("p1rtm")
                NR = 4
                rawf = [sb(ph, "p1rawf%d" % i, [128, TB], F32) for i in range(NR)]
                Brawf = [k.buf("p1rawf%d" % i) for i in range(NR)]
                rawb = [sb(ph, "p1rawb%d" % i, [128, TB], BF16) for i in range(NR)]
                t1 = [sb(ph, "p1t1%d" % i, [128, TB], F32) for i in range(NR)]
                t2 = [sb(ph, "p1t2%d" % i, [128, TB], F32) for i in range(NR)]
                Braw = [k.buf("p1raw%d" % i) for i in range(NR)]
                Bt1 = [k.buf("p1t1%d" % i) for i in range(NR)]
                Bt2 = [k.buf("p1t2%d" % i) for i in range(NR)]
                NS = 6
                stg = [sb(ph, "p1stg%d" % i, [128, TB], BF16) for i in range(NS)]
                Bstg = [k.buf("p1stg%d" % i) for i in range(NS)]
                vstA = sb(ph, "p1vstA", [128, 4, 4, 129], BF16); BvstA = k.buf("p1vstA")
                vstB = sb(ph, "p1vstB", [128, 4, 8, 65], BF16); BvstB = k.buf("p1vstB")
                vstC = sb(ph, "p1vstC", [128, 4, 12, 129], BF16); BvstC = k.buf("p1vstC")
                k.op("pool", lambda e: e.memset(vstA[:], 1.0), writes=[BvstA])
                k.op("pool", lambda e: e.memset(vstB[:], 1.0), writes=[BvstB])
                k.op("pool", lambda e: e.memset(vstC[:], 1.0), writes=[BvstC])
                B_qA = k.buf("qA"); B_kA = k.buf("kA"); B_vA = k.buf("vA"); B_qB = k.buf("qB"); B_kB = k.buf("kB")
                B_vB = k.buf("vB"); B_qC = k.buf("qC"); B_kC = k.buf("kC"); B_vC = k.buf("vC"); B_gT = k.buf("gT")
                cnt = {"w": 0, "p": 0, "r": 0, "s": 0, "wv": 0, "pr": 0, "sq": 0}

                def rope_out(pst, Bps, rows, tabset, tbi, rstd_ap, dst_ap, Bdst):
                    ri = cnt["r"] % NR; cnt["r"] += 1
                    si = cnt["s"] % NS; cnt["s"] += 1
                    pi_ = cnt["pr"] % 2; cnt["pr"] += 1
                    cosT = tb_t[tbi][0:rows, 2 * tabset, :]
                    sinT = tb_t[tbi][0:rows, 2 * tabset + 1, :]
                    if rstd_ap is None:
                        k.op("act", lambda e: e.activation(out=rawb[ri][0:rows, :], in_=pst[0:rows, :], func=AF.Copy),
                             reads=[Bps], writes=[Braw[ri]])
                        k.op("dve", lambda e: e.tensor_tensor(out=t1[ri][0:rows, :], in0=pst[0:rows, :], in1=cosT,
                                                               op=ALU.mult), reads=[Bps, Btb[tbi], Braw[ri]], writes=[Bt1[ri]])
                    else:
                        k.op("dve", lambda e: e.tensor_tensor(out=rawf[ri][0:rows, :], in0=pst[0:rows, :], in1=rstd_ap,
                                                               op=ALU.mult), reads=[Bps, Brstd], writes=[Brawf[ri]])
                        k.op("act", lambda e: e.activation(out=rawb[ri][0:rows, :], in_=rawf[ri][0:rows, :], func=AF.Copy),
                             reads=[Brawf[ri]], writes=[Braw[ri]])
                        k.op("dve", lambda e: e.tensor_tensor(out=t1[ri][0:rows, :], in0=rawf[ri][0:rows, :], in1=cosT,
                                                               op=ALU.mult), reads=[Brawf[ri], Btb[tbi]], writes=[Bt1[ri]])
                    def part2():
                        k.mm_group([lambda e: e.matmul(out=pr[pi_][0:rows, :], lhsT=perm_b[0:rows, tabset, 0:rows],
                                                       rhs=rawb[ri][0:rows, :], start=True, stop=True)],
                                   reads=[Braw[ri]], writes=[Bpr[pi_]])
                        k.op("dve", lambda e: e.tensor_tensor(out=t2[ri][0:rows, :], in0=pr[pi_][0:rows, :], in1=sinT,
                                                               op=ALU.mult), reads=[Bpr[pi_], Btb[tbi]], writes=[Bt2[ri]])
                        k.op("pool", lambda e: e.tensor_tensor(out=stg[si][0:rows, :], in0=t1[ri][0:rows, :],
                                                                in1=t2[ri][0:rows, :], op=ALU.add),
                             reads=[Bt1[ri], Bt2[ri]], writes=[Bstg[si]])
                        k.dma("pool", [(dst_ap, stg[si][0:rows, :])], reads=[Bstg[si]], writes=[Bdst], partial=True)

                    pend.append(part2)

                pend = []

                def flush():
                    while pend:
                        pend.pop(0)()

                for tb in range(NTB):
                    tbi = tb % 2
                    tsl = slice(tb * TB, (tb + 1) * TB)
                    k.dma("sp", [(xb[tbi][:], xT[:, :, tsl])], writes=[Bxb[tbi]])
                    k.dma("sp", [(tb_t[tbi][:], tabs[:, :, tsl].rearrange("a p t -> p a t"))], writes=[Btb[tbi]])
                    for bi, (kind, idx, col0, ncol) in enumerate(FM_BLOCKS):
                        wi = cnt["w"] % NW; cnt["w"] += 1
                        pi = cnt["p"] % NP; cnt["p"] += 1
                        k.dma("sp", [(wt[wi][:], W["w1"][bi])], writes=[Bwt[wi]])
                        k.mm_group([(lambda e, c=c: e.matmul(out=pp[pi][0:ncol, :], lhsT=wt[wi][:, c, 0:ncol],
                                                              rhs=xb[tbi][:, c, :], start=(c == 0), stop=(c == 7)))
                                    for c in range(8)], reads=[Bwt[wi], Bxb[tbi]], writes=[Bpp[pi]])
                        flush()
                        if kind in ("aq", "ak", "cq", "ck"):
                            dst = {"aq": qA, "ak": kA, "cq": qC, "ck": kC}[kind]
                            Bd = {"aq": B_qA, "ak": B_kA, "cq": B_qC, "ck": B_kC}[kind]
                            rope_out(pp[pi], Bpp[pi], 128, 0, tbi, None, dst[idx, :, tsl], Bd)
                        elif kind in ("bcq", "bckv"):
                            ci = idx if kind == "bcq" else 3 + idx
                            k.op("act", lambda e: e.activation(out=cT[:, ci, :], in_=pp[pi][:], func=AF.Copy),
                                 reads=[Bpp[pi]], writes=[BcT])
                            sqi = cnt["sq"] % 2; cnt["sq"] += 1
                            k.op("act", lambda e: e.activation(out=sq[sqi][:], in_=pp[pi][:], func=AF.Square),
                                 reads=[Bpp[pi]], writes=[Bsq[sqi]])
                            first = idx == 0
                            last = (kind == "bcq" and idx == 2) or (kind == "bckv" and idx == 1)
                            k.mm_group([lambda e: e.matmul(out=pz[:], lhsT=ones_f[:], rhs=sq[sqi][:], start=first, stop=last)],
                                       reads=[Bsq[sqi], B_const], writes=[Bpz])
                            if kind == "bckv":
                                k.mm_group([(lambda e, j=j: e.matmul(out=pq[:, 256 + j:256 + j + 1],
                                                                      lhsT=sq[sqi][:, j * 128:(j + 1) * 128], rhs=ones_f[:, 0:1],
                                                                      start=(first and j == 0), stop=last,
                                                                      skip_group_check=True)) for j in range(4)],
                                           reads=[Bsq[sqi], B_const], writes=[Bpq])
                            if last:
                                ri_ = 0 if kind == "bcq" else 1
                                nfe = 384.0 if kind == "bcq" else 256.0
                                k.op("act", lambda e: e.activation(out=rstd[:, ri_, :], in_=pz[:], func=AF.Sqrt,
                                                                    bias=eps6[:, 0:1], scale=1.0 / nfe),
                                     reads=[Bpz], writes=[Brstd])
                                k.op("dve", lambda e: e.reciprocal(out=rstd[:, ri_, :], in_=rstd[:, ri_, :]),
                                     reads=[Brstd], writes=[Brstd])
                                if kind == "bckv":
                                    k.op("act", lambda e: e.activation(out=rtm[:], in_=pq[:, 256:260], func=AF.Sqrt,
                                                                        bias=eps6[:, 0:1], scale=1.0 / nfe),
                                         reads=[Bpq], writes=[Brtm])
                                    k.op("dve", lambda e: e.reciprocal(out=rtm[:], in_=rtm[:]), reads=[Brtm], writes=[Brtm])
                        elif kind == "bkr":
                            for h in range(8):
                                pass
                            ri = cnt["r"] % NR
                            si_peek = cnt["s"] % NS
                            rope_out(pp[pi], Bpp[pi], 32, 2, tbi, None, kB[0, 64:96, tsl], B_kB)
                            pend.append(lambda si_peek=si_peek, tsl=tsl: k.dma(
                                "pool", [(kB[h, 64:96, tsl], stg[si_peek][0:32, :]) for h in range(1, 8)],
                                reads=[Bstg[si_peek]], writes=[B_kB], partial=True))
                        elif kind == "g":
                            si = cnt["s"] % NS; cnt["s"] += 1
                            k.op("act", lambda e: e.activation(out=stg[si][:], in_=pp[pi][:], func=AF.Sigmoid,
                                                                bias=bg_t[:, idx:idx + 1], scale=1.0),
                                 reads=[Bpp[pi], B_res], writes=[Bstg[si]])
                            k.dma("pool", [(gT[idx, :, tsl], stg[si][:])], reads=[Bstg[si]], writes=[B_gT], partial=True)
                        if kind == "bckv" and idx == 1:
                            for h in range(8):
                                pi2 = cnt["p"] % NP; cnt["p"] += 1
                                k.mm_group([(lambda e, c=c: e.matmul(out=pp[pi2][0:96, :], lhsT=wqb_t[:, c, h * 96:(h + 1) * 96],
                                                                      rhs=cT[:, c, :], start=(c == 0), stop=(c == 2)))
                                            for c in range(3)], reads=[BcT, B_res], writes=[Bpp[pi2]])
                                flush()
                                rope_out(pp[pi2], Bpp[pi2], 96, 1, tbi, rstd[0:96, 0, :], qB[h, :, tsl], B_qB)
                            for h in range(8):
                                pi2 = cnt["p"] % NP; cnt["p"] += 1
                                si = cnt["s"] % NS; cnt["s"] += 1
                                k.mm_group([(lambda e, c=c: e.matmul(out=pp[pi2][0:64, :], lhsT=wkk_t[:, c, h * 64:(h + 1) * 64],
                                                                      rhs=cT[:, 3 + c, :], start=(c == 0), stop=(c == 1)))
                                            for c in range(2)], reads=[BcT, B_res], writes=[Bpp[pi2]])
                                k.op("dve", lambda e: e.tensor_tensor(out=stg[si][0:64, :], in0=pp[pi2][0:64, :],
                                                                       in1=rstd[0:64, 1, :], op=ALU.mult),
                                     reads=[Bpp[pi2], Brstd], writes=[Bstg[si]])
                                k.dma("pool", [(kB[h, 0:64, tsl], stg[si][0:64, :])], reads=[Bstg[si]], writes=[B_kB],
                                      partial=True)
                            for j in range(4):
                                pi2 = cnt["p"] % NP; cnt["p"] += 1
                                k.mm_group([(lambda e, c=c: e.matmul(out=pp[pi2][:, :], lhsT=cT[:, 3 + c, j * 128:(j + 1) * 128],
                                                                      rhs=wkv_t[:, c, :], start=(c == 0), stop=(c == 1)))
                                            for c in range(2)], reads=[BcT, B_res], writes=[Bpp[pi2]])
                                k.op("dve", lambda e: e.tensor_scalar(
                                    out=vstB[:, j, :, 0:64], in0=pp[pi2][:].rearrange("p (h e) -> p h e", h=8),
                                    scalar1=rtm[:, j:j + 1], scalar2=None, op0=ALU.mult),
                                     reads=[Bpp[pi2], Brtm], writes=[BvstB])
                            k.dma("pool", [(vB[h, :, tb * 4:(tb + 1) * 4, :], vstB[:, :, h, :]) for h in range(8)],
                                  reads=[BvstB], writes=[B_vB], partial=True)
                    flush()
                    for bi, (kind, idx, col0) in enumerate(TM_BLOCKS):
                        wvi = cnt["wv"] % 2; cnt["wv"] += 1
                        k.dma("sp", [(wv[wvi][:], W["w1v"][bi])], writes=[Bwv[wvi]])
                        for j in range(4):
                            pi2 = cnt["p"] % NP; cnt["p"] += 1
                            k.mm_group([(lambda e, c=c: e.matmul(out=pp[pi2][:, :], lhsT=xb[tbi][:, c, j * 128:(j + 1) * 128],
                                                                  rhs=wv[wvi][:, c, :], start=(c == 0), stop=(c == 7)))
                                        for c in range(8)], reads=[Bwv[wvi], Bxb[tbi]], writes=[Bpp[pi2]])
                            if kind == "av":
                                k.op("act", lambda e: e.activation(out=vstA[:, j, :, 0:128],
                                                                    in_=pp[pi2][:].rearrange("p (h e) -> p h e", h=4),
                                                                    func=AF.Copy), reads=[Bpp[pi2]], writes=[BvstA])
                            else:
                                k.op("act", lambda e: e.activation(out=vstC[:, j, idx * 4:(idx + 1) * 4, 0:128],
                                                                    in_=pp[pi2][:].rearrange("p (h e) -> p h e", h=4),
                                                                    func=AF.Copy), reads=[Bpp[pi2]], writes=[BvstC])
                        if kind == "av":
                            k.dma("pool", [(vA[h, :, tb * 4:(tb + 1) * 4, :], vstA[:, :, h, :]) for h in range(4)],
                                  reads=[BvstA], writes=[B_vA], partial=True)
                        elif idx == 2:
                            k.dma("pool", [(vC[(tb * 4 + j) * 128:(tb * 4 + j + 1) * 128, :],
                                            vstC[:, j, :, :].rearrange("p h e -> p (h e)")) for j in range(4)],
                                  reads=[BvstC], writes=[B_vC], partial=True)
                k.barrier()

        def lam_setup(ph, l, psm, Bpsm):
            lam_init = 0.8 - 0.6 * math.exp(-0.3 * l)
            lv = sb(ph, "lamv", [1, 4, 64], F32)
            lp = sb(ph, "lamp", [1, 2, 64], F32)
            ls = sb(ph, "lams", [1, 4], F32)
            neglam = sb(ph, "neglam", [128, 1], F32)
            gfac = sb(ph, "gfac", [128, 128], F32)
            B_l = k.buf("lam")
            k.dma("sp", [(lv[:], lam_in[l:l + 1, :, :]), (gfac[:], diff_g[l].partition_broadcast(128))], writes=[B_l])
            k.op("dve", lambda e: e.tensor_tensor(out=lp[:, 0, :], in0=lv[:, 0, :], in1=lv[:, 1, :], op=ALU.mult),
                 reads=[B_l], writes=[B_l])
            k.op("dve", lambda e: e.tensor_tensor(out=lp[:, 1, :], in0=lv[:, 2, :], in1=lv[:, 3, :], op=ALU.mult),
                 reads=[B_l], writes=[B_l])
            k.op("dve", lambda e: e.tensor_reduce(out=ls[:, 0:2], in_=lp[:], axis=AX.X, op=ALU.add), reads=[B_l], writes=[B_l])
            k.op("act", lambda e: e.activation(out=ls[:, 0:2], in_=ls[:, 0:2], func=AF.Exp), reads=[B_l], writes=[B_l])
            k.op("dve", lambda e: e.tensor_tensor(out=ls[:, 2:3], in0=ls[:, 1:2], in1=ls[:, 0:1], op=ALU.subtract),
                 reads=[B_l], writes=[B_l])
            k.op("dve", lambda e: e.tensor_scalar(out=ls[:, 3:4], in0=ls[:, 2:3], scalar1=-lam_init, scalar2=None, op0=ALU.add),
                 reads=[B_l], writes=[B_l])
            k.mm_group([lambda e: e.matmul(out=psm[:, 0:1], lhsT=ones_f[0:1, :], rhs=ls[0:1, 3:4], start=True, stop=True)],
                       reads=[B_l, B_const], writes=[Bpsm])
            k.op("dve", lambda e: e.tensor_copy(out=neglam[:], in_=psm[:, 0:1]), reads=[Bpsm], writes=[B_l])
            k.op("dve", lambda e: e.tensor_scalar(out=gfac[:], in0=gfac[:], scalar1=1.0 - lam_init, scalar2=None, op0=ALU.mult),
                 reads=[B_l], writes=[B_l])
            return neglam, gfac, B_l

        def phase_attn_dense(l):
            with contextlib.ExitStack() as ph:
                neglam_gf = {}
                with contextlib.ExitStack() as ph0:
                    pM = ps(ph0, "aM", [128, 512]); BpM = k.buf("aM")
                    neglam, gfac, B_l = lam_setup(ph, l, pM, BpM)
                    k.barrier()
                B_l = k.buf("lamc")
                NSR = 3
                pS = [ps(ph, "aS%d" % i, [128, 1024]) for i in range(NSR)]
                BpS = [k.buf("aS%d" % i) for i in range(NSR)]
                pAcc = ps(ph, "aAcc", [128, 1024]); BpAcc = k.buf("aAcc")
                qk = [(sb(ph, "aq%d" % i, [128, S], BF16), sb(ph, "ak%d" % i, [128, S], BF16),
                       sb(ph, "av%d" % i, [128, NT, 129], BF16)) for i in range(2)]
                Bqk = [k.buf("aqkv%d" % i) for i in range(2)]
                NE = 4
                E = [sb(ph, "aE%d" % i, [128, 1024], BF16) for i in range(NE)]
                BE = [k.buf("aE%d" % i) for i in range(NE)]
                accs = [sb(ph, "aaccs%d" % i, [128, 4, 129], F32) for i in range(2)]
                Baccs = [k.buf("aaccs%d" % i) for i in range(2)]
                rz = [sb(ph, "arz%d" % i, [128, 4], F32) for i in range(2)]
                Brz = [k.buf("arz%d" % i) for i in range(2)]
                oA = [sb(ph, "aoA%d" % i, [128, 4, 128], F32) for i in range(2)]
                BoA = [k.buf("aoA%d" % i) for i in range(2)]
                acomb = sb(ph, "acomb", [128, 4, 128], F32); Bac = k.buf("acomb")
                asq = sb(ph, "asq", [128, 4, 128], F32)
                ass = sb(ph, "ass", [128, 12], F32); Bass_ = k.buf("ass")
                ob = [sb(ph, "aob%d" % i, [128, 4, 128], BF16) for i in range(2)]
                Bob = [k.buf("aob%d" % i) for i in range(2)]
                ostg = [sb(ph, "aostg%d" % i, [128, 512], BF16) for i in range(2)]
                Bostg = [k.buf("aostg%d" % i) for i in range(2)]
                B_oT = k.buf("oT")
                scale_b = 96.0 ** -0.5

                heads = []
                for h in range(4):
                    heads.append(("A", h))
                for h in range(8):
                    heads.append(("B", h))

                def load_head(hi):
                    kind, h = heads[hi]
                    qb, kb, vb = qk[hi % 2]
                    if kind == "A":
                        k.dma("sp", [(qb[:], qA[h]), (kb[:], kA[h]), (vb[:], vA[h])], writes=[Bqk[hi % 2]])
                    else:
                        k.dma("sp", [(qb[0:96, :], qB[h]), (kb[0:96, :], kB[h]), (vb[:, :, 0:65], vB[h])],
                              writes=[Bqk[hi % 2]])

                items = []
                for hi, (kind, h) in enumerate(heads):
                    for qg in range(2 * NTB if kind == "A" else NTB):
                        for kt2 in range(NT // 2):
                            items.append((hi, kind, h, qg, 0, kt2))
                n = len(items)
                LA = 2
                delayed = {}

                def defer(idx, fn):
                    delayed.setdefault(idx, []).append(fn)

                def ops_of(it):
                    hi, kind, h, qg, m, kt2 = it
                    qb, kb, vb = qk[hi % 2]
                    if kind == "A":
                        return (qb[m * 64:(m + 1) * 64, :], kb[m * 64:(m + 1) * 64, :], vb, 129, 0.125, 256)
                    return (qb[0:96, :], kb[0:96, :], vb, 65, scale_b, 128)

                def emit_qk(i):
                    hi, kind, h, qg, m, kt2 = items[i]
                    if qg == 0 and m == 0 and kt2 == 0 and hi == 0:
                        load_head(0)
                    qT, kT, vt, dv1, scale, accw = ops_of(items[i])
                    si = i % NSR
                    ei = i % NE
                    Bin = Bqk[hi % 2]
                    if kind == "A":
                        qb, kb, vb = qk[hi % 2]
                        k.mm_group([(lambda e, j=j, m=m: e.matmul(
                            out=pS[si][:, m * 512 + j * 256:m * 512 + (j + 1) * 256],
                            lhsT=kb[m * 64:(m + 1) * 64, (kt2 * 2 + j) * 128:(kt2 * 2 + j + 1) * 128],
                            rhs=qb[m * 64:(m + 1) * 64, qg * 256:(qg + 1) * 256], start=True, stop=True))
                                    for j in range(2) for m in range(2)], reads=[Bin], writes=[BpS[si]])
                    else:
                        k.mm_group([(lambda e, j=j: e.matmul(out=pS[si][:, j * 512:(j + 1) * 512],
                                                              lhsT=kT[:, (kt2 * 2 + j) * 128:(kt2 * 2 + j + 1) * 128],
                                                              rhs=qT[:, qg * 512:(qg + 1) * 512], start=True, stop=True))
                                    for j in range(2)], reads=[Bin], writes=[BpS[si]])
                    k.op("act", lambda e: e.activation(out=E[ei][:], in_=pS[si][:], func=AF.Exp, scale=scale),
                         reads=[BpS[si]], writes=[BE[ei]])

                def emit_pv(i):
                    hi, kind, h, qg, m, kt2 = items[i]
                    if qg == 0 and m == 0 and kt2 == 0 and hi + 1 < len(heads):
                        load_head(hi + 1)
                    qT, kT, vt, dv1, scale, accw = ops_of(items[i])
                    ei = i % NE
                    Bin = Bqk[hi % 2]
                    fns = []
                    if kind == "A":
                        for j in range(2):
                            kt = kt2 * 2 + j
                            for m in range(2):
                                for js in range(2):
                                    a_ = m * 2 + js
                                    fns.append(lambda e, j=j, m=m, js=js, kt=kt, a_=a_: e.matmul(
                                        out=pAcc[:, a_ * 256:a_ * 256 + 129],
                                        lhsT=E[ei][:, m * 512 + j * 256 + js * 128:m * 512 + j * 256 + (js + 1) * 128],
                                        rhs=vt[:, kt, 0:129], start=(kt == 0 and a_ % 2 == 0), stop=(kt == NT - 1),
                                        skip_group_check=True))
                    for j in range(2 if kind == "B" else 0):
                        kt = kt2 * 2 + j
                        for js in range(4):
                            fns.append(lambda e, j=j, js=js, kt=kt: e.matmul(
                                out=pAcc[:, js * accw:js * accw + dv1],
                                lhsT=E[ei][:, j * 512 + js * 128:j * 512 + (js + 1) * 128],
                                rhs=vt[:, kt, 0:dv1], start=(kt == 0 and (js * accw) % 512 == 0), stop=(kt == NT - 1),
                                skip_group_check=True))
                    k.mm_group(fns, reads=[BE[ei], Bin], writes=[BpAcc])
                    if kt2 == NT // 2 - 1:
                        post(i)

                gcount = [0]

                def post(i):
                    hi, kind, h, qg, m, kt2 = items[i]
                    g = gcount[0]; gcount[0] += 1
                    ai = g % 2
                    if kind == "A":
                        oi = g % 2
                        k.op("dve", lambda e: e.tensor_copy(
                            out=accs[ai][:], in_=pAcc[:].rearrange("p (j w) -> p j w", w=256)[:, :, 0:129]),
                             reads=[BpAcc], writes=[Baccs[ai]])
                        k.op("dve", lambda e: e.reciprocal(out=rz[ai][:], in_=accs[ai][:, :, 128]),
                             reads=[Baccs[ai]], writes=[Brz[ai]])
                        k.op("pool", lambda e: e.tensor_tensor(
                            out=oA[0][:], in0=accs[ai][:, :, 0:128],
                            in1=rz[ai][:].unsqueeze(2).to_broadcast([128, 4, 128]), op=ALU.mult),
                             reads=[Baccs[ai], Brz[ai]], writes=[BoA[0]])

                        def p1():
                            k.op("dve", lambda e: e.scalar_tensor_tensor(out=acomb[:, 0:2, :], in0=oA[0][:, 2:4, :],
                                                                          scalar=neglam[:, 0:1], in1=oA[0][:, 0:2, :],
                                                                          op0=ALU.mult, op1=ALU.add),
                                 reads=[BoA[0]], writes=[Bac])
                            k.op("pool", lambda e: e.tensor_tensor(out=asq[:, 0:2, :], in0=acomb[:, 0:2, :],
                                                                    in1=acomb[:, 0:2, :], op=ALU.mult),
                                 reads=[Bac], writes=[Bass_])
                            k.op("dve", lambda e: e.tensor_reduce(out=ass[:, 0:2], in_=asq[:, 0:2, :], axis=AX.X, op=ALU.add),
                                 reads=[Bass_], writes=[Bass_])

                        def p2():
                            k.op("act", lambda e: e.activation(out=ass[:, 4:6], in_=ass[:, 0:2], func=AF.Ln,
                                                                bias=eps6[:, 0:1], scale=1.0 / 128.0),
                                 reads=[Bass_], writes=[Bass_])

                        def p3():
                            k.op("act", lambda e: e.activation(out=ass[:, 8:10], in_=ass[:, 4:6], func=AF.Exp, scale=-0.5),
                                 reads=[Bass_], writes=[Bass_])

                        def p4():
                            for js in range(2):
                                k.op("dve", lambda e, js=js: e.scalar_tensor_tensor(
                                    out=ob[oi][:, js, :], in0=acomb[:, js, :], scalar=ass[:, 8 + js:9 + js], in1=gfac[:],
                                    op0=ALU.mult, op1=ALU.mult), reads=[Bac, Bass_], writes=[Bob[oi]])

                        def p5():
                            si = (i + 8 + LA - 1) % NSR
                            pTrb = pS[si][:, 0:512].bitcast(BF16)
                            k.mm_group([(lambda e, js=js: e.transpose(out=pTrb[:, js * 128:(js + 1) * 128],
                                                                       in_=ob[oi][:, js, :], identity=ident_b[:]))
                                        for js in range(2)], reads=[Bob[oi], B_const], writes=[BpS[si]])
                            k.op("dve", lambda e: e.tensor_copy(out=ostg[oi][:, 0:256], in_=pTrb[:, 0:256]), reads=[BpS[si]],
                                 writes=[Bostg[oi]])
                            k.dma("pool", [(oT[h, :, qg * 256:(qg + 1) * 256], ostg[oi][:, 0:256])], reads=[Bostg[oi]],
                                  writes=[B_oT], partial=True)

                        defer(i + 1, p1); defer(i + 3, p2); defer(i + 4, p3); defer(i + 6, p4); defer(i + 8, p5)
                    else:
                        oi = g % 2
                        k.op("dve", lambda e: e.tensor_copy(
                            out=accs[ai][:, :, 0:65], in_=pAcc[:, 0:512].rearrange("p (j w) -> p j w", w=128)[:, :, 0:65]),
                             reads=[BpAcc], writes=[Baccs[ai]])
                        k.op("dve", lambda e: e.reciprocal(out=rz[ai][:], in_=accs[ai][:, :, 64]),
                             reads=[Baccs[ai]], writes=[Brz[ai]])
                        k.op("pool", lambda e: e.tensor_tensor(
                            out=ob[oi][:, :, 0:64], in0=accs[ai][:, :, 0:64],
                            in1=rz[ai][:].unsqueeze(2).to_broadcast([128, 4, 64]), op=ALU.mult),
                             reads=[Baccs[ai], Brz[ai]], writes=[Bob[oi]])

                        def p5():
                            si = (i + 4 + LA - 1) % NSR
                            pTrb = pS[si][:, 0:512].bitcast(BF16)
                            k.mm_group([(lambda e, js=js: e.transpose(out=pTrb[0:64, js * 128:(js + 1) * 128],
                                                                       in_=ob[oi][:, js, 0:64], identity=ident_b[:]))
                                        for js in range(4)], reads=[Bob[oi], B_const], writes=[BpS[si]])
                            k.op("dve", lambda e: e.tensor_copy(out=ostg[oi][0:64, :], in_=pTrb[0:64, 0:512]),
                                 reads=[BpS[si]], writes=[Bostg[oi]])
                            k.dma("pool", [(oT[4 + h // 2, (h % 2) * 64:(h % 2) * 64 + 64, qg * 512:(qg + 1) * 512],
                                            ostg[oi][0:64, :])], reads=[Bostg[oi]], writes=[B_oT], partial=True)

                        defer(i + 4, p5)

                wjobs = list(late_jobs.pop(l, []))
                if l + 1 < depth:
                    e1, l1 = wprep_jobs(l + 1, ph)
                    wjobs += e1 + l1
                for idx in range(n + LA + 10):
                    j = idx - LA
                    if wjobs and idx % 40 == 20:
                        wjobs.pop(0)()
                    if idx < n:
                        emit_qk(idx)
                    if 0 <= j < n:
                        emit_pv(j)
                    if j in delayed:
                        for fn in delayed.pop(j):
                            fn()
                assert not delayed
                while wjobs:
                    wjobs.pop(0)()
                k.barrier()

        def phase_attn_dil(l):
            with contextlib.ExitStack() as ph:
                NSB = 3
                pS = [ps(ph, "cS%d" % i, [128, 512]) for i in range(NSB)]
                BpS = [k.buf("cS%d" % i) for i in range(NSB)]
                pAcc = [ps(ph, "cAcc%d" % i, [128, 1024]) for i in range(2)]
                BpAcc = [k.buf("cAcc%d" % i) for i in range(2)]
                pTr = ps(ph, "cTr", [128, 512]); BpTr = k.buf("cTr")
                pTrb = pTr[:].bitcast(BF16)
                maskf = sb(ph, "cmaskf", [128, len(C_MASKS), 128], F32)
                maskb = sb(ph, "cmaskb", [128, len(C_MASKS), 128], BF16)
                B_m = k.buf("cmask")
                k.dma("sp", [(maskf[:], c_maskb[:, :, :])], writes=[B_m])
                k.op("dve", lambda e: e.tensor_copy(out=maskb[:], in_=maskf[:]), reads=[B_m], writes=[B_m])
                kr = [sb(ph, "ckr%d" % i, [128, 6, 128], BF16) for i in range(RING)]
                vr = [sb(ph, "cvr%d" % i, [128, 12, 129], BF16) for i in range(RING)]
                Bkv = [k.buf("ckv%d" % i) for i in range(RING)]
                NQ = 3
                qr = [sb(ph, "cqr%d" % i, [128, 6, 128], BF16) for i in range(NQ)]
                Bq = [k.buf("cq%d" % i) for i in range(NQ)]
                NE = 4
                E = [sb(ph, "cE%d" % i, [128, 512], BF16) for i in range(NE)]
                BE = [k.buf("cE%d" % i) for i in range(NE)]
                rz = [sb(ph, "crz%d" % i, [128, 4], F32) for i in range(2)]
                Brz = [k.buf("crz%d" % i) for i in range(2)]
                ob = [sb(ph, "cob%d" % i, [128, 4, 128], BF16) for i in range(2)]
                Bob = [k.buf("cob%d" % i) for i in range(2)]
                ostg = [sb(ph, "costg%d" % i, [128, 4, 512], BF16) for i in range(2)]
                Bostg = [k.buf("costg%d" % i) for i in range(2)]
                B_oT = k.buf("oTc")

                def load_kv(kt):
                    sl = kt % RING
                    k.dma("sp", [(kr[sl][:], kC[:, :, kt * 128:(kt + 1) * 128].rearrange("j p t -> p j t")),
                                 (vr[sl][:].rearrange("p h e -> p (h e)"), vC[kt * 128:(kt + 1) * 128, :])],
                          writes=[Bkv[sl]])

                items = []
                for qt in range(NT):
                    for c in range(4):
                        tiles = []
                        for g in range(3):
                            for dl in range(-C_DELTA[g], C_DELTA[g] + 1):
                                kt = qt + dl
                                if 0 <= kt < NT:
                                    tiles.append((g, dl, kt))
                        ntile = len(tiles)
                        for b0 in range(0, ntile, 4):
                            items.append((qt, c, b0, tiles[b0:b0 + 4], ntile, b0 + 4 >= ntile))
                n = len(items)
                LA = 2
                delayed = {}

                def emit_qk(i):
                    qt, c, b0, grp, ntile, lastg = items[i]
                    if c == 0 and b0 == 0:
                        if qt == 0:
                            for kt in range(0, 9):
                                load_kv(kt)
                        elif qt + 8 < NT:
                            load_kv(qt + 8)
                        k.dma("sp", [(qr[qt % NQ][:], qC[:, :, qt * 128:(qt + 1) * 128].rearrange("j p t -> p j t"))],
                              writes=[Bq[qt % NQ]])
                    qi = qt % NQ
                    si = i % NSB
                    ei = i % NE
                    fns = []
                    rd = [Bq[qi], B_m, B_const]
                    for s_, (g, dl, kt) in enumerate(grp):
                        f = g * 4 + c
                        j, hf = f // 2, f % 2
                        mi = C_MASKS.index((g, dl))
                        sl = kt % RING
                        rd.append(Bkv[sl])
                        fns.append(lambda e, s_=s_, j=j, hf=hf, sl=sl: e.matmul(
                            out=pS[si][:, s_ * 128:(s_ + 1) * 128], lhsT=kr[sl][hf * 64:(hf + 1) * 64, j, :],
                            rhs=qr[qi][hf * 64:(hf + 1) * 64, j, :], start=True, stop=True))
                    k.mm_group(fns, reads=rd, writes=[BpS[si]])
                    nn = len(grp)
                    k.op("act", lambda e: e.activation(out=E[ei][:, 0:nn * 128], in_=pS[si][:, 0:nn * 128], func=AF.Exp,
                                                        scale=0.125), reads=[BpS[si]], writes=[BE[ei]])
                    mis = [C_MASKS.index((g, dl)) for (g, dl, kt) in grp]
                    r0 = 0
                    while r0 < nn:
                        r1 = r0 + 1
                        while r1 < nn and mis[r1] == mis[r1 - 1] + 1:
                            r1 += 1
                        meng = "dve" if (i % 2 == 0) else "pool"
                        k.op(meng, lambda e, r0=r0, r1=r1: e.tensor_tensor(
                            out=E[ei][:, r0 * 128:r1 * 128], in0=E[ei][:, r0 * 128:r1 * 128],
                            in1=maskb[:, mis[r0]:mis[r0] + (r1 - r0), :].rearrange("p m t -> p (m t)"), op=ALU.mult),
                             reads=[BE[ei], B_m], writes=[BE[ei]])
                        r0 = r1

                def emit_pv(i):
                    qt, c, b0, grp, ntile, lastg = items[i]
                    ei = i % NE
                    ai = qt % 2
                    fns = []
                    rd = [BE[ei]]
                    for s_, (g, dl, kt) in enumerate(grp):
                        f = g * 4 + c
                        sl = kt % RING
                        rd.append(Bkv[sl])
                        gi = b0 + s_
                        fns.append(lambda e, s_=s_, f=f, sl=sl, gi=gi: e.matmul(
                            out=pAcc[ai][:, c * 256:c * 256 + 129], lhsT=E[ei][:, s_ * 128:(s_ + 1) * 128],
                            rhs=vr[sl][:, f, :], start=(gi == 0), stop=(gi == ntile - 1)))
                    k.mm_group(fns, reads=rd, writes=[BpAcc[ai]])
                    if c == 3 and lastg:
                        oi = qt % 2
                        k.op("dve", lambda e: e.reciprocal(
                            out=rz[oi][:], in_=pAcc[ai][:].rearrange("p (j w) -> p j w", w=256)[:, :, 128]),
                             reads=[BpAcc[ai]], writes=[Brz[oi]])
                        for cc in range(4):
                            k.op("dve", lambda e, cc=cc: e.tensor_scalar(
                                out=ob[oi][:, cc, :], in0=pAcc[ai][:, cc * 256:cc * 256 + 128], scalar1=rz[oi][:, cc:cc + 1],
                                scalar2=None, op0=ALU.mult), reads=[BpAcc[ai], Brz[oi]], writes=[Bob[oi]])

                        def p5():
                            k.mm_group([(lambda e, cc=cc: e.transpose(out=pTrb[:, cc * 128:(cc + 1) * 128], in_=ob[oi][:, cc, :],
                                                                       identity=ident_b[:])) for cc in range(4)],
                                       reads=[Bob[oi], B_const], writes=[BpTr])
                            gi_ = (qt // 4) % 2
                            k.op("dve", lambda e: e.tensor_copy(
                                out=ostg[gi_][:, :, (qt % 4) * 128:(qt % 4 + 1) * 128],
                                in_=pTrb[:, 0:512].rearrange("p (c t) -> p c t", c=4)), reads=[BpTr], writes=[Bostg[gi_]])
                            if qt % 4 == 3:
                                qg = qt // 4
                                k.dma("pool", [(oT[8 + cc, :, qg * 512:(qg + 1) * 512], ostg[gi_][:, cc, :]) for cc in range(4)],
                                      reads=[Bostg[gi_]], writes=[B_oT], partial=True)

                        delayed.setdefault(i + 6, []).append(p5)

                for idx in range(n + LA + 8):
                    j = idx - LA
                    if idx < n:
                        emit_qk(idx)
                    if 0 <= j < n:
                        emit_pv(j)
                    if j in delayed:
                        for fn in delayed.pop(j):
                            fn()
                assert not delayed
                k.barrier()

        def phase_merge(l):
            with contextlib.ExitStack() as ph:
                st = ln_setup(ph, "l1")
                gam, bet, B_gb = load_gb(ph, "l1", ln1_g[l], ln1_b[l])
                wbr_t = sb(ph, "m_wbr", [128, 12, D], BF16)
                wout_t = sb(ph, "m_wout", [128, 8, D], BF16)
                rw_t = sb(ph, "m_rw", [128, 8, 16], F32)
                B_w = k.buf("m_w")
                W = WS[l]
                k.dma("sp", [(wbr_t[:], W["wbr_s"][:, :, :]), (wout_t[:], W["wout_s"][:, :, :]),
                             (rw_t[:], router_w.rearrange("(c p) e -> p c e", p=128))], writes=[B_w])
                ot = [sb(ph, "m_ot%d" % i, [128, 12, TB], BF16) for i in range(2)]
                gt = [sb(ph, "m_gt%d" % i, [128, 24, TB], BF16) for i in range(2)]
                Bin = [k.buf("m_in%d" % i) for i in range(2)]
                NPB = 3
                pb = [ps(ph, "m_p%d" % i, [128, 512]) for i in range(NPB)]
                Bpb = [k.buf("m_p%d" % i) for i in range(NPB)]
                NPM = 3
                pmx = [ps(ph, "m_mix%d" % i, [128, 512]) for i in range(NPM)]
                Bpmx = [k.buf("m_mix%d" % i) for i in range(NPM)]
                psT = ps(ph, "m_psT", [128, 512]); B_psT = k.buf("m_psT")
                prt = ps(ph, "m_prt", [128, 512]); Bprt = k.buf("m_prt")
                mm_ = [[sb(ph, "m_m%d_%d" % (s_, i), [128, TB], F32) for i in range(3)] for s_ in range(2)]
                Bmm = [[k.buf("m_m%d_%d" % (s_, i)) for i in range(3)] for s_ in range(2)]
                yT = [sb(ph, "m_yT%d" % i, [128, 8, TB], BF16) for i in range(2)]
                ByT = [k.buf("m_yT%d" % i) for i in range(2)]
                NRT = 3
                rt = [sb(ph, "m_r%d" % i, [128, D], F32) for i in range(NRT)]
                Brt = [k.buf("m_r%d" % i) for i in range(NRT)]
                xs = [sb(ph, "m_xs%d" % i, [128, 8, 512], BF16) for i in range(2)]
                Bxs = [k.buf("m_xs%d" % i) for i in range(2)]
                xf = [sb(ph, "m_xf%d" % i, [128, 8, 128], F32) for i in range(2)]
                B_xf = [k.buf("m_xf%d" % i) for i in range(2)]
                router = {"xf": xf, "B_xf": B_xf, "ps": prt, "B_ps": Bprt, "w": rw_t}
                B_x1T = k.buf("x1T")
                cnt = {"p": 0, "m": 0}

                def load_in(tb):
                    bi = tb % 2
                    tsl = slice(tb * TB, (tb + 1) * TB)
                    k.dma("sp", [(ot[bi][:], oT[:, :, tsl].rearrange("j p t -> p j t")),
                                 (gt[bi][:], gT[:, :, tsl].rearrange("j p t -> p j t"))], writes=[Bin[bi]])

                def stage_a(tb, dc):
                    bi = tb % 2
                    ms = dc % 2
                    for br in range(3):
                        pi = cnt["p"] % NPB; cnt["p"] += 1
                        k.mm_group([(lambda e, c=c: e.matmul(out=pb[pi][:], lhsT=wbr_t[:, br * 4 + c, dc * 128:(dc + 1) * 128],
                                                              rhs=ot[bi][:, br * 4 + c, :], start=(c == 0), stop=(c == 3)))
                                    for c in range(4)], reads=[B_w, Bin[bi]], writes=[Bpb[pi]])
                        k.op("dve", lambda e, br=br, pi=pi: e.tensor_tensor(
                            out=mm_[ms][br][:], in0=pb[pi][:], in1=gt[bi][:, br * 8 + dc, :], op=ALU.mult),
                             reads=[Bpb[pi], Bin[bi]], writes=[Bmm[ms][br]])
                    k.op("pool", lambda e: e.tensor_tensor(out=mm_[ms][0][:], in0=mm_[ms][0][:], in1=mm_[ms][1][:], op=ALU.add),
                         reads=[Bmm[ms][0], Bmm[ms][1]], writes=[Bmm[ms][0]])
                    k.op("pool", lambda e: e.tensor_tensor(out=yT[bi][:, dc, :], in0=mm_[ms][0][:], in1=mm_[ms][2][:],
                                                            op=ALU.add), reads=[Bmm[ms][0], Bmm[ms][2]], writes=[ByT[bi]])

                def stage_b1(tb, j):
                    bi = tb % 2
                    t = tb * 4 + j
                    ri = t % NRT
                    k.dma("sp", [(rt[ri][:], xres[t * 128:(t + 1) * 128, :])], writes=[Brt[ri]])
                    for hf in range(2):
                        pm = cnt["m"] % NPM; cnt["m"] += 1
                        k.mm_group([(lambda e, dc=dc: e.matmul(out=pmx[pm][:], lhsT=yT[bi][:, dc, j * 128:(j + 1) * 128],
                                                                rhs=wout_t[:, dc, hf * 512:(hf + 1) * 512],
                                                                start=(dc == 0), stop=(dc == 7))) for dc in range(8)],
                                   reads=[ByT[bi], B_w], writes=[Bpmx[pm]])
                        k.op("dve", lambda e, hf=hf, pm=pm: e.scalar_tensor_tensor(
                            out=rt[ri][:, hf * 512:(hf + 1) * 512], in0=rt[ri][:, hf * 512:(hf + 1) * 512], scalar=ALPHA,
                            in1=pmx[pm][:], op0=ALU.mult, op1=ALU.add), reads=[Brt[ri], Bpmx[pm]], writes=[Brt[ri]])
                    sidx = tb % 2
                    return finish_tile(st, "l1", Brt[ri], rt[ri], gam, bet, B_gb, t, x1res, xs[sidx], Bxs[sidx], psT, B_psT,
                                       router=router, defer_tr=True)

                load_in(0)
                for dc in range(8):
                    stage_a(0, dc)
                for tb in range(NTB):
                    nxt = tb + 1 < NTB
                    if nxt:
                        load_in(tb + 1)
                    trs = []
                    for j in range(4):
                        trs.append(stage_b1(tb, j))
                        if nxt:
                            stage_a(tb + 1, 2 * j)
                            stage_a(tb + 1, 2 * j + 1)
                        if j >= 1:
                            trs[j - 1]()
                    trs[3]()
                    tsl = slice(tb * TB, (tb + 1) * TB)
                    k.dma("pool", [(x1T[:, :, tsl], xs[tb % 2][:])], reads=[Bxs[tb % 2]], writes=[B_x1T], partial=True)
                k.barrier()

        def phase_moe(l, last):
            with contextlib.ExitStack() as ph:
                with contextlib.ExitStack() as rs:
                    NN = NT * 16
                    bias_t = sb(rs, "r_bias", [128, NN], F32)
                    biased = sb(rs, "r_biased", [128, NN], F32)
                    p6 = sb(rs, "r_p6", [128, NT * 4, 6], F32)
                    gs = sb(rs, "r_gs", [128, NT, 4], F32)
                    gmax = sb(rs, "r_gmax", [128, NT], F32)
                    gmask = sb(rs, "r_gmask", [128, NT, 4], F32)
                    emask = sb(rs, "r_emask", [128, NN], F32)
                    tneg = sb(rs, "r_tneg", [128, NN], F32)
                    masked = sb(rs, "r_masked", [128, NN], F32)
                    m1 = sb(rs, "r_m1", [128, NT], F32)
                    sel1 = sb(rs, "r_sel1", [128, NN], F32)
                    sel2 = sb(rs, "r_sel2", [128, NN], F32)
                    gate = sb(rs, "r_gate", [128, NT, 16], F32)
                    gts = sb(rs, "r_gts", [16, 512], F32)
                    pg = ps(rs, "r_pg", [128, 512]); Bpg = k.buf("r_pg")
                    B_r = k.buf("r")
                    B_gs = k.buf("r_gts"); B_gateT = k.buf("gateT")
                    k.dma("sp", [(bias_t[:], router_bias.partition_broadcast(128))], writes=[B_r])
                    sc = scores_all[:].rearrange("p t e -> p (t e)")
                    v3 = lambda a: a[:].rearrange("p (t e) -> p t e", e=16)
                    v4 = lambda a: a[:].rearrange("p (g e) -> p g e", e=4)
                    R = [B_r]

                    def dv(fn, extra=()):
                        k.op("dve", fn, reads=[B_r] + list(extra), writes=[B_r])

                    dv(lambda e: e.tensor_tensor(out=biased[:], in0=sc, in1=bias_t[:], op=ALU.add), [B_scores])
                    b4 = v4(biased)
                    pairs = [(0, 1), (0, 2), (0, 3), (1, 2), (1, 3), (2, 3)]
                    for pi_, (a_, b_) in enumerate(pairs):
                        dv(lambda e, pi_=pi_, a_=a_, b_=b_: e.tensor_tensor(out=p6[:, :, pi_], in0=b4[:, :, a_],
                                                                           in1=b4[:, :, b_], op=ALU.add))
                    dv(lambda e: e.tensor_reduce(out=gs[:].rearrange("p t g -> p (t g)"), in_=p6[:], axis=AX.X, op=ALU.max))
                    dv(lambda e: e.tensor_reduce(out=gmax[:], in_=gs[:], axis=AX.X, op=ALU.max))
                    dv(lambda e: e.tensor_tensor(out=gmask[:], in0=gs[:], in1=gmax[:].unsqueeze(2).to_broadcast([128, NT, 4]),
                                                 op=ALU.is_equal))
                    dv(lambda e: e.tensor_copy(out=v4(emask), in_=gmask[:].rearrange("p t g -> p (t g)").unsqueeze(2)
                                               .to_broadcast([128, NT * 4, 4])))
                    dv(lambda e: e.tensor_scalar(out=tneg[:], in0=emask[:], scalar1=1e9, scalar2=-1e9, op0=ALU.mult,
                                                 op1=ALU.add))
                    dv(lambda e: e.tensor_tensor(out=masked[:], in0=biased[:], in1=emask[:], op=ALU.mult))
                    dv(lambda e: e.tensor_tensor(out=masked[:], in0=masked[:], in1=tneg[:], op=ALU.add))
                    dv(lambda e: e.tensor_reduce(out=m1[:], in_=v3(masked), axis=AX.X, op=ALU.max))
                    dv(lambda e: e.tensor_tensor(out=v3(sel1), in0=v3(masked), in1=m1[:].unsqueeze(2).to_broadcast([128, NT, 16]),
                                                 op=ALU.is_equal))
                    dv(lambda e: e.scalar_tensor_tensor(out=masked[:], in0=sel1[:], scalar=-1e9, in1=masked[:], op0=ALU.mult,
                                                        op1=ALU.add))
                    dv(lambda e: e.tensor_reduce(out=m1[:], in_=v3(masked), axis=AX.X, op=ALU.max))
                    dv(lambda e: e.tensor_tensor(out=v3(sel2), in0=v3(masked), in1=m1[:].unsqueeze(2).to_broadcast([128, NT, 16]),
                                                 op=ALU.is_equal))
                    dv(lambda e: e.tensor_tensor(out=sel1[:], in0=sel1[:], in1=sel2[:], op=ALU.add))
                    dv(lambda e: e.tensor_tensor(out=sel1[:], in0=sel1[:], in1=sc, op=ALU.mult), [B_scores])
                    dv(lambda e: e.tensor_reduce(out=m1[:], in_=v3(sel1), axis=AX.X, op=ALU.add))
                    dv(lambda e: e.reciprocal(out=m1[:], in_=m1[:]))
                    dv(lambda e: e.tensor_tensor(out=gate[:], in0=v3(sel1), in1=m1[:].unsqueeze(2).to_broadcast([128, NT, 16]),
                                                 op=ALU.mult))
                    for t4 in range(NT // 4):
                        k.mm_group([(lambda e, j=j: e.transpose(out=pg[0:16, j * 128:(j + 1) * 128], in_=gate[:, t4 * 4 + j, :],
                                                                 identity=ident_f[:])) for j in range(4)],
                                   reads=[B_r, B_const], writes=[Bpg])
                        k.op("dve", lambda e: e.tensor_copy(out=gts[:], in_=pg[0:16, :]), reads=[Bpg], writes=[B_gs])
                        k.dma("sp", [(gateT[:, t4 * 512:(t4 + 1) * 512], gts[:])], reads=[B_gs], writes=[B_gateT], partial=True)
                    k.barrier()
                st = ln_setup(ph, "l2")
                gam, bet, B_gb = load_gb(ph, "l2", ln2_g[l], ln2_b[l])
                wd_t = sb(ph, "e_wd", [128, 34, D], BF16)
                selc = sb(ph, "e_selc", [16, 16, 128], F32)
                B_w = k.buf("e_w")
                W = WS[l]
                k.dma("sp", [(wd_t[:], W["wed_s"][:, :, :]), (selc[:], c_selc[:, :, :])], writes=[B_w])
                xb = [sb(ph, "e_xb%d" % i, [128, 8, TB], BF16) for i in range(2)]
                gtb = [sb(ph, "e_gtb%d" % i, [16, TB], F32) for i in range(2)]
                Bxb = [k.buf("e_xb%d" % i) for i in range(2)]
                NW = 2
                wg = [sb(ph, "e_wg%d" % i, [128, 8, 256], BF16) for i in range(NW)]
                wu = [sb(ph, "e_wu%d" % i, [128, 8, 256], BF16) for i in range(NW)]
                Bwgu = [k.buf("e_wgu%d" % i) for i in range(NW)]
                pgu = [ps(ph, "e_pgu%d" % i, [128, 1024]) for i in range(2)]
                Bpgu = [k.buf("e_pgu%d" % i) for i in range(2)]
                pgb = ps(ph, "e_pgb", [128, 512]); Bpgb = k.buf("e_pgb")
                pdn = ps(ph, "e_pdn", [128, 1024]); Bpdn = k.buf("e_pdn")
                psT = ps(ph, "e_psT", [128, 512]); B_psT = k.buf("e_psT")
                gbc = [sb(ph, "e_gbc%d" % i, [128, TB], F32) for i in range(2)]
                Bgbc = [k.buf("e_gbc%d" % i) for i in range(2)]
                sg = [sb(ph, "e_sg%d" % i, [128, TB], F32) for i in range(2)]
                Bsg = [k.buf("e_sg%d" % i) for i in range(2)]
                h1 = [sb(ph, "e_h1%d" % i, [128, TB], F32) for i in range(2)]
                Bh1 = [k.buf("e_h1%d" % i) for i in range(2)]
                hg = sb(ph, "e_hg", [128, 34, TB], BF16); Bhg = k.buf("e_hg")
                xr = [sb(ph, "e_x%d" % i, [128, D], F32) for i in range(2)]
                Bxr = [k.buf("e_x%d" % i) for i in range(2)]
                rt, Brt = xr, Bxr
                xs = [sb(ph, "e_xs%d" % i, [128, 8, 512], BF16) for i in range(2)]
                Bxs = [k.buf("e_xs%d" % i) for i in range(2)]
                B_xT = k.buf("xTn")
                wc = 0; pc = 0; hc_ = 0
                for tb in range(NTB):
                    bi = tb % 2
                    tsl = slice(tb * TB, (tb + 1) * TB)
                    k.dma("sp", [(xb[bi][:], x1T[:, :, tsl]), (gtb[bi][:], gateT[:, tsl])], writes=[Bxb[bi]])
                    for e_ in range(17):
                        wi = wc % NW; wc += 1
                        k.dma("sp", [(wg[wi][:], W["weg_s"][e_]), (wu[wi][:], W["weu_s"][e_])], writes=[Bwgu[wi]])
                        gi = e_ % 2
                        if e_ < 16:
                            k.mm_group([lambda e: e.matmul(out=pgb[:], lhsT=selc[:, e_, :], rhs=gtb[bi][:], start=True, stop=True)],
                                       reads=[B_w, Bxb[bi]], writes=[Bpgb])
                            k.op("act", lambda e: e.activation(out=gbc[gi][:], in_=pgb[:], func=AF.Copy), reads=[Bpgb],
                                 writes=[Bgbc[gi]])
                        for hf in range(2):
                            pi = pc % 2; pc += 1
                            hi = hc_ % 2; hc_ += 1
                            fns = []
                            for c in range(8):
                                fns.append(lambda e, c=c: e.matmul(out=pgu[pi][:, 0:512], lhsT=wg[wi][:, c, hf * 128:(hf + 1) * 128],
                                                                   rhs=xb[bi][:, c, :], start=(c == 0), stop=(c == 7)))
                            for c in range(8):
                                fns.append(lambda e, c=c: e.matmul(out=pgu[pi][:, 512:1024],
                                                                   lhsT=wu[wi][:, c, hf * 128:(hf + 1) * 128],
                                                                   rhs=xb[bi][:, c, :], start=(c == 0), stop=(c == 7)))
                            k.mm_group(fns, reads=[Bwgu[wi], Bxb[bi]], writes=[Bpgu[pi]])
                            k.op("act", lambda e: e.activation(out=sg[hi][:], in_=pgu[pi][:, 0:512], func=AF.Silu),
                                 reads=[Bpgu[pi]], writes=[Bsg[hi]])
                            ch = e_ * 2 + hf
                            if e_ < 16:
                                k.op("dve", lambda e: e.tensor_tensor(out=h1[hi][:], in0=pgu[pi][:, 512:1024], in1=sg[hi][:],
                                                                       op=ALU.mult), reads=[Bpgu[pi], Bsg[hi]], writes=[Bh1[hi]])
                                k.op("pool", lambda e: e.tensor_tensor(out=hg[:, ch, :], in0=h1[hi][:], in1=gbc[gi][:],
                                                                        op=ALU.mult), reads=[Bh1[hi], Bgbc[gi]], writes=[Bhg])
                            else:
                                k.op("dve", lambda e: e.tensor_tensor(out=hg[:, ch, :], in0=pgu[pi][:, 512:1024], in1=sg[hi][:],
                                                                       op=ALU.mult), reads=[Bpgu[pi], Bsg[hi]], writes=[Bhg])
                    for j in range(4):
                        t = tb * 4 + j
                        ri = t % 2
                        k.dma("sp", [(xr[ri][:], x1res[t * 128:(t + 1) * 128, :])], writes=[Bxr[ri]])
                        for hf in range(2):
                            k.mm_group([(lambda e, ch=ch: e.matmul(out=pdn[:, hf * 512:(hf + 1) * 512],
                                                                    lhsT=hg[:, ch, j * 128:(j + 1) * 128],
                                                                    rhs=wd_t[:, ch, hf * 512:(hf + 1) * 512],
                                                                    start=(ch == 0), stop=(ch == 33))) for ch in range(34)],
                                       reads=[Bhg, B_w], writes=[Bpdn])
                        k.op("dve", lambda e: e.scalar_tensor_tensor(out=rt[ri][:], in0=xr[ri][:], scalar=ALPHA, in1=pdn[:],
                                                                      op0=ALU.mult, op1=ALU.add),
                             reads=[Bxr[ri], Bpdn], writes=[Brt[ri]])
                        sidx = tb % 2
                        finish_tile(st, "l2", Brt[ri], rt[ri], gam, bet, B_gb, t, xres, xs[sidx], Bxs[sidx], psT, B_psT,
                                    final_out=(out if last else None))
                    if not last:
                        k.dma("pool", [(xT[:, :, tsl], xs[tb % 2][:])], reads=[Bxs[tb % 2]], writes=[B_xT], partial=True)
                k.barrier()

        phases = [("tables", phase_tables), ("ln_in", phase_ln_in)]
        for l in range(depth):
            phases += [("proj%d" % l, lambda l=l: phase_proj(l)),
                       ("dense%d" % l, lambda l=l: phase_attn_dense(l)), ("dil%d" % l, lambda l=l: phase_attn_dil(l)),
                       ("merge%d" % l, lambda l=l: phase_merge(l)),
                       ("moe%d" % l, lambda l=l: phase_moe(l, l == depth - 1))]
        skip = dbg.get("skip", ())
        for name, fn in phases:
            if name in skip:
                continue
            fn()
            if stop_after == name:
                break
        k.barrier()
    return nc


def make_in_maps(inputs, ncores=8):
    c = _host_consts()
    f = lambda a: np.ascontiguousarray(np.asarray(a, dtype=np.float32))
    shared = {
        "ln_in_g": f(inputs["ln_in_g"]), "ln_in_b": f(inputs["ln_in_b"]),
        "w_in": f(inputs["w_in"]), "b_gate": f(inputs["b_gate"]),
        "lam_all": np.ascontiguousarray(np.stack([f(inputs["lam_q1"]), f(inputs["lam_k1"]), f(inputs["lam_q2"]),
                                                  f(inputs["lam_k2"])], axis=1)),
        "diff_norm_g": f(inputs["diff_norm_g"]), "mla_q_norm_g": f(inputs["mla_q_norm_g"]),
        "mla_kv_norm_g": f(inputs["mla_kv_norm_g"]), "w_mla_qb": f(inputs["w_mla_qb"]),
        "w_mla_kvb": f(inputs["w_mla_kvb"]),
        "w_branch": np.ascontiguousarray(np.stack([f(inputs["w_branch_a"]), f(inputs["w_branch_b"]),
                                                   f(inputs["w_branch_c"])], axis=1)),
        "w_out": f(inputs["w_out"]), "ln1_g": f(inputs["ln1_g"]), "ln1_b": f(inputs["ln1_b"]),
        "router_w": f(inputs["router_w"]),
        "router_bias_t": np.ascontiguousarray(np.tile(f(inputs["router_bias"]), NT)),
        "w_eg": np.ascontiguousarray(np.concatenate([f(inputs["w_exp_gate"]), f(inputs["w_sh_gate"])[:, None]], axis=1)),
        "w_eu": np.ascontiguousarray(np.concatenate([f(inputs["w_exp_up"]), f(inputs["w_sh_up"])[:, None]], axis=1)),
        "w_ed": np.ascontiguousarray(np.concatenate([f(inputs["w_exp_down"]), f(inputs["w_sh_down"])[:, None]], axis=1)),
        "ln2_g": f(inputs["ln2_g"]), "ln2_b": f(inputs["ln2_b"]),
        "c_ident_f": c["ident_f"], "c_perm": c["perm"], "c_fs": c["fs"], "c_maskb": c["maskb"], "c_selc": c["selc"],
    }
    x = np.asarray(inputs["x"], dtype=np.float32)
    pos = np.asarray(inputs["positions"]).astype(np.int32)
    maps = []
    for b in range(ncores):
        m = dict(shared)
        m["x"] = np.ascontiguousarray(x[b])
        m["positions"] = np.ascontiguousarray(pos[b:b + 1])
        maps.append(m)
    return maps


def kernel(**inputs):
    nc = build_program()
    maps = make_in_maps(inputs, 8)
    res = run_bass_kernel_spmd(nc, maps, core_ids=list(range(8)))
    return np.stack([np.asarray(r["out"], dtype=np.float32) for r in res.results], axis=0)
```

```python
import contextlib
import math
import numpy as np
import concourse.bass as bass
import concourse.mybir as mybir
from concourse.bass_utils import run_bass_kernel_spmd

F32 = mybir.dt.float32
BF16 = mybir.dt.bfloat16
I32 = mybir.dt.int32
AF = mybir.ActivationFunctionType
ALU = mybir.AluOpType
AX = mybir.AxisListType

S = 8192
D = 1024
NT = S // 128
TB = 512
NTB = S // TB
DEPTH = 2
INW = 8352
ALPHA = (2 * DEPTH) ** 0.25
THETA = 500000.0
TWO_PI = 2.0 * math.pi
C1 = 6.28125
C2 = TWO_PI - C1
NEG = -30000.0

O_AQ, O_AK, O_AV, O_BCQ, O_BCKV, O_BKR, O_CQ, O_CK, O_CV, O_G = (
    0, 512, 1024, 1536, 1920, 2176, 2208, 2976, 3744, 5280)

FM_BLOCKS = []
for i in range(4):
    FM_BLOCKS.append(("aq", i, O_AQ + 128 * i, 128))
for i in range(4):
    FM_BLOCKS.append(("ak", i, O_AK + 128 * i, 128))
for i in range(3):
    FM_BLOCKS.append(("bcq", i, O_BCQ + 128 * i, 128))
for i in range(2):
    FM_BLOCKS.append(("bckv", i, O_BCKV + 128 * i, 128))
FM_BLOCKS.append(("bkr", 0, O_BKR, 32))
for i in range(6):
    FM_BLOCKS.append(("cq", i, O_CQ + 128 * i, 128))
for i in range(6):
    FM_BLOCKS.append(("ck", i, O_CK + 128 * i, 128))
for i in range(24):
    FM_BLOCKS.append(("g", i, O_G + 128 * i, 128))
NFM = len(FM_BLOCKS)
TM_BLOCKS = [("av", 0, O_AV)] + [("cv", i, O_CV + 512 * i) for i in range(3)]

C_DELTA = (1, 2, 8)
C_DIL = (1, 4, 16)
C_MASKS = [(g, dl) for g in range(3) for dl in range(-C_DELTA[g], C_DELTA[g] + 1)]
RING = 20


class Buf:
    __slots__ = ("name", "w", "r", "dsem")

    def __init__(self, name):
        self.name = name
        self.w = None
        self.r = {}
        self.dsem = None


class K:
    def __init__(self, nc, es):
        self.nc = nc
        self.eng = {"pe": nc.tensor, "act": nc.scalar, "dve": nc.vector, "pool": nc.gpsimd, "sp": nc.sync}
        self.sem = {}
        self.cnt = {}
        for e in ("pe", "act", "dve", "pool"):
            self.sem[e] = es.enter_context(nc.semaphore("e_" + e))
            self.cnt[e] = 0
        self.waited = {e: {} for e in self.eng}
        self.pool_sems = []
        self.free_hw = []
        self.free_sw = []
        for i in range(88):
            self.pool_sems.append([es.enter_context(nc.semaphore("d%d" % i)), 0])
            (self.free_hw if i < 62 else self.free_sw).append(i)
        self.semkey = {}
        self.phase_bufs = []

    def buf(self, name):
        b = Buf(name)
        self.phase_bufs.append(b)
        return b

    def _key(self, sem):
        return id(sem)

    def _wait(self, e, deps):
        for (sem, val, owner) in deps:
            k = id(sem)
            if self.waited[e].get(k, 0) >= val:
                continue
            self.eng[e].wait_ge(sem, val)
            self.waited[e][k] = val

    def _deps(self, e, reads, writes, partial=False):
        deps = []
        for b in reads:
            if b.w is not None:
                if not (b.w[2] == e and e == "pe"):
                    deps.append(b.w)
        same = (lambda owner: owner == e and e != "dma")
        for b in writes:
            if b.w is not None and not same(b.w[2]):
                if not (partial and b.w[2] == "dma"):
                    deps.append(b.w)
            for t in b.r.values():
                if not same(t[2]):
                    deps.append(t)
        return deps

    def _commit(self, tok, reads, writes):
        for b in reads:
            b.r[id(tok[0])] = tok
        for b in writes:
            b.w = tok
            b.r = {}

    def op(self, e, fn, reads=(), writes=()):
        self._wait(e, self._deps(e, reads, writes))
        ins = fn(self.eng[e])
        ins.then_inc(self.sem[e], 1)
        self.cnt[e] += 1
        tok = (self.sem[e], self.cnt[e], e)
        self._commit(tok, reads, writes)
        return tok

    def mm_group(self, fns, reads=(), writes=()):
        self._wait("pe", self._deps("pe", reads, writes))
        ins = None
        for f in fns:
            ins = f(self.nc.tensor)
        ins.then_inc(self.sem["pe"], 1)
        self.cnt["pe"] += 1
        tok = (self.sem["pe"], self.cnt["pe"], "pe")
        self._commit(tok, reads, writes)
        return tok

    def dma(self, q, pairs, reads=(), writes=(), partial=False):
        self._wait(q, self._deps("dma", reads, writes, partial=partial))
        b = writes[0]
        if b.dsem is None:
            b.dsem = (self.free_sw if q == "pool" else self.free_hw).pop()
        assert (b.dsem >= 62) == (q == "pool"), "buffer %s mixes DMA queue kinds" % b.name
        ent = self.pool_sems[b.dsem]
        for (o, i) in pairs:
            self.eng[q].dma_start(out=o, in_=i).then_inc(ent[0], 16)
            ent[1] += 16
        tok = (ent[0], ent[1], "dma")
        self._commit(tok, reads, writes)
        return tok

    def barrier(self):
        deps = [(self.sem[e], self.cnt[e], e) for e in self.sem if self.cnt[e] > 0]
        for ent in self.pool_sems:
            if ent[1] > 0:
                deps.append((ent[0], ent[1], "dma"))
        for e in self.eng:
            self._wait(e, deps)
        for b in self.phase_bufs:
            if b.dsem is not None:
                (self.free_sw if b.dsem >= 62 else self.free_hw).append(b.dsem)
                b.dsem = None
            b.w = None
            b.r = {}
        self.phase_bufs = []


def _host_consts():
    c = {}
    c["ident_f"] = np.eye(128, dtype=np.float32)
    pa = np.zeros((128, 128), np.float32)
    for hh in range(2):
        for i in range(8):
            pa[hh * 64 + i + 8, hh * 64 + i] = 1.0
            pa[hh * 64 + i, hh * 64 + i + 8] = 1.0
    pb = np.zeros((128, 128), np.float32)
    for i in range(16):
        pb[64 + i + 16, 64 + i] = 1.0
        pb[64 + i, 64 + i + 16] = 1.0
    pk = np.zeros((128, 128), np.float32)
    for i in range(16):
        pk[i + 16, i] = 1.0
        pk[i, i + 16] = 1.0
    c["perm"] = np.stack([pa, pb, pk], 0)
    fs = np.zeros((128, 6), np.float32)
    invp = (THETA ** (-np.arange(0, 16, 2, dtype=np.float32) / np.float32(16))).astype(np.float32)
    invm = (THETA ** (-np.arange(0, 32, 2, dtype=np.float32) / np.float32(32))).astype(np.float32)
    for hh in range(2):
        for i in range(8):
            fs[hh * 64 + i, 0] = invp[i]
            fs[hh * 64 + i, 1] = -1.0
            fs[hh * 64 + 8 + i, 0] = invp[i]
            fs[hh * 64 + 8 + i, 1] = 1.0
    for i in range(16):
        fs[64 + i, 2] = invm[i]
        fs[64 + i, 3] = -1.0
        fs[80 + i, 2] = invm[i]
        fs[80 + i, 3] = 1.0
        fs[i, 4] = invm[i]
        fs[i, 5] = -1.0
        fs[16 + i, 4] = invm[i]
        fs[16 + i, 5] = 1.0
    c["fs"] = fs
    kk = np.arange(128)[:, None]
    qq = np.arange(128)[None, :]
    mb = np.zeros((128, len(C_MASKS), 128), np.float32)
    for mi, (g, dl) in enumerate(C_MASKS):
        d = C_DIL[g]
        diff = dl * 128 + kk - qq
        ok = (diff % d == 0) & (np.abs(diff) <= 64 * d)
        mb[:, mi, :] = np.where(ok, 1.0, 0.0)
    c["maskb"] = mb
    sel = np.zeros((16, 16, 128), np.float32)
    for e in range(16):
        sel[e, e, :] = 1.0
    c["selc"] = sel
    return c


def build_program(depth=DEPTH, dbg=None):
    nc = bass.Bass("TRN2", target_bir_lowering=False)
    dbg = dbg or {}

    def din(name, shape, dt=F32):
        return nc.dram_tensor(name, list(shape), dt, kind="ExternalInput").ap()

    def dscr(name, shape, dt):
        kind = "ExternalOutput" if name in dbg.get("outs", ()) else "Internal"
        return nc.dram_tensor(name, list(shape), dt, kind=kind).ap()

    x_in = din("x", [S, D])
    pos_in = din("positions", [1, S], I32)
    ln_in_g = din("ln_in_g", [D])
    ln_in_b = din("ln_in_b", [D])
    w_in = din("w_in", [DEPTH, D, INW])
    b_gate = din("b_gate", [DEPTH, 3 * D])
    lam_in = din("lam_all", [DEPTH, 4, 64])
    diff_g = din("diff_norm_g", [DEPTH, 128])
    q_norm_g = din("mla_q_norm_g", [DEPTH, 384])
    kv_norm_g = din("mla_kv_norm_g", [DEPTH, 256])
    w_qb = din("w_mla_qb", [DEPTH, 384, 768])
    w_kvb = din("w_mla_kvb", [DEPTH, 256, 1024])
    w_br = din("w_branch", [DEPTH, 3, 512, D])
    w_out = din("w_out", [DEPTH, D, D])
    ln1_g = din("ln1_g", [DEPTH, D])
    ln1_b = din("ln1_b", [DEPTH, D])
    router_w = din("router_w", [D, 16])
    router_bias = din("router_bias_t", [NT * 16])
    w_eg = din("w_eg", [DEPTH, 17, D, 256])
    w_eu = din("w_eu", [DEPTH, 17, D, 256])
    w_ed = din("w_ed", [DEPTH, 17, 256, D])
    ln2_g = din("ln2_g", [DEPTH, D])
    ln2_b = din("ln2_b", [DEPTH, D])
    c_ident = din("c_ident_f", [128, 128])
    c_perm = din("c_perm", [3, 128, 128])
    c_fs = din("c_fs", [128, 6])
    c_maskb = din("c_maskb", [128, len(C_MASKS), 128])
    c_selc = din("c_selc", [16, 16, 128])
    out = nc.dram_tensor("out", [S, D], F32, kind="ExternalOutput").ap()

    xres = dscr("xres", [S, D], F32)
    x1res = dscr("x1res", [S, D], F32)
    xT = dscr("xT", [128, 8, S], BF16)
    x1T = dscr("x1T", [128, 8, S], BF16)
    tabs = dscr("tabs", [6, 128, S], F32)
    WS = []
    for l_ in range(DEPTH):
        WS.append(dict(
            w1=dscr("w1_%d" % l_, [NFM, 128, 8, 128], BF16), w1v=dscr("w1v_%d" % l_, [4, 128, 8, 512], BF16),
            wqb_s=dscr("wqb_s_%d" % l_, [128, 3, 768], BF16), wkvk_s=dscr("wkvk_s_%d" % l_, [128, 2, 512], BF16),
            wkvv_s=dscr("wkvv_s_%d" % l_, [128, 2, 512], BF16), wbr_s=dscr("wbr_s_%d" % l_, [128, 12, D], BF16),
            wout_s=dscr("wout_s_%d" % l_, [128, 8, D], BF16), weg_s=dscr("weg_s_%d" % l_, [17, 128, 8, 256], BF16),
            weu_s=dscr("weu_s_%d" % l_, [17, 128, 8, 256], BF16), wed_s=dscr("wed_s_%d" % l_, [128, 34, D], BF16)))
    qA = dscr("qA", [4, 128, S], BF16)
    kA = dscr("kA", [4, 128, S], BF16)
    vA = dscr("vA", [4, 128, NT, 129], BF16)
    qB = dscr("qB", [8, 96, S], BF16)
    kB = dscr("kB", [8, 96, S], BF16)
    vB = dscr("vB", [8, 128, NT, 65], BF16)
    qC = dscr("qC", [6, 128, S], BF16)
    kC = dscr("kC", [6, 128, S], BF16)
    vC = dscr("vC", [S, 12 * 129], BF16)
    gT = dscr("gT", [24, 128, S], BF16)
    oT = dscr("oT", [12, 128, S], BF16)
    gateT = dscr("gateT", [16, S], F32)

    stop_after = dbg.get("stop_after", None)

    with contextlib.ExitStack() as es:
        es.enter_context(nc.allow_non_contiguous_dma(reason="small strided parameter loads"))
        k = K(nc, es)
        uniq = [0]

        def sb(stack, name, shape, dt):
            uniq[0] += 1
            return stack.enter_context(nc.sbuf_tensor("%s_%d" % (name, uniq[0]), list(shape), dt))

        def ps(stack, name, shape, dt=F32):
            uniq[0] += 1
            return stack.enter_context(nc.psum_tensor("%s_%d" % (name, uniq[0]), list(shape), dt))

        ident_f = sb(es, "ident_f", [128, 128], F32)
        ident_b = sb(es, "ident_b", [128, 128], BF16)
        ones_f = sb(es, "ones_f", [128, 128], F32)
        perm_b = sb(es, "perm_b", [128, 3, 128], BF16)
        fs_t = sb(es, "fs_t", [128, 6], F32)
        eps5 = sb(es, "eps5", [128, 1], F32)
        eps6 = sb(es, "eps6", [128, 1], F32)
        pi_t = sb(es, "pi_t", [128, 1], F32)
        scores_all = sb(es, "scores_all", [128, NT, 16], F32)
        B_const = k.buf("const")
        B_scores = Buf("scores")
        perm_f = sb(es, "perm_f", [128, 3, 128], F32)
        k.dma("sp", [(ident_f[:], c_ident[:, :]), (fs_t[:], c_fs[:, :]),
                     (perm_f[:], c_perm.rearrange("a p n -> p a n"))], writes=[B_const])
        k.op("dve", lambda e: e.tensor_copy(out=ident_b[:], in_=ident_f[:]), reads=[B_const], writes=[B_const])
        k.op("dve", lambda e: e.tensor_copy(out=perm_b[:], in_=perm_f[:]), reads=[B_const], writes=[B_const])
        k.op("pool", lambda e: e.memset(ones_f[:], 1.0), writes=[B_const])
        k.op("pool", lambda e: e.memset(eps5[:], 1e-5), writes=[B_const])
        k.op("pool", lambda e: e.memset(eps6[:], 1e-6), writes=[B_const])
        k.op("pool", lambda e: e.memset(pi_t[:], math.pi), writes=[B_const])
        k.barrier()

        def finish_tile(st, pfx, rbuf, r_t, gam, bet, B_gb, tidx, res_dram, xT_stage, B_xTs, psT, B_psT,
                        router=None, final_out=None, defer_tr=False):
            ring = st["ring"][tidx % len(st["ring"])]
            stats, mv, B_st = ring
            k.op("dve", lambda e: e.bn_stats(out=stats[:, 0, :], in_=r_t[:, 0:512]), reads=[rbuf], writes=[B_st])
            k.op("dve", lambda e: e.bn_stats(out=stats[:, 1, :], in_=r_t[:, 512:1024]), reads=[rbuf], writes=[B_st])
            k.op("dve", lambda e: e.bn_aggr(out=mv[:, 0:2], in_=stats[:]), reads=[B_st], writes=[B_st])
            k.op("act", lambda e: e.activation(out=mv[:, 2:3], in_=mv[:, 1:2], func=AF.Sqrt, bias=eps5[:, 0:1], scale=1.0),
                 reads=[B_st], writes=[B_st])
            k.op("dve", lambda e: e.reciprocal(out=mv[:, 3:4], in_=mv[:, 2:3]), reads=[B_st], writes=[B_st])
            k.op("dve", lambda e: e.tensor_scalar(out=r_t[:], in0=r_t[:], scalar1=mv[:, 0:1], scalar2=mv[:, 3:4],
                                                   op0=ALU.subtract, op1=ALU.mult), reads=[B_st, rbuf], writes=[rbuf])
            k.op("pool", lambda e: e.tensor_tensor(out=r_t[:], in0=r_t[:], in1=gam[:], op=ALU.mult),
                 reads=[rbuf, B_gb], writes=[rbuf])
            k.op("pool", lambda e: e.tensor_tensor(out=r_t[:], in0=r_t[:], in1=bet[:], op=ALU.add),
                 reads=[rbuf, B_gb], writes=[rbuf])
            if final_out is not None:
                k.dma("sp", [(final_out[tidx * 128:(tidx + 1) * 128, :], r_t[:])], reads=[rbuf], writes=[st["B_out"]],
                      partial=True)
                return
            k.dma("sp", [(res_dram[tidx * 128:(tidx + 1) * 128, :], r_t[:])], reads=[rbuf], writes=[st["B_res"]],
                  partial=True)
            if defer_tr:
                return lambda: finish_tr(rbuf, r_t, tidx, xT_stage, B_xTs, psT, B_psT, router)
            finish_tr(rbuf, r_t, tidx, xT_stage, B_xTs, psT, B_psT, router)

        def finish_tr(rbuf, r_t, tidx, xT_stage, B_xTs, psT, B_psT, router):
            j = tidx % 4
            for half in range(2):
                k.mm_group([(lambda e, c=c: e.transpose(out=psT[:, (c % 4) * 128:(c % 4 + 1) * 128],
                                                         in_=r_t[:, c * 128:(c + 1) * 128], identity=ident_f[:]))
                            for c in range(half * 4, half * 4 + 4)], reads=[rbuf], writes=[B_psT])
                if router is not None:
                    xf = router["xf"][tidx % 2]
                    Bxf_ = router["B_xf"][tidx % 2]
                    k.op("dve", lambda e, half=half: e.tensor_copy(
                        out=xf[:, half * 4:half * 4 + 4, :],
                        in_=psT[:].rearrange("p (c t) -> p c t", c=4)), reads=[B_psT], writes=[Bxf_])
                    k.op("pool", lambda e, half=half: e.tensor_copy(
                        out=xT_stage[:, half * 4:half * 4 + 4, j * 128:(j + 1) * 128],
                        in_=xf[:, half * 4:half * 4 + 4, :]), reads=[Bxf_], writes=[B_xTs])
                else:
                    k.op("act", lambda e, half=half: e.activation(
                        out=xT_stage[:, half * 4:half * 4 + 4, j * 128:(j + 1) * 128],
                        in_=psT[:].rearrange("p (c t) -> p c t", c=4), func=AF.Copy), reads=[B_psT], writes=[B_xTs])
            if router is not None:
                xf = router["xf"][tidx % 2]
                Bxf_ = router["B_xf"][tidx % 2]
                k.mm_group([(lambda e, c=c: e.matmul(out=router["ps"][:, 0:16], lhsT=xf[:, c, :], rhs=router["w"][:, c, :],
                                                      start=(c == 0), stop=(c == 7))) for c in range(8)],
                           reads=[Bxf_, B_const], writes=[router["B_ps"]])
                k.op("act", lambda e: e.activation(out=scores_all[:, tidx, :], in_=router["ps"][:, 0:16], func=AF.Sigmoid),
                     reads=[router["B_ps"]], writes=[B_scores])

        def ln_setup(stack, pfx):
            st = {}
            st["ring"] = [(sb(stack, pfx + "stats%d" % i, [128, 2, 6], F32), sb(stack, pfx + "mv%d" % i, [128, 4], F32),
                           k.buf(pfx + "st%d" % i)) for i in range(3)]
            st["B_res"] = k.buf(pfx + "res")
            st["B_out"] = k.buf(pfx + "out")
            return st

        def load_gb(stack, pfx, g_ap, b_ap):
            gam = sb(stack, pfx + "gam", [128, D], F32)
            bet = sb(stack, pfx + "bet", [128, D], F32)
            B_gb = k.buf(pfx + "gb")
            k.dma("sp", [(gam[:], g_ap.partition_broadcast(128)), (bet[:], b_ap.partition_broadcast(128))], writes=[B_gb])
            return gam, bet, B_gb

        def phase_tables():
            with contextlib.ExitStack() as ph:
                posi = sb(ph, "posi", [128, 1024], I32)
                posf = sb(ph, "posf", [128, 1024], F32)
                t_ang = sb(ph, "t_ang", [128, 1024], F32)
                t_k = sb(ph, "t_k", [128, 1024], F32)
                t_n = sb(ph, "t_n", [128, 1024], F32)
                t_ni = sb(ph, "t_ni", [128, 1024], I32)
                t_r = sb(ph, "t_r", [128, 1024], F32)
                t_a = sb(ph, "t_a", [128, 1024], F32)
                t_o = sb(ph, "t_o", [128, 2, 1024], F32)
                B_pos = k.buf("pos"); B_w = k.buf("tw"); B_o = k.buf("to"); B_tab = k.buf("tabs")
                early0, late0 = wprep_jobs(0, ph)
                late_jobs[0] = late0
                for job in early0:
                    job()
                for tb in range(8):
                    sl = slice(tb * 1024, (tb + 1) * 1024)
                    k.dma("sp", [(posi[:], pos_in[0, sl].partition_broadcast(128))], writes=[B_pos])
                    k.op("dve", lambda e: e.tensor_copy(out=posf[:], in_=posi[:]), reads=[B_pos], writes=[B_w])
                    for s in range(3):
                        k.op("dve", lambda e: e.tensor_scalar(out=t_ang[:], in0=posf[:], scalar1=fs_t[:, 2 * s:2 * s + 1],
                                                               scalar2=None, op0=ALU.mult), reads=[B_w], writes=[B_w])
                        k.op("dve", lambda e: e.tensor_scalar(out=t_k[:], in0=t_ang[:], scalar1=1.0 / TWO_PI, scalar2=None,
                                                               op0=ALU.mult), reads=[B_w], writes=[B_w])
                        k.op("dve", lambda e: e.tensor_copy(out=t_ni[:], in_=t_k[:]), reads=[B_w], writes=[B_w])
                        k.op("dve", lambda e: e.tensor_copy(out=t_n[:], in_=t_ni[:]), reads=[B_w], writes=[B_w])
                        k.op("dve", lambda e: e.scalar_tensor_tensor(out=t_r[:], in0=t_n[:], scalar=-C1, in1=t_ang[:],
                                                                      op0=ALU.mult, op1=ALU.add), reads=[B_w], writes=[B_w])
                        k.op("dve", lambda e: e.scalar_tensor_tensor(out=t_r[:], in0=t_n[:], scalar=-C2, in1=t_r[:],
                                                                      op0=ALU.mult, op1=ALU.add), reads=[B_w], writes=[B_w])
                        k.op("dve", lambda e: e.tensor_scalar(out=t_a[:], in0=t_r[:], scalar1=0.0, scalar2=None,
                                                               op0=ALU.is_lt), reads=[B_w], writes=[B_w])
                        k.op("dve", lambda e: e.scalar_tensor_tensor(out=t_r[:], in0=t_a[:], scalar=TWO_PI, in1=t_r[:],
                                                                      op0=ALU.mult, op1=ALU.add), reads=[B_w], writes=[B_w])
                        k.op("dve", lambda e: e.tensor_scalar(out=t_n[:], in0=t_r[:], scalar1=math.pi / 2, scalar2=None,
                                                               op0=ALU.add), reads=[B_w], writes=[B_w])
                        k.op("dve", lambda e: e.tensor_scalar(out=t_a[:], in0=t_n[:], scalar1=TWO_PI, scalar2=None,
                                                               op0=ALU.is_ge), reads=[B_w], writes=[B_w])
                        k.op("dve", lambda e: e.scalar_tensor_tensor(out=t_a[:], in0=t_a[:], scalar=-TWO_PI, in1=t_n[:],
                                                                      op0=ALU.mult, op1=ALU.add), reads=[B_w], writes=[B_w])
                        k.op("dve", lambda e: e.tensor_scalar(out=t_a[:], in0=t_a[:], scalar1=-1.0, scalar2=math.pi,
                                                               op0=ALU.mult, op1=ALU.add), reads=[B_w], writes=[B_w])
                        k.op("dve", lambda e: e.tensor_scalar(out=t_a[:], in0=t_a[:], scalar1=math.pi, scalar2=-math.pi,
                                                               op0=ALU.min, op1=ALU.max), reads=[B_w], writes=[B_w])
                        k.op("act", lambda e: e.activation(out=t_o[:, 0, :], in_=t_a[:], func=AF.Sin),
                             reads=[B_w], writes=[B_o])
                        k.op("dve", lambda e: e.tensor_scalar(out=t_k[:], in0=t_r[:], scalar1=-1.0, scalar2=math.pi,
                                                               op0=ALU.mult, op1=ALU.add), reads=[B_w], writes=[B_w])
                        k.op("dve", lambda e: e.tensor_scalar(out=t_k[:], in0=t_k[:], scalar1=math.pi, scalar2=-math.pi,
                                                               op0=ALU.min, op1=ALU.max), reads=[B_w], writes=[B_w])
                        k.op("act", lambda e: e.activation(out=t_o[:, 1, :], in_=t_k[:], func=AF.Sin),
                             reads=[B_w, B_o], writes=[B_o])
                        k.op("dve", lambda e: e.tensor_scalar(out=t_o[:, 1, :], in0=t_o[:, 1, :],
                                                               scalar1=fs_t[:, 2 * s + 1:2 * s + 2], scalar2=None, op0=ALU.mult),
                             reads=[B_o], writes=[B_o])
                        k.dma("sp", [(tabs[2 * s:2 * s + 2, :, sl].rearrange("a p t -> p a t"), t_o[:])],
                              reads=[B_o], writes=[B_tab], partial=True)
                k.barrier()

        def phase_ln_in():
            with contextlib.ExitStack() as ph:
                st = ln_setup(ph, "l0")
                gam, bet, B_gb = load_gb(ph, "l0", ln_in_g, ln_in_b)
                rt = [sb(ph, "l0r%d" % i, [128, D], F32) for i in range(3)]
                Br = [k.buf("l0r%d" % i) for i in range(3)]
                xs = [sb(ph, "l0xs%d" % i, [128, 8, 512], BF16) for i in range(2)]
                Bxs = [k.buf("l0xs%d" % i) for i in range(2)]
                psT = ps(ph, "l0psT", [128, 512])
                B_psT = k.buf("l0psT")
                B_xT = k.buf("xT")
                for t in range(NT):
                    i = t % 3
                    k.dma("sp", [(rt[i][:], x_in[t * 128:(t + 1) * 128, :])], writes=[Br[i]])
                    sidx = (t // 4) % 2
                    finish_tile(st, "l0", Br[i], rt[i], gam, bet, B_gb, t, xres, xs[sidx], Bxs[sidx], psT, B_psT)
                    if t % 4 == 3:
                        k.dma("pool", [(xT[:, :, (t // 4) * 512:(t // 4 + 1) * 512], xs[sidx][:])], reads=[Bxs[sidx]],
                              writes=[B_xT], partial=True)
                k.barrier()

        B_wprep = [Buf("wprep%d" % i) for i in range(DEPTH)]
        late_jobs = {}

        def wprep_jobs(l, ph):
            W = WS[l]
            B_w = B_wprep[l]
            jobs = []
            pairs = []
            for bi, (kind, idx, col0, ncol) in enumerate(FM_BLOCKS):
                for c in range(8):
                    pairs.append((W["w1"][bi, :, c, 0:ncol], w_in[l, c * 128:(c + 1) * 128, col0:col0 + ncol]))
            for bi, (kind, idx, col0) in enumerate(TM_BLOCKS):
                for c in range(8):
                    pairs.append((W["w1v"][bi, :, c, :], w_in[l, c * 128:(c + 1) * 128, col0:col0 + 512]))
            for br in range(3):
                for c in range(4):
                    pairs.append((W["wbr_s"][:, br * 4 + c, :], w_br[l, br, c * 128:(c + 1) * 128, :]))
            for c in range(8):
                pairs.append((W["wout_s"][:, c, :], w_out[l, c * 128:(c + 1) * 128, :]))
            for e_ in range(17):
                for c in range(8):
                    pairs.append((W["weg_s"][e_, :, c, :], w_eg[l, e_, c * 128:(c + 1) * 128, :]))
                    pairs.append((W["weu_s"][e_, :, c, :], w_eu[l, e_, c * 128:(c + 1) * 128, :]))
                for hc in range(2):
                    pairs.append((W["wed_s"][:, e_ * 2 + hc, :], w_ed[l, e_, hc * 128:(hc + 1) * 128, :]))
            n_early = (NFM + 4) * 8
            early, late = [], []
            for i in range(0, len(pairs), 8):
                (early if i < n_early else late).append(
                    lambda i=i: k.dma("pool", pairs[i:i + 8], writes=[B_w], partial=True))

            def mla_job():
                wq_f = sb(ph, "wq_f", [128, 3, 768], F32)
                wk_f = sb(ph, "wk_f", [128, 2, 1024], F32)
                gq = sb(ph, "gq", [128, 3], F32)
                gk = sb(ph, "gk", [128, 2], F32)
                wq_b = sb(ph, "wq_b", [128, 3, 768], BF16)
                wkk_b = sb(ph, "wkk_b", [128, 2, 512], BF16)
                wkv_b = sb(ph, "wkv_b", [128, 2, 512], BF16)
                B_l = k.buf("wp_l"); B_o = k.buf("wp_o")
                k.dma("sp", [(wq_f[:], w_qb[l].rearrange("(c p) n -> p c n", p=128)),
                             (wk_f[:], w_kvb[l].rearrange("(c p) n -> p c n", p=128)),
                             (gq[:], q_norm_g[l].rearrange("(c p) -> p c", p=128)),
                             (gk[:], kv_norm_g[l].rearrange("(c p) -> p c", p=128))], writes=[B_l])
                for c in range(3):
                    k.op("dve", lambda e, c=c: e.tensor_scalar(out=wq_b[:, c, :], in0=wq_f[:, c, :], scalar1=gq[:, c:c + 1],
                                                               scalar2=None, op0=ALU.mult), reads=[B_l], writes=[B_o])
                for c in range(2):
                    src = wk_f[:, c, :].rearrange("p (h t e) -> p h t e", h=8, t=2)
                    k.op("dve", lambda e, c=c, src=src: e.tensor_scalar(
                        out=wkk_b[:, c, :].rearrange("p (h e) -> p h e", h=8), in0=src[:, :, 0, :],
                        scalar1=gk[:, c:c + 1], scalar2=None, op0=ALU.mult), reads=[B_l], writes=[B_o])
                    k.op("dve", lambda e, c=c, src=src: e.tensor_scalar(
                        out=wkv_b[:, c, :].rearrange("p (h e) -> p h e", h=8), in0=src[:, :, 1, :],
                        scalar1=gk[:, c:c + 1], scalar2=None, op0=ALU.mult), reads=[B_l], writes=[B_o])
                k.dma("pool", [(W["wqb_s"][:, :, :], wq_b[:]), (W["wkvk_s"][:, :, :], wkk_b[:]),
                             (W["wkvv_s"][:, :, :], wkv_b[:])], reads=[B_o], writes=[B_w], partial=True)

            early.append(mla_job)
            return early, late

        def phase_proj(l):
            with contextlib.ExitStack() as ph:
                wqb_t = sb(ph, "p1wqb", [128, 3, 768], BF16)
                wkk_t = sb(ph, "p1wkk", [128, 2, 512], BF16)
                wkv_t = sb(ph, "p1wkv", [128, 2, 512], BF16)
                bg_t = sb(ph, "p1bg", [128, 24], F32)
                B_res = k.buf("p1res")
                W = WS[l]
                k.dma("sp", [(wqb_t[:], W["wqb_s"][:, :, :]), (wkk_t[:], W["wkvk_s"][:, :, :]), (wkv_t[:], W["wkvv_s"][:, :, :]),
                             (bg_t[:], b_gate[l].rearrange("(j p) -> p j", p=128))], writes=[B_res])
                xb = [sb(ph, "p1xb%d" % i, [128, 8, TB], BF16) for i in range(2)]
                Bxb = [k.buf("p1xb%d" % i) for i in range(2)]
                tb_t = [sb(ph, "p1tab%d" % i, [128, 6, TB], F32) for i in range(2)]
                Btb = [k.buf("p1tab%d" % i) for i in range(2)]
                NW = 4
                wt = [sb(ph, "p1w%d" % i, [128, 8, 128], BF16) for i in range(NW)]
                Bwt = [k.buf("p1w%d" % i) for i in range(NW)]
                wv = [sb(ph, "p1wv%d" % i, [128, 8, 512], BF16) for i in range(2)]
                Bwv = [k.buf("p1wv%d" % i) for i in range(2)]
                NP = 4
                pp = [ps(ph, "p1ps%d" % i, [128, 512]) for i in range(NP)]
                Bpp = [k.buf("p1ps%d" % i) for i in range(NP)]
                pr = [ps(ph, "p1pr%d" % i, [128, 512]) for i in range(2)]
                Bpr = [k.buf("p1pr%d" % i) for i in range(2)]
                pq = ps(ph, "p1pq", [128, 512]); Bpq = k.buf("p1pq")
                pz = ps(ph, "p1pz", [128, 512]); Bpz = k.buf("p1pz")
                cT = sb(ph, "p1cT", [128, 5, TB], BF16); BcT = k.buf("p1cT")
                sq = [sb(ph, "p1sq%d" % i, [128, TB], F32) for i in range(2)]
                Bsq = [k.buf("p1sq%d" % i) for i in range(2)]
                rstd = sb(ph, "p1rstd", [128, 2, TB], F32); Brstd = k.buf("p1rstd")
                rtm = sb(ph, "p1rtm", [128, 4], F32); Brtm = k.buf("p1rtm")
                NR = 4
                rawf = [sb(ph, "p1rawf%d" % i, [128, TB], F32) for i in range(NR)]
                Brawf = [k.buf("p1rawf%d" % i) for i in range(NR)]
                rawb = [sb(ph, "p1rawb%d" % i, [128, TB], BF16) for i in range(NR)]
                t1 = [sb(ph, "p1t1%d" % i, [128, TB], F32) for i in range(NR)]
                t2 = [sb(ph, "p1t2%d" % i, [128, TB], F32) for i in range(NR)]
                Braw = [k.buf("p1raw%d" % i) for i in range(NR)]
                Bt1 = [k.buf("p1t1%d" % i) for i in range(NR)]
                Bt2 = [k.buf("p1t2%d" % i) for i in range(NR)]
                NS = 6
                stg = [sb(ph, "p1stg%d" % i, [128, TB], BF16) for i in range(NS)]
                Bstg = [k.buf("p1stg%d" % i) for i in range(NS)]
                vstA = sb(ph, "p1vstA", [128, 4, 4, 129], BF16); BvstA = k.buf("p1vstA")
                vstB = sb(ph, "p1vstB", [128, 4, 8, 65], BF16); BvstB = k.buf("p1vstB")
                vstC = sb(ph, "p1vstC", [128, 4, 12, 129], BF16); BvstC = k.buf("p1vstC")
                k.op("pool", lambda e: e.memset(vstA[:], 1.0), writes=[BvstA])
                k.op("pool", lambda e: e.memset(vstB[:], 1.0), writes=[BvstB])
                k.op("pool", lambda e: e.memset(vstC[:], 1.0), writes=[BvstC])
                B_qA = k.buf("qA"); B_kA = k.buf("kA"); B_vA = k.buf("vA"); B_qB = k.buf("qB"); B_kB = k.buf("kB")
                B_vB = k.buf("vB"); B_qC = k.buf("qC"); B_kC = k.buf("kC"); B_vC = k.buf("vC"); B_gT = k.buf("gT")
                cnt = {"w": 0, "p": 0, "r": 0, "s": 0, "wv": 0, "pr": 0, "sq": 0}

                def rope_out(pst, Bps, rows, tabset, tbi, rstd_ap, dst_ap, Bdst):
                    ri = cnt["r"] % NR; cnt["r"] += 1
                    si = cnt["s"] % NS; cnt["s"] += 1
                    pi_ = cnt["pr"] % 2; cnt["pr"] += 1
                    cosT = tb_t[tbi][0:rows, 2 * tabset, :]
                    sinT = tb_t[tbi][0:rows, 2 * tabset + 1, :]
                    if rstd_ap is None:
                        k.op("act", lambda e: e.activation(out=rawb[ri][0:rows, :], in_=pst[0:rows, :], func=AF.Copy),
                             reads=[Bps], writes=[Braw[ri]])
                        k.op("dve", lambda e: e.tensor_tensor(out=t1[ri][0:rows, :], in0=pst[0:rows, :], in1=cosT,
                                                               op=ALU.mult), reads=[Bps, Btb[tbi], Braw[ri]], writes=[Bt1[ri]])
                    else:
                        k.op("dve", lambda e: e.tensor_tensor(out=rawf[ri][0:rows, :], in0=pst[0:rows, :], in1=rstd_ap,
                                                               op=ALU.mult), reads=[Bps, Brstd], writes=[Brawf[ri]])
                        k.op("act", lambda e: e.activation(out=rawb[ri][0:rows, :], in_=rawf[ri][0:rows, :], func=AF.Copy),
                             reads=[Brawf[ri]], writes=[Braw[ri]])
                        k.op("dve", lambda e: e.tensor_tensor(out=t1[ri][0:rows, :], in0=rawf[ri][0:rows, :], in1=cosT,
                                                               op=ALU.mult), reads=[Brawf[ri], Btb[tbi]], writes=[Bt1[ri]])
                    def part2():
                        k.mm_group([lambda e: e.matmul(out=pr[pi_][0:rows, :], lhsT=perm_b[0:rows, tabset, 0:rows],
                                                       rhs=rawb[ri][0:rows, :], start=True, stop=True)],
                                   reads=[Braw[ri]], writes=[Bpr[pi_]])
                        k.op("dve", lambda e: e.tensor_tensor(out=t2[ri][0:rows, :], in0=pr[pi_][0:rows, :], in1=sinT,
                                                               op=ALU.mult), reads=[Bpr[pi_], Btb[tbi]], writes=[Bt2[ri]])
                        k.op("pool", lambda e: e.tensor_tensor(out=stg[si][0:rows, :], in0=t1[ri][0:rows, :],
                                                                in1=t2[ri][0:rows, :], op=ALU.add),
                             reads=[Bt1[ri], Bt2[ri]], writes=[Bstg[si]])
                        k.dma("pool", [(dst_ap, stg[si][0:rows, :])], reads=[Bstg[si]], writes=[Bdst], partial=True)

                    pend.append(part2)

                pend = []

                def flush():
                    while pend:
                        pend.pop(0)()

                for tb in range(NTB):
                    tbi = tb % 2
                    tsl = slice(tb * TB, (tb + 1) * TB)
                    k.dma("sp", [(xb[tbi][:], xT[:, :, tsl])], writes=[Bxb[tbi]])
                    k.dma("sp", [(tb_t[tbi][:], tabs[:, :, tsl].rearrange("a p t -> p a t"))], writes=[Btb[tbi]])
                    for bi, (kind, idx, col0, ncol) in enumerate(FM_BLOCKS):
                        wi = cnt["w"] % NW; cnt["w"] += 1
                        pi = cnt["p"] % NP; cnt["p"] += 1
                        k.dma("sp", [(wt[wi][:], W["w1"][bi])], writes=[Bwt[wi]])
                        k.mm_group([(lambda e, c=c: e.matmul(out=pp[pi][0:ncol, :], lhsT=wt[wi][:, c, 0:ncol],
                                                              rhs=xb[tbi][:, c, :], start=(c == 0), stop=(c == 7)))
                                    for c in range(8)], reads=[Bwt[wi], Bxb[tbi]], writes=[Bpp[pi]])
                        flush()
                        if kind in ("aq", "ak", "cq", "ck"):
                            dst = {"aq": qA, "ak": kA, "cq": qC, "ck": kC}[kind]
                            Bd = {"aq": B_qA, "ak": B_kA, "cq": B_qC, "ck": B_kC}[kind]
                            rope_out(pp[pi], Bpp[pi], 128, 0, tbi, None, dst[idx, :, tsl], Bd)
                        elif kind in ("bcq", "bckv"):
                            ci = idx if kind == "bcq" else 3 + idx
                            k.op("act", lambda e: e.activation(out=cT[:, ci, :], in_=pp[pi][:], func=AF.Copy),
                                 reads=[Bpp[pi]], writes=[BcT])
                            sqi = cnt["sq"] % 2; cnt["sq"] += 1
                            k.op("act", lambda e: e.activation(out=sq[sqi][:], in_=pp[pi][:], func=AF.Square),
                                 reads=[Bpp[pi]], writes=[Bsq[sqi]])
                            first = idx == 0
                            last = (kind == "bcq" and idx == 2) or (kind == "bckv" and idx == 1)
                            k.mm_group([lambda e: e.matmul(out=pz[:], lhsT=ones_f[:], rhs=sq[sqi][:], start=first, stop=last)],
                                       reads=[Bsq[sqi], B_const], writes=[Bpz])
                            if kind == "bckv":
                                k.mm_group([(lambda e, j=j: e.matmul(out=pq[:, 256 + j:256 + j + 1],
                                                                      lhsT=sq[sqi][:, j * 128:(j + 1) * 128], rhs=ones_f[:, 0:1],
                                                                      start=(first and j == 0), stop=last,
                                                                      skip_group_check=True)) for j in range(4)],
                                           reads=[Bsq[sqi], B_const], writes=[Bpq])
                            if last:
                                ri_ = 0 if kind == "bcq" else 1
                                nfe = 384.0 if kind == "bcq" else 256.0
                                k.op("act", lambda e: e.activation(out=rstd[:, ri_, :], in_=pz[:], func=AF.Sqrt,
                                                                    bias=eps6[:, 0:1], scale=1.0 / nfe),
                                     reads=[Bpz], writes=[Brstd])
                                k.op("dve", lambda e: e.reciprocal(out=rstd[:, ri_, :], in_=rstd[:, ri_, :]),
                                     reads=[Brstd], writes=[Brstd])
                                if kind == "bckv":
                                    k.op("act", lambda e: e.activation(out=rtm[:], in_=pq[:, 256:260], func=AF.Sqrt,
                                                                        bias=eps6[:, 0:1], scale=1.0 / nfe),
                                         reads=[Bpq], writes=[Brtm])
                                    k.op("dve", lambda e: e.reciprocal(out=rtm[:], in_=rtm[:]), reads=[Brtm], writes=[Brtm])
                        elif kind == "bkr":
                            for h in range(8):
                                pass
                            ri = cnt["r"] % NR
                            si_peek = cnt["s"] % NS
                            rope_out(pp[pi], Bpp[pi], 32, 2, tbi, None, kB[0, 64:96, tsl], B_kB)
                            pend.append(lambda si_peek=si_peek, tsl=tsl: k.dma(
                                "pool", [(kB[h, 64:96, tsl], stg[si_peek][0:32, :]) for h in range(1, 8)],
                                reads=[Bstg[si_peek]], writes=[B_kB], partial=True))
                        elif kind == "g":
                            si = cnt["s"] % NS; cnt["s"] += 1
                            k.op("act", lambda e: e.activation(out=stg[si][:], in_=pp[pi][:], func=AF.Sigmoid,
                                                                bias=bg_t[:, idx:idx + 1], scale=1.0),
                                 reads=[Bpp[pi], B_res], writes=[Bstg[si]])
                            k.dma("pool", [(gT[idx, :, tsl], stg[si][:])], reads=[Bstg[si]], writes=[B_gT], partial=True)
                        if kind == "bckv" and idx == 1:
                            for h in range(8):
                                pi2 = cnt["p"] % NP; cnt["p"] += 1
                                k.mm_group([(lambda e, c=c: e.matmul(out=pp[pi2][0:96, :], lhsT=wqb_t[:, c, h * 96:(h + 1) * 96],
                                                                      rhs=cT[:, c, :], start=(c == 0), stop=(c == 2)))
                                            for c in range(3)], reads=[BcT, B_res], writes=[Bpp[pi2]])
                                flush()
                                rope_out(pp[pi2], Bpp[pi2], 96, 1, tbi, rstd[0:96, 0, :], qB[h, :, tsl], B_qB)
                            for h in range(8):
                                pi2 = cnt["p"] % NP; cnt["p"] += 1
                                si = cnt["s"] % NS; cnt["s"] += 1
                                k.mm_group([(lambda e, c=c: e.matmul(out=pp[pi2][0:64, :], lhsT=wkk_t[:, c, h * 64:(h + 1) * 64],
                                                                      rhs=cT[:, 3 + c, :], start=(c == 0), stop=(c == 1)))
                                            for c in range(2)], reads=[BcT, B_res], writes=[Bpp[pi2]])
                                k.op("dve", lambda e: e.tensor_tensor(out=stg[si][0:64, :], in0=pp[pi2][0:64, :],
                                                                       in1=rstd[0:64, 1, :], op=ALU.mult),
                                     reads=[Bpp[pi2], Brstd], writes=[Bstg[si]])
                                k.dma("pool", [(kB[h, 0:64, tsl], stg[si][0:64, :])], reads=[Bstg[si]], writes=[B_kB],
                                      partial=True)
                            for j in range(4):
                                pi2 = cnt["p"] % NP; cnt["p"] += 1
                                k.mm_group([(lambda e, c=c: e.matmul(out=pp[pi2][:, :], lhsT=cT[:, 3 + c, j * 128:(j + 1) * 128],
                                                                      rhs=wkv_t[:, c, :], start=(c == 0), stop=(c == 1)))
                                            for c in range(2)], reads=[BcT, B_res], writes=[Bpp[pi2]])
                                k.op("dve", lambda e: e.tensor_scalar(
                                    out=vstB[:, j, :, 0:64], in0=pp[pi2][:].rearrange("p (h e) -> p h e", h=8),
                                    scalar1=rtm[:, j:j + 1], scalar2=None, op0=ALU.mult),
                                     reads=[Bpp[pi2], Brtm], writes=[BvstB])
                            k.dma("pool", [(vB[h, :, tb * 4:(tb + 1) * 4, :], vstB[:, :, h, :]) for h in range(8)],
                                  reads=[BvstB], writes=[B_vB], partial=True)
                    flush()
                    for bi, (kind, idx, col0) in enumerate(TM_BLOCKS):
                        wvi = cnt["wv"] % 2; cnt["wv"] += 1
                        k.dma("sp", [(wv[wvi][:], W["w1v"][bi])], writes=[Bwv[wvi]])
                        for j in range(4):
                            pi2 = cnt["p"] % NP; cnt["p"] += 1
                            k.mm_group([(lambda e, c=c: e.matmul(out=pp[pi2][:, :], lhsT=xb[tbi][:, c, j * 128:(j + 1) * 128],
                                                                  rhs=wv[wvi][:, c, :], start=(c == 0), stop=(c == 7)))
                                        for c in range(8)], reads=[Bwv[wvi], Bxb[tbi]], writes=[Bpp[pi2]])
                            if kind == "av":
                                k.op("act", lambda e: e.activation(out=vstA[:, j, :, 0:128],
                                                                    in_=pp[pi2][:].rearrange("p (h e) -> p h e", h=4),
                                                                    func=AF.Copy), reads=[Bpp[pi2]], writes=[BvstA])
                            else:
                                k.op("act", lambda e: e.activation(out=vstC[:, j, idx * 4:(idx + 1) * 4, 0:128],
                                                                    in_=pp[pi2][:].rearrange("p (h e) -> p h e", h=4),
                                                                    func=AF.Copy), reads=[Bpp[pi2]], writes=[BvstC])
                        if kind == "av":
                            k.dma("pool", [(vA[h, :, tb * 4:(tb + 1) * 4, :], vstA[:, :, h, :]) for h in range(4)],
                                  reads=[BvstA], writes=[B_vA], partial=True)
                        elif idx == 2:
                            k.dma("pool", [(vC[(tb * 4 + j) * 128:(tb * 4 + j + 1) * 128, :],
                                            vstC[:, j, :, :].rearrange("p h e -> p (h e)")) for j in range(4)],
                                  reads=[BvstC], writes=[B_vC], partial=True)
                k.barrier()

        def lam_setup(ph, l, psm, Bpsm):
            lam_init = 0.8 - 0.6 * math.exp(-0.3 * l)
            lv = sb(ph, "lamv", [1, 4, 64], F32)
            lp = sb(ph, "lamp", [1, 2, 64], F32)
            ls = sb(ph, "lams", [1, 4], F32)
            neglam = sb(ph, "neglam", [128, 1], F32)
            gfac = sb(ph, "gfac", [128, 128], F32)
            B_l = k.buf("lam")
            k.dma("sp", [(lv[:], lam_in[l:l + 1, :, :]), (gfac[:], diff_g[l].partition_broadcast(128))], writes=[B_l])
            k.op("dve", lambda e: e.tensor_tensor(out=lp[:, 0, :], in0=lv[:, 0, :], in1=lv[:, 1, :], op=ALU.mult),
                 reads=[B_l], writes=[B_l])
            k.op("dve", lambda e: e.tensor_tensor(out=lp[:, 1, :], in0=lv[:, 2, :], in1=lv[:, 3, :], op=ALU.mult),
                 reads=[B_l], writes=[B_l])
            k.op("dve", lambda e: e.tensor_reduce(out=ls[:, 0:2], in_=lp[:], axis=AX.X, op=ALU.add), reads=[B_l], writes=[B_l])
            k.op("act", lambda e: e.activation(out=ls[:, 0:2], in_=ls[:, 0:2], func=AF.Exp), reads=[B_l], writes=[B_l])
            k.op("dve", lambda e: e.tensor_tensor(out=ls[:, 2:3], in0=ls[:, 1:2], in1=ls[:, 0:1], op=ALU.subtract),
                 reads=[B_l], writes=[B_l])
            k.op("dve", lambda e: e.tensor_scalar(out=ls[:, 3:4], in0=ls[:, 2:3], scalar1=-lam_init, scalar2=None, op0=ALU.add),
                 reads=[B_l], writes=[B_l])
            k.mm_group([lambda e: e.matmul(out=psm[:, 0:1], lhsT=ones_f[0:1, :], rhs=ls[0:1, 3:4], start=True, stop=True)],
                       reads=[B_l, B_const], writes=[Bpsm])
            k.op("dve", lambda e: e.tensor_copy(out=neglam[:], in_=psm[:, 0:1]), reads=[Bpsm], writes=[B_l])
            k.op("dve", lambda e: e.tensor_scalar(out=gfac[:], in0=gfac[:], scalar1=1.0 - lam_init, scalar2=None, op0=ALU.mult),
                 reads=[B_l], writes=[B_l])
            return neglam, gfac, B_l

        def phase_attn_dense(l):
            with contextlib.ExitStack() as ph:
                neglam_gf = {}
                with contextlib.ExitStack() as ph0:
                    pM = ps(ph0, "aM", [128, 512]); BpM = k.buf("aM")
                    neglam, gfac, B_l = lam_setup(ph, l, pM, BpM)
                    k.barrier()
                B_l = k.buf("lamc")
                NSR = 3
                pS = [ps(ph, "aS%d" % i, [128, 1024]) for i in range(NSR)]
                BpS = [k.buf("aS%d" % i) for i in range(NSR)]
                pAcc = ps(ph, "aAcc", [128, 1024]); BpAcc = k.buf("aAcc")
                qk = [(sb(ph, "aq%d" % i, [128, S], BF16), sb(ph, "ak%d" % i, [128, S], BF16),
                       sb(ph, "av%d" % i, [128, NT, 129], BF16)) for i in range(2)]
                Bqk = [k.buf("aqkv%d" % i) for i in range(2)]
                NE = 4
                E = [sb(ph, "aE%d" % i, [128, 1024], BF16) for i in range(NE)]
                BE = [k.buf("aE%d" % i) for i in range(NE)]
                accs = [sb(ph, "aaccs%d" % i, [128, 4, 129], F32) for i in range(2)]
                Baccs = [k.buf("aaccs%d" % i) for i in range(2)]
                rz = [sb(ph, "arz%d" % i, [128, 4], F32) for i in range(2)]
                Brz = [k.buf("arz%d" % i) for i in range(2)]
                oA = [sb(ph, "aoA%d" % i, [128, 4, 128], F32) for i in range(2)]
                BoA = [k.buf("aoA%d" % i) for i in range(2)]
                acomb = sb(ph, "acomb", [128, 4, 128], F32); Bac = k.buf("acomb")
                asq = sb(ph, "asq", [128, 4, 128], F32)
                ass = sb(ph, "ass", [128, 12], F32); Bass_ = k.buf("ass")
                ob = [sb(ph, "aob%d" % i, [128, 4, 128], BF16) for i in range(2)]
                Bob = [k.buf("aob%d" % i) for i in range(2)]
                ostg = [sb(ph, "aostg%d" % i, [128, 512], BF16) for i in range(2)]
                Bostg = [k.buf("aostg%d" % i) for i in range(2)]
                B_oT = k.buf("oT")
                scale_b = 96.0 ** -0.5

                heads = []
                for h in range(4):
                    heads.append(("A", h))
                for h in range(8):
                    heads.append(("B", h))

                def load_head(hi):
                    kind, h = heads[hi]
                    qb, kb, vb = qk[hi % 2]
                    if kind == "A":
                        k.dma("sp", [(qb[:], qA[h]), (kb[:], kA[h]), (vb[:], vA[h])], writes=[Bqk[hi % 2]])
                    else:
                        k.dma("sp", [(qb[0:96, :], qB[h]), (kb[0:96, :], kB[h]), (vb[:, :, 0:65], vB[h])],
                              writes=[Bqk[hi % 2]])

                items = []
                for hi, (kind, h) in enumerate(heads):
                    for qg in range(2 * NTB if kind == "A" else NTB):
                        for kt2 in range(NT // 2):
                            items.append((hi, kind, h, qg, 0, kt2))
                n = len(items)
                LA = 2
                delayed = {}

                def defer(idx, fn):
                    delayed.setdefault(idx, []).append(fn)

                def ops_of(it):
                    hi, kind, h, qg, m, kt2 = it
                    qb, kb, vb = qk[hi % 2]
                    if kind == "A":
                        return (qb[m * 64:(m + 1) * 64, :], kb[m * 64:(m + 1) * 64, :], vb, 129, 0.125, 256)
                    return (qb[0:96, :], kb[0:96, :], vb, 65, scale_b, 128)

                def emit_qk(i):
                    hi, kind, h, qg, m, kt2 = items[i]
                    if qg == 0 and m == 0 and kt2 == 0 and hi == 0:
                        load_head(0)
                    qT, kT, vt, dv1, scale, accw = ops_of(items[i])
                    si = i % NSR
                    ei = i % NE
                    Bin = Bqk[hi % 2]
                    if kind == "A":
                        qb, kb, vb = qk[hi % 2]
                        k.mm_group([(lambda e, j=j, m=m: e.matmul(
                            out=pS[si][:, m * 512 + j * 256:m * 512 + (j + 1) * 256],
                            lhsT=kb[m * 64:(m + 1) * 64, (kt2 * 2 + j) * 128:(kt2 * 2 + j + 1) * 128],
                            rhs=qb[m * 64:(m + 1) * 64, qg * 256:(qg + 1) * 256], start=True, stop=True))
                                    for j in range(2) for m in range(2)], reads=[Bin], writes=[BpS[si]])
                    else:
                        k.mm_group([(lambda e, j=j: e.matmul(out=pS[si][:, j * 512:(j + 1) * 512],
                                                              lhsT=kT[:, (kt2 * 2 + j) * 128:(kt2 * 2 + j + 1) * 128],
                                                              rhs=qT[:, qg * 512:(qg + 1) * 512], start=True, stop=True))
                                    for j in range(2)], reads=[Bin], writes=[BpS[si]])
                    k.op("act", lambda e: e.activation(out=E[ei][:], in_=pS[si][:], func=AF.Exp, scale=scale),
                         reads=[BpS[si]], writes=[BE[ei]])

                def emit_pv(i):
                    hi, kind, h, qg, m, kt2 = items[i]
                    if qg == 0 and m == 0 and kt2 == 0 and hi + 1 < len(heads):
                        load_head(hi + 1)
                    qT, kT, vt, dv1, scale, accw = ops_of(items[i])
                    ei = i % NE
                    Bin = Bqk[hi % 2]
                    fns = []
                    if kind == "A":
                        for j in range(2):
                            kt = kt2 * 2 + j
                            for m in range(2):
                                for js in range(2):
                                    a_ = m * 2 + js
                                    fns.append(lambda e, j=j, m=m, js=js, kt=kt, a_=a_: e.matmul(
                                        out=pAcc[:, a_ * 256:a_ * 256 + 129],
                                        lhsT=E[ei][:, m * 512 + j * 256 + js * 128:m * 512 + j * 256 + (js + 1) * 128],
                                        rhs=vt[:, kt, 0:129], start=(kt == 0 and a_ % 2 == 0), stop=(kt == NT - 1),
                                        skip_group_check=True))
                    for j in range(2 if kind == "B" else 0):
                        kt = kt2 * 2 + j
                        for js in range(4):
                            fns.append(lambda e, j=j, js=js, kt=kt: e.matmul(
                                out=pAcc[:, js * accw:js * accw + dv1],
                                lhsT=E[ei][:, j * 512 + js * 128:j * 512 + (js + 1) * 128],
                                rhs=vt[:, kt, 0:dv1], start=(kt == 0 and (js * accw) % 512 == 0), stop=(kt == NT - 1),
                                skip_group_check=True))
                    k.mm_group(fns, reads=[BE[ei], Bin], writes=[BpAcc])
                    if kt2 == NT // 2 - 1:
                        post(i)

                gcount = [0]

                def post(i):
                    hi, kind, h, qg, m, kt2 = items[i]
                    g = gcount[0]; gcount[0] += 1
                    ai = g % 2
                    if kind == "A":
                        oi = g % 2
                        k.op("dve", lambda e: e.tensor_copy(
                            out=accs[ai][:], in_=pAcc[:].rearrange("p (j w) -> p j w", w=256)[:, :, 0:129]),
                             reads=[BpAcc], writes=[Baccs[ai]])
                        k.op("dve", lambda e: e.reciprocal(out=rz[ai][:], in_=accs[ai][:, :, 128]),
                             reads=[Baccs[ai]], writes=[Brz[ai]])
                        k.op("pool", lambda e: e.tensor_tensor(
                            out=oA[0][:], in0=accs[ai][:, :, 0:128],
                            in1=rz[ai][:].unsqueeze(2).to_broadcast([128, 4, 128]), op=ALU.mult),
                             reads=[Baccs[ai], Brz[ai]], writes=[BoA[0]])

                        def p1():
                            k.op("dve", lambda e: e.scalar_tensor_tensor(out=acomb[:, 0:2, :], in0=oA[0][:, 2:4, :],
                                                                          scalar=neglam[:, 0:1], in1=oA[0][:, 0:2, :],
                                                                          op0=ALU.mult, op1=ALU.add),
                                 reads=[BoA[0]], writes=[Bac])
                            k.op("pool", lambda e: e.tensor_tensor(out=asq[:, 0:2, :], in0=acomb[:, 0:2, :],
                                                                    in1=acomb[:, 0:2, :], op=ALU.mult),
                                 reads=[Bac], writes=[Bass_])
                            k.op("dve", lambda e: e.tensor_reduce(out=ass[:, 0:2], in_=asq[:, 0:2, :], axis=AX.X, op=ALU.add),
                                 reads=[Bass_], writes=[Bass_])

                        def p2():
                            k.op("act", lambda e: e.activation(out=ass[:, 4:6], in_=ass[:, 0:2], func=AF.Ln,
                                                                bias=eps6[:, 0:1], scale=1.0 / 128.0),
                                 reads=[Bass_], writes=[Bass_])

                        def p3():
                            k.op("act", lambda e: e.activation(out=ass[:, 8:10], in_=ass[:, 4:6], func=AF.Exp, scale=-0.5),
                                 reads=[Bass_], writes=[Bass_])

                        def p4():
                            for js in range(2):
                                k.op("dve", lambda e, js=js: e.scalar_tensor_tensor(
                                    out=ob[oi][:, js, :], in0=acomb[:, js, :], scalar=ass[:, 8 + js:9 + js], in1=gfac[:],
                                    op0=ALU.mult, op1=ALU.mult), reads=[Bac, Bass_], writes=[Bob[oi]])

                        def p5():
                            si = (i + 8 + LA - 1) % NSR
                            pTrb = pS[si][:, 0:512].bitcast(BF16)
                            k.mm_group([(lambda e, js=js: e.transpose(out=pTrb[:, js * 128:(js + 1) * 128],
                                                                       in_=ob[oi][:, js, :], identity=ident_b[:]))
                                        for js in range(2)], reads=[Bob[oi], B_const], writes=[BpS[si]])
                            k.op("dve", lambda e: e.tensor_copy(out=ostg[oi][:, 0:256], in_=pTrb[:, 0:256]), reads=[BpS[si]],
                                 writes=[Bostg[oi]])
                            k.dma("pool", [(oT[h, :, qg * 256:(qg + 1) * 256], ostg[oi][:, 0:256])], reads=[Bostg[oi]],
                                  writes=[B_oT], partial=True)

                        defer(i + 1, p1); defer(i + 3, p2); defer(i + 4, p3); defer(i + 6, p4); defer(i + 8, p5)
                    else:
                        oi = g % 2
                        k.op("dve", lambda e: e.tensor_copy(
                            out=accs[ai][:, :, 0:65], in_=pAcc[:, 0:512].rearrange("p (j w) -> p j w", w=128)[:, :, 0:65]),
                             reads=[BpAcc], writes=[Baccs[ai]])
                        k.op("dve", lambda e: e.reciprocal(out=rz[ai][:], in_=accs[ai][:, :, 64]),
                             reads=[Baccs[ai]], writes=[Brz[ai]])
                        k.op("pool", lambda e: e.tensor_tensor(
                            out=ob[oi][:, :, 0:64], in0=accs[ai][:, :, 0:64],
                            in1=rz[ai][:].unsqueeze(2).to_broadcast([128, 4, 64]), op=ALU.mult),
                             reads=[Baccs[ai], Brz[ai]], writes=[Bob[oi]])

                        def p5():
                            si = (i + 4 + LA - 1) % NSR
                            pTrb = pS[si][:, 0:512].bitcast(BF16)
                            k.mm_group([(lambda e, js=js: e.transpose(out=pTrb[0:64, js * 128:(js + 1) * 128],
                                                                       in_=ob[oi][:, js, 0:64], identity=ident_b[:]))
                                        for js in range(4)], reads=[Bob[oi], B_const], writes=[BpS[si]])
                            k.op("dve", lambda e: e.tensor_copy(out=ostg[oi][0:64, :], in_=pTrb[0:64, 0:512]),
                                 reads=[BpS[si]], writes=[Bostg[oi]])
                            k.dma("pool", [(oT[4 + h // 2, (h % 2) * 64:(h % 2) * 64 + 64, qg * 512:(qg + 1) * 512],
                                            ostg[oi][0:64, :])], reads=[Bostg[oi]], writes=[B_oT], partial=True)

                        defer(i + 4, p5)

                wjobs = list(late_jobs.pop(l, []))
                if l + 1 < depth:
                    e1, l1 = wprep_jobs(l + 1, ph)
                    wjobs += e1 + l1
                for idx in range(n + LA + 10):
                    j = idx - LA
                    if wjobs and idx % 40 == 20:
                        wjobs.pop(0)()
                    if idx < n:
                        emit_qk(idx)
                    if 0 <= j < n:
                        emit_pv(j)
                    if j in delayed:
                        for fn in delayed.pop(j):
                            fn()
                assert not delayed
                while wjobs:
                    wjobs.pop(0)()
                k.barrier()

        def phase_attn_dil(l):
            with contextlib.ExitStack() as ph:
                NSB = 3
                pS = [ps(ph, "cS%d" % i, [128, 512]) for i in range(NSB)]
                BpS = [k.buf("cS%d" % i) for i in range(NSB)]
                pAcc = [ps(ph, "cAcc%d" % i, [128, 1024]) for i in range(2)]
                BpAcc = [k.buf("cAcc%d" % i) for i in range(2)]
                pTr = ps(ph, "cTr", [128, 512]); BpTr = k.buf("cTr")
                pTrb = pTr[:].bitcast(BF16)
                maskf = sb(ph, "cmaskf", [128, len(C_MASKS), 128], F32)
                maskb = sb(ph, "cmaskb", [128, len(C_MASKS), 128], BF16)
                B_m = k.buf("cmask")
                k.dma("sp", [(maskf[:], c_maskb[:, :, :])], writes=[B_m])
                k.op("dve", lambda e: e.tensor_copy(out=maskb[:], in_=maskf[:]), reads=[B_m], writes=[B_m])
                kr = [sb(ph, "ckr%d" % i, [128, 6, 128], BF16) for i in range(RING)]
                vr = [sb(ph, "cvr%d" % i, [128, 12, 129], BF16) for i in range(RING)]
                Bkv = [k.buf("ckv%d" % i) for i in range(RING)]
                NQ = 3
                qr = [sb(ph, "cqr%d" % i, [128, 6, 128], BF16) for i in range(NQ)]
                Bq = [k.buf("cq%d" % i) for i in range(NQ)]
                NE = 4
                E = [sb(ph, "cE%d" % i, [128, 512], BF16) for i in range(NE)]
                BE = [k.buf("cE%d" % i) for i in range(NE)]
                rz = [sb(ph, "crz%d" % i, [128, 4], F32) for i in range(2)]
                Brz = [k.buf("crz%d" % i) for i in range(2)]
                ob = [sb(ph, "cob%d" % i, [128, 4, 128], BF16) for i in range(2)]
                Bob = [k.buf("cob%d" % i) for i in range(2)]
                ostg = [sb(ph, "costg%d" % i, [128, 4, 512], BF16) for i in range(2)]
                Bostg = [k.buf("costg%d" % i) for i in range(2)]
                B_oT = k.buf("oTc")

                def load_kv(kt):
                    sl = kt % RING
                    k.dma("sp", [(kr[sl][:], kC[:, :, kt * 128:(kt + 1) * 128].rearrange("j p t -> p j t")),
                                 (vr[sl][:].rearrange("p h e -> p (h e)"), vC[kt * 128:(kt + 1) * 128, :])],
                          writes=[Bkv[sl]])

                items = []
                for qt in range(NT):
                    for c in range(4):
                        tiles = []
                        for g in range(3):
                            for dl in range(-C_DELTA[g], C_DELTA[g] + 1):
                                kt = qt + dl
                                if 0 <= kt < NT:
                                    tiles.append((g, dl, kt))
                        ntile = len(tiles)
                        for b0 in range(0, ntile, 4):
                            items.append((qt, c, b0, tiles[b0:b0 + 4], ntile, b0 + 4 >= ntile))
                n = len(items)
                LA = 2
                delayed = {}

                def emit_qk(i):
                    qt, c, b0, grp, ntile, lastg = items[i]
                    if c == 0 and b0 == 0:
                        if qt == 0:
                            for kt in range(0, 9):
                                load_kv(kt)
                        elif qt + 8 < NT:
                            load_kv(qt + 8)
                        k.dma("sp", [(qr[qt % NQ][:], qC[:, :, qt * 128:(qt + 1) * 128].rearrange("j p t -> p j t"))],
                              writes=[Bq[qt % NQ]])
                    qi = qt % NQ
                    si = i % NSB
                    ei = i % NE
                    fns = []
                    rd = [Bq[qi], B_m, B_const]
                    for s_, (g, dl, kt) in enumerate(grp):
                        f = g * 4 + c
                        j, hf = f // 2, f % 2
                        mi = C_MASKS.index((g, dl))
                        sl = kt % RING
                        rd.append(Bkv[sl])
                        fns.append(lambda e, s_=s_, j=j, hf=hf, sl=sl: e.matmul(
                            out=pS[si][:, s_ * 128:(s_ + 1) * 128], lhsT=kr[sl][hf * 64:(hf + 1) * 64, j, :],
                            rhs=qr[qi][hf * 64:(hf + 1) * 64, j, :], start=True, stop=True))
                    k.mm_group(fns, reads=rd, writes=[BpS[si]])
                    nn = len(grp)
                    k.op("act", lambda e: e.activation(out=E[ei][:, 0:nn * 128], in_=pS[si][:, 0:nn * 128], func=AF.Exp,
                                                        scale=0.125), reads=[BpS[si]], writes=[BE[ei]])
                    mis = [C_MASKS.index((g, dl)) for (g, dl, kt) in grp]
                    r0 = 0
                    while r0 < nn:
                        r1 = r0 + 1
                        while r1 < nn and mis[r1] == mis[r1 - 1] + 1:
                            r1 += 1
                        meng = "dve" if (i % 2 == 0) else "pool"
                        k.op(meng, lambda e, r0=r0, r1=r1: e.tensor_tensor(
                            out=E[ei][:, r0 * 128:r1 * 128], in0=E[ei][:, r0 * 128:r1 * 128],
                            in1=maskb[:, mis[r0]:mis[r0] + (r1 - r0), :].rearrange("p m t -> p (m t)"), op=ALU.mult),
                             reads=[BE[ei], B_m], writes=[BE[ei]])
                        r0 = r1

                def emit_pv(i):
                    qt, c, b0, grp, ntile, lastg = items[i]
                    ei = i % NE
                    ai = qt % 2
                    fns = []
                    rd = [BE[ei]]
                    for s_, (g, dl, kt) in enumerate(grp):
                        f = g * 4 + c
                        sl = kt % RING
                        rd.append(Bkv[sl])
                        gi = b0 + s_
                        fns.append(lambda e, s_=s_, f=f, sl=sl, gi=gi: e.matmul(
                            out=pAcc[ai][:, c * 256:c * 256 + 129], lhsT=E[ei][:, s_ * 128:(s_ + 1) * 128],
                            rhs=vr[sl][:, f, :], start=(gi == 0), stop=(gi == ntile - 1)))
                    k.mm_group(fns, reads=rd, writes=[BpAcc[ai]])
                    if c == 3 and lastg:
                        oi = qt % 2
                        k.op("dve", lambda e: e.reciprocal(
                            out=rz[oi][:], in_=pAcc[ai][:].rearrange("p (j w) -> p j w", w=256)[:, :, 128]),
                             reads=[BpAcc[ai]], writes=[Brz[oi]])
                        for cc in range(4):
                            k.op("dve", lambda e, cc=cc: e.tensor_scalar(
                                out=ob[oi][:, cc, :], in0=pAcc[ai][:, cc * 256:cc * 256 + 128], scalar1=rz[oi][:, cc:cc + 1],
                                scalar2=None, op0=ALU.mult), reads=[BpAcc[ai], Brz[oi]], writes=[Bob[oi]])

                        def p5():
                            k.mm_group([(lambda e, cc=cc: e.transpose(out=pTrb[:, cc * 128:(cc + 1) * 128], in_=ob[oi][:, cc, :],
                                                                       identity=ident_b[:])) for cc in range(4)],
                                       reads=[Bob[oi], B_const], writes=[BpTr])
                            gi_ = (qt // 4) % 2
                            k.op("dve", lambda e: e.tensor_copy(
                                out=ostg[gi_][:, :, (qt % 4) * 128:(qt % 4 + 1) * 128],
                                in_=pTrb[:, 0:512].rearrange("p (c t) -> p c t", c=4)), reads=[BpTr], writes=[Bostg[gi_]])
                            if qt % 4 == 3:
                                qg = qt // 4
                                k.dma("pool", [(oT[8 + cc, :, qg * 512:(qg + 1) * 512], ostg[gi_][:, cc, :]) for cc in range(4)],
                                      reads=[Bostg[gi_]], writes=[B_oT], partial=True)

                        delayed.setdefault(i + 6, []).append(p5)

                for idx in range(n + LA + 8):
                    j = idx - LA
                    if idx < n:
                        emit_qk(idx)
                    if 0 <= j < n:
                        emit_pv(j)
                    if j in delayed:
                        for fn in delayed.pop(j):
                            fn()
                assert not delayed
                k.barrier()

        def phase_merge(l):
            with contextlib.ExitStack() as ph:
                st = ln_setup(ph, "l1")
                gam, bet, B_gb = load_gb(ph, "l1", ln1_g[l], ln1_b[l])
                wbr_t = sb(ph, "m_wbr", [128, 12, D], BF16)
                wout_t = sb(ph, "m_wout", [128, 8, D], BF16)
                rw_t = sb(ph, "m_rw", [128, 8, 16], F32)
                B_w = k.buf("m_w")
                W = WS[l]
                k.dma("sp", [(wbr_t[:], W["wbr_s"][:, :, :]), (wout_t[:], W["wout_s"][:, :, :]),
                             (rw_t[:], router_w.rearrange("(c p) e -> p c e", p=128))], writes=[B_w])
                ot = [sb(ph, "m_ot%d" % i, [128, 12, TB], BF16) for i in range(2)]
                gt = [sb(ph, "m_gt%d" % i, [128, 24, TB], BF16) for i in range(2)]
                Bin = [k.buf("m_in%d" % i) for i in range(2)]
                NPB = 3
                pb = [ps(ph, "m_p%d" % i, [128, 512]) for i in range(NPB)]
                Bpb = [k.buf("m_p%d" % i) for i in range(NPB)]
                NPM = 3
                pmx = [ps(ph, "m_mix%d" % i, [128, 512]) for i in range(NPM)]
                Bpmx = [k.buf("m_mix%d" % i) for i in range(NPM)]
                psT = ps(ph, "m_psT", [128, 512]); B_psT = k.buf("m_psT")
                prt = ps(ph, "m_prt", [128, 512]); Bprt = k.buf("m_prt")
                mm_ = [[sb(ph, "m_m%d_%d" % (s_, i), [128, TB], F32) for i in range(3)] for s_ in range(2)]
                Bmm = [[k.buf("m_m%d_%d" % (s_, i)) for i in range(3)] for s_ in range(2)]
                yT = [sb(ph, "m_yT%d" % i, [128, 8, TB], BF16) for i in range(2)]
                ByT = [k.buf("m_yT%d" % i) for i in range(2)]
                NRT = 3
                rt = [sb(ph, "m_r%d" % i, [128, D], F32) for i in range(NRT)]
                Brt = [k.buf("m_r%d" % i) for i in range(NRT)]
                xs = [sb(ph, "m_xs%d" % i, [128, 8, 512], BF16) for i in range(2)]
                Bxs = [k.buf("m_xs%d" % i) for i in range(2)]
                xf = [sb(ph, "m_xf%d" % i, [128, 8, 128], F32) for i in range(2)]
                B_xf = [k.buf("m_xf%d" % i) for i in range(2)]
                router = {"xf": xf, "B_xf": B_xf, "ps": prt, "B_ps": Bprt, "w": rw_t}
                B_x1T = k.buf("x1T")
                cnt = {"p": 0, "m": 0}

                def load_in(tb):
                    bi = tb % 2
                    tsl = slice(tb * TB, (tb + 1) * TB)
                    k.dma("sp", [(ot[bi][:], oT[:, :, tsl].rearrange("j p t -> p j t")),
                                 (gt[bi][:], gT[:, :, tsl].rearrange("j p t -> p j t"))], writes=[Bin[bi]])

                def stage_a(tb, dc):
                    bi = tb % 2
                    ms = dc % 2
                    for br in range(3):
                        pi = cnt["p"] % NPB; cnt["p"] += 1
                        k.mm_group([(lambda e, c=c: e.matmul(out=pb[pi][:], lhsT=wbr_t[:, br * 4 + c, dc * 128:(dc + 1) * 128],
                                                              rhs=ot[bi][:, br * 4 + c, :], start=(c == 0), stop=(c == 3)))
                                    for c in range(4)], reads=[B_w, Bin[bi]], writes=[Bpb[pi]])
                        k.op("dve", lambda e, br=br, pi=pi: e.tensor_tensor(
                            out=mm_[ms][br][:], in0=pb[pi][:], in1=gt[bi][:, br * 8 + dc, :], op=ALU.mult),
                             reads=[Bpb[pi], Bin[bi]], writes=[Bmm[ms][br]])
                    k.op("pool", lambda e: e.tensor_tensor(out=mm_[ms][0][:], in0=mm_[ms][0][:], in1=mm_[ms][1][:], op=ALU.add),
                         reads=[Bmm[ms][0], Bmm[ms][1]], writes=[Bmm[ms][0]])
                    k.op("pool", lambda e: e.tensor_tensor(out=yT[bi][:, dc, :], in0=mm_[ms][0][:], in1=mm_[ms][2][:],
                                                            op=ALU.add), reads=[Bmm[ms][0], Bmm[ms][2]], writes=[ByT[bi]])

                def stage_b1(tb, j):
                    bi = tb % 2
                    t = tb * 4 + j
                    ri = t % NRT
                    k.dma("sp", [(rt[ri][:], xres[t * 128:(t + 1) * 128, :])], writes=[Brt[ri]])
                    for hf in range(2):
                        pm = cnt["m"] % NPM; cnt["m"] += 1
                        k.mm_group([(lambda e, dc=dc: e.matmul(out=pmx[pm][:], lhsT=yT[bi][:, dc, j * 128:(j + 1) * 128],
                                                                rhs=wout_t[:, dc, hf * 512:(hf + 1) * 512],
                                                                start=(dc == 0), stop=(dc == 7))) for dc in range(8)],
                                   reads=[ByT[bi], B_w], writes=[Bpmx[pm]])
                        k.op("dve", lambda e, hf=hf, pm=pm: e.scalar_tensor_tensor(
                            out=rt[ri][:, hf * 512:(hf + 1) * 512], in0=rt[ri][:, hf * 512:(hf + 1) * 512], scalar=ALPHA,
                            in1=pmx[pm][:], op0=ALU.mult, op1=ALU.add), reads=[Brt[ri], Bpmx[pm]], writes=[Brt[ri]])
                    sidx = tb % 2
                    return finish_tile(st, "l1", Brt[ri], rt[ri], gam, bet, B_gb, t, x1res, xs[sidx], Bxs[sidx], psT, B_psT,
                                       router=router, defer_tr=True)

                load_in(0)
                for dc in range(8):
                    stage_a(0, dc)
                for tb in range(NTB):
                    nxt = tb + 1 < NTB
                    if nxt:
                        load_in(tb + 1)
                    trs = []
                    for j in range(4):
                        trs.append(stage_b1(tb, j))
                        if nxt:
                            stage_a(tb + 1, 2 * j)
                            stage_a(tb + 1, 2 * j + 1)
                        if j >= 1:
                            trs[j - 1]()
                    trs[3]()
                    tsl = slice(tb * TB, (tb + 1) * TB)
                    k.dma("pool", [(x1T[:, :, tsl], xs[tb % 2][:])], reads=[Bxs[tb % 2]], writes=[B_x1T], partial=True)
                k.barrier()

        def phase_moe(l, last):
            with contextlib.ExitStack() as ph:
                with contextlib.ExitStack() as rs:
                    NN = NT * 16
                    bias_t = sb(rs, "r_bias", [128, NN], F32)
                    biased = sb(rs, "r_biased", [128, NN], F32)
                    p6 = sb(rs, "r_p6", [128, NT * 4, 6], F32)
                    gs = sb(rs, "r_gs", [128, NT, 4], F32)
                    gmax = sb(rs, "r_gmax", [128, NT], F32)
                    gmask = sb(rs, "r_gmask", [128, NT, 4], F32)
                    emask = sb(rs, "r_emask", [128, NN], F32)
                    tneg = sb(rs, "r_tneg", [128, NN], F32)
                    masked = sb(rs, "r_masked", [128, NN], F32)
                    m1 = sb(rs, "r_m1", [128, NT], F32)
                    sel1 = sb(rs, "r_sel1", [128, NN], F32)
                    sel2 = sb(rs, "r_sel2", [128, NN], F32)
                    gate = sb(rs, "r_gate", [128, NT, 16], F32)
                    gts = sb(rs, "r_gts", [16, 512], F32)
                    pg = ps(rs, "r_pg", [128, 512]); Bpg = k.buf("r_pg")
                    B_r = k.buf("r")
                    B_gs = k.buf("r_gts"); B_gateT = k.buf("gateT")
                    k.dma("sp", [(bias_t[:], router_bias.partition_broadcast(128))], writes=[B_r])
                    sc = scores_all[:].rearrange("p t e -> p (t e)")
                    v3 = lambda a: a[:].rearrange("p (t e) -> p t e", e=16)
                    v4 = lambda a: a[:].rearrange("p (g e) -> p g e", e=4)
                    R = [B_r]

                    def dv(fn, extra=()):
                        k.op("dve", fn, reads=[B_r] + list(extra), writes=[B_r])

                    dv(lambda e: e.tensor_tensor(out=biased[:], in0=sc, in1=bias_t[:], op=ALU.add), [B_scores])
                    b4 = v4(biased)
                    pairs = [(0, 1), (0, 2), (0, 3), (1, 2), (1, 3), (2, 3)]
                    for pi_, (a_, b_) in enumerate(pairs):
                        dv(lambda e, pi_=pi_, a_=a_, b_=b_: e.tensor_tensor(out=p6[:, :, pi_], in0=b4[:, :, a_],
                                                                           in1=b4[:, :, b_], op=ALU.add))
                    dv(lambda e: e.tensor_reduce(out=gs[:].rearrange("p t g -> p (t g)"), in_=p6[:], axis=AX.X, op=ALU.max))
                    dv(lambda e: e.tensor_reduce(out=gmax[:], in_=gs[:], axis=AX.X, op=ALU.max))
                    dv(lambda e: e.tensor_tensor(out=gmask[:], in0=gs[:], in1=gmax[:].unsqueeze(2).to_broadcast([128, NT, 4]),
                                                 op=ALU.is_equal))
                    dv(lambda e: e.tensor_copy(out=v4(emask), in_=gmask[:].rearrange("p t g -> p (t g)").unsqueeze(2)
                                               .to_broadcast([128, NT * 4, 4])))
                    dv(lambda e: e.tensor_scalar(out=tneg[:], in0=emask[:], scalar1=1e9, scalar2=-1e9, op0=ALU.mult,
                                                 op1=ALU.add))
                    dv(lambda e: e.tensor_tensor(out=masked[:], in0=biased[:], in1=emask[:], op=ALU.mult))
                    dv(lambda e: e.tensor_tensor(out=masked[:], in0=masked[:], in1=tneg[:], op=ALU.add))
                    dv(lambda e: e.tensor_reduce(out=m1[:], in_=v3(masked), axis=AX.X, op=ALU.max))
                    dv(lambda e: e.tensor_tensor(out=v3(sel1), in0=v3(masked), in1=m1[:].unsqueeze(2).to_broadcast([128, NT, 16]),
                                                 op=ALU.is_equal))
                    dv(lambda e: e.scalar_tensor_tensor(out=masked[:], in0=sel1[:], scalar=-1e9, in1=masked[:], op0=ALU.mult,
                                                        op1=ALU.add))
                    dv(lambda e: e.tensor_reduce(out=m1[:], in_=v3(masked), axis=AX.X, op=ALU.max))
                    dv(lambda e: e.tensor_tensor(out=v3(sel2), in0=v3(masked), in1=m1[:].unsqueeze(2).to_broadcast([128, NT, 16]),
                                                 op=ALU.is_equal))
                    dv(lambda e: e.tensor_tensor(out=sel1[:], in0=sel1[:], in1=sel2[:], op=ALU.add))
                    dv(lambda e: e.tensor_tensor(out=sel1[:], in0=sel1[:], in1=sc, op=ALU.mult), [B_scores])
                    dv(lambda e: e.tensor_reduce(out=m1[:], in_=v3(sel1), axis=AX.X, op=ALU.add))
                    dv(lambda e: e.reciprocal(out=m1[:], in_=m1[:]))
                    dv(lambda e: e.tensor_tensor(out=gate[:], in0=v3(sel1), in1=m1[:].unsqueeze(2).to_broadcast([128, NT, 16]),
                                                 op=ALU.mult))
                    for t4 in range(NT // 4):
                        k.mm_group([(lambda e, j=j: e.transpose(out=pg[0:16, j * 128:(j + 1) * 128], in_=gate[:, t4 * 4 + j, :],
                                                                 identity=ident_f[:])) for j in range(4)],
                                   reads=[B_r, B_const], writes=[Bpg])
                        k.op("dve", lambda e: e.tensor_copy(out=gts[:], in_=pg[0:16, :]), reads=[Bpg], writes=[B_gs])
                        k.dma("sp", [(gateT[:, t4 * 512:(t4 + 1) * 512], gts[:])], reads=[B_gs], writes=[B_gateT], partial=True)
                    k.barrier()
                st = ln_setup(ph, "l2")
                gam, bet, B_gb = load_gb(ph, "l2", ln2_g[l], ln2_b[l])
                wd_t = sb(ph, "e_wd", [128, 34, D], BF16)
                selc = sb(ph, "e_selc", [16, 16, 128], F32)
                B_w = k.buf("e_w")
                W = WS[l]
                k.dma("sp", [(wd_t[:], W["wed_s"][:, :, :]), (selc[:], c_selc[:, :, :])], writes=[B_w])
                xb = [sb(ph, "e_xb%d" % i, [128, 8, TB], BF16) for i in range(2)]
                gtb = [sb(ph, "e_gtb%d" % i, [16, TB], F32) for i in range(2)]
                Bxb = [k.buf("e_xb%d" % i) for i in range(2)]
                NW = 2
                wg = [sb(ph, "e_wg%d" % i, [128, 8, 256], BF16) for i in range(NW)]
                wu = [sb(ph, "e_wu%d" % i, [128, 8, 256], BF16) for i in range(NW)]
                Bwgu = [k.buf("e_wgu%d" % i) for i in range(NW)]
                pgu = [ps(ph, "e_pgu%d" % i, [128, 1024]) for i in range(2)]
                Bpgu = [k.buf("e_pgu%d" % i) for i in range(2)]
                pgb = ps(ph, "e_pgb", [128, 512]); Bpgb = k.buf("e_pgb")
                pdn = ps(ph, "e_pdn", [128, 1024]); Bpdn = k.buf("e_pdn")
                psT = ps(ph, "e_psT", [128, 512]); B_psT = k.buf("e_psT")
                gbc = [sb(ph, "e_gbc%d" % i, [128, TB], F32) for i in range(2)]
                Bgbc = [k.buf("e_gbc%d" % i) for i in range(2)]
                sg = [sb(ph, "e_sg%d" % i, [128, TB], F32) for i in range(2)]
                Bsg = [k.buf("e_sg%d" % i) for i in range(2)]
                h1 = [sb(ph, "e_h1%d" % i, [128, TB], F32) for i in range(2)]
                Bh1 = [k.buf("e_h1%d" % i) for i in range(2)]
                hg = sb(ph, "e_hg", [128, 34, TB], BF16); Bhg = k.buf("e_hg")
                xr = [sb(ph, "e_x%d" % i, [128, D], F32) for i in range(2)]
                Bxr = [k.buf("e_x%d" % i) for i in range(2)]
                rt, Brt = xr, Bxr
                xs = [sb(ph, "e_xs%d" % i, [128, 8, 512], BF16) for i in range(2)]
                Bxs = [k.buf("e_xs%d" % i) for i in range(2)]
                B_xT = k.buf("xTn")
                wc = 0; pc = 0; hc_ = 0
                for tb in range(NTB):
                    bi = tb % 2
                    tsl = slice(tb * TB, (tb + 1) * TB)
                    k.dma("sp", [(xb[bi][:], x1T[:, :, tsl]), (gtb[bi][:], gateT[:, tsl])], writes=[Bxb[bi]])
                    for e_ in range(17):
                        wi = wc % NW; wc += 1
                        k.dma("sp", [(wg[wi][:], W["weg_s"][e_]), (wu[wi][:], W["weu_s"][e_])], writes=[Bwgu[wi]])
                        gi = e_ % 2
                        if e_ < 16:
                            k.mm_group([lambda e: e.matmul(out=pgb[:], lhsT=selc[:, e_, :], rhs=gtb[bi][:], start=True, stop=True)],
                                       reads=[B_w, Bxb[bi]], writes=[Bpgb])
                            k.op("act", lambda e: e.activation(out=gbc[gi][:], in_=pgb[:], func=AF.Copy), reads=[Bpgb],
                                 writes=[Bgbc[gi]])
                        for hf in range(2):
                            pi = pc % 2; pc += 1
                            hi = hc_ % 2; hc_ += 1
                            fns = []
                            for c in range(8):
                                fns.append(lambda e, c=c: e.matmul(out=pgu[pi][:, 0:512], lhsT=wg[wi][:, c, hf * 128:(hf + 1) * 128],
                                                                   rhs=xb[bi][:, c, :], start=(c == 0), stop=(c == 7)))
                            for c in range(8):
                                fns.append(lambda e, c=c: e.matmul(out=pgu[pi][:, 512:1024],
                                                                   lhsT=wu[wi][:, c, hf * 128:(hf + 1) * 128],
                                                                   rhs=xb[bi][:, c, :], start=(c == 0), stop=(c == 7)))
                            k.mm_group(fns, reads=[Bwgu[wi], Bxb[bi]], writes=[Bpgu[pi]])
                            k.op("act", lambda e: e.activation(out=sg[hi][:], in_=pgu[pi][:, 0:512], func=AF.Silu),
                                 reads=[Bpgu[pi]], writes=[Bsg[hi]])
                            ch = e_ * 2 + hf
                            if e_ < 16:
                                k.op("dve", lambda e: e.tensor_tensor(out=h1[hi][:], in0=pgu[pi][:, 512:1024], in1=sg[hi][:],
                                                                       op=ALU.mult), reads=[Bpgu[pi], Bsg[hi]], writes=[Bh1[hi]])
                                k.op("pool", lambda e: e.tensor_tensor(out=hg[:, ch, :], in0=h1[hi][:], in1=gbc[gi][:],
                                                                        op=ALU.mult), reads=[Bh1[hi], Bgbc[gi]], writes=[Bhg])
                            else:
                                k.op("dve", lambda e: e.tensor_tensor(out=hg[:, ch, :], in0=pgu[pi][:, 512:1024], in1=sg[hi][:],
                                                                       op=ALU.mult), reads=[Bpgu[pi], Bsg[hi]], writes=[Bhg])
                    for j in range(4):
                        t = tb * 4 + j
                        ri = t % 2
                        k.dma("sp", [(xr[ri][:], x1res[t * 128:(t + 1) * 128, :])], writes=[Bxr[ri]])
                        for hf in range(2):
                            k.mm_group([(lambda e, ch=ch: e.matmul(out=pdn[:, hf * 512:(hf + 1) * 512],
                                                                    lhsT=hg[:, ch, j * 128:(j + 1) * 128],
                                                                    rhs=wd_t[:, ch, hf * 512:(hf + 1) * 512],
                                                                    start=(ch == 0), stop=(ch == 33))) for ch in range(34)],
                                       reads=[Bhg, B_w], writes=[Bpdn])
                        k.op("dve", lambda e: e.scalar_tensor_tensor(out=rt[ri][:], in0=xr[ri][:], scalar=ALPHA, in1=pdn[:],
                                                                      op0=ALU.mult, op1=ALU.add),
                             reads=[Bxr[ri], Bpdn], writes=[Brt[ri]])
                        sidx = tb % 2
                        finish_tile(st, "l2", Brt[ri], rt[ri], gam, bet, B_gb, t, xres, xs[sidx], Bxs[sidx], psT, B_psT,
                                    final_out=(out if last else None))
                    if not last:
                        k.dma("pool", [(xT[:, :, tsl], xs[tb % 2][:])], reads=[Bxs[tb % 2]], writes=[B_xT], partial=True)
                k.barrier()

        phases = [("tables", phase_tables), ("ln_in", phase_ln_in)]
        for l in range(depth):
            phases += [("proj%d" % l, lambda l=l: phase_proj(l)),
                       ("dense%d" % l, lambda l=l: phase_attn_dense(l)), ("dil%d" % l, lambda l=l: phase_attn_dil(l)),
                       ("merge%d" % l, lambda l=l: phase_merge(l)),
                       ("moe%d" % l, lambda l=l: phase_moe(l, l == depth - 1))]
        skip = dbg.get("skip", ())
        for name, fn in phases:
            if name in skip:
                continue
            fn()
            if stop_after == name:
                break
        k.barrier()
    return nc


def make_in_maps(inputs, ncores=8):
    c = _host_consts()
    f = lambda a: np.ascontiguousarray(np.asarray(a, dtype=np.float32))
    shared = {
        "ln_in_g": f(inputs["ln_in_g"]), "ln_in_b": f(inputs["ln_in_b"]),
        "w_in": f(inputs["w_in"]), "b_gate": f(inputs["b_gate"]),
        "lam_all": np.ascontiguousarray(np.stack([f(inputs["lam_q1"]), f(inputs["lam_k1"]), f(inputs["lam_q2"]),
                                                  f(inputs["lam_k2"])], axis=1)),
        "diff_norm_g": f(inputs["diff_norm_g"]), "mla_q_norm_g": f(inputs["mla_q_norm_g"]),
        "mla_kv_norm_g": f(inputs["mla_kv_norm_g"]), "w_mla_qb": f(inputs["w_mla_qb"]),
        "w_mla_kvb": f(inputs["w_mla_kvb"]),
        "w_branch": np.ascontiguousarray(np.stack([f(inputs["w_branch_a"]), f(inputs["w_branch_b"]),
                                                   f(inputs["w_branch_c"])], axis=1)),
        "w_out": f(inputs["w_out"]), "ln1_g": f(inputs["ln1_g"]), "ln1_b": f(inputs["ln1_b"]),
        "router_w": f(inputs["router_w"]),
        "router_bias_t": np.ascontiguousarray(np.tile(f(inputs["router_bias"]), NT)),
        "w_eg": np.ascontiguousarray(np.concatenate([f(inputs["w_exp_gate"]), f(inputs["w_sh_gate"])[:, None]], axis=1)),
        "w_eu": np.ascontiguousarray(np.concatenate([f(inputs["w_exp_up"]), f(inputs["w_sh_up"])[:, None]], axis=1)),
        "w_ed": np.ascontiguousarray(np.concatenate([f(inputs["w_exp_down"]), f(inputs["w_sh_down"])[:, None]], axis=1)),
        "ln2_g": f(inputs["ln2_g"]), "ln2_b": f(inputs["ln2_b"]),
        "c_ident_f": c["ident_f"], "c_perm": c["perm"], "c_fs": c["fs"], "c_maskb": c["maskb"], "c_selc": c["selc"],
    }
    x = np.asarray(inputs["x"], dtype=np.float32)
    pos = np.asarray(inputs["positions"]).astype(np.int32)
    maps = []
    for b in range(ncores):
        m = dict(shared)
        m["x"] = np.ascontiguousarray(x[b])
        m["positions"] = np.ascontiguousarray(pos[b:b + 1])
        maps.append(m)
    return maps


def kernel(**inputs):
    nc = build_program()
    maps = make_in_maps(inputs, 8)
    res = run_bass_kernel_spmd(nc, maps, core_ids=list(range(8)))
    return np.stack([np.asarray(r["out"], dtype=np.float32) for r in res.results], axis=0)
```

```python
import contextlib
import math
import numpy as np
import concourse.bass as bass
import concourse.mybir as mybir
from concourse.bass_utils import run_bass_kernel_spmd

F32 = mybir.dt.float32
BF16 = mybir.dt.bfloat16
I32 = mybir.dt.int32
AF = mybir.ActivationFunctionType
ALU = mybir.AluOpType
AX = mybir.AxisListType

S = 8192
D = 1024
NT = S // 128
TB = 512
NTB = S // TB
DEPTH = 2
INW = 8352
ALPHA = (2 * DEPTH) ** 0.25
THETA = 500000.0
TWO_PI = 2.0 * math.pi
C1 = 6.28125
C2 = TWO_PI - C1
NEG = -30000.0

O_AQ, O_AK, O_AV, O_BCQ, O_BCKV, O_BKR, O_CQ, O_CK, O_CV, O_G = (
    0, 512, 1024, 1536, 1920, 2176, 2208, 2976, 3744, 5280)

FM_BLOCKS = []
for i in range(4):
    FM_BLOCKS.append(("aq", i, O_AQ + 128 * i, 128))
for i in range(4):
    FM_BLOCKS.append(("ak", i, O_AK + 128 * i, 128))
for i in range(3):
    FM_BLOCKS.append(("bcq", i, O_BCQ + 128 * i, 128))
for i in range(2):
    FM_BLOCKS.append(("bckv", i, O_BCKV + 128 * i, 128))
FM_BLOCKS.append(("bkr", 0, O_BKR, 32))
for i in range(6):
    FM_BLOCKS.append(("cq", i, O_CQ + 128 * i, 128))
for i in range(6):
    FM_BLOCKS.append(("ck", i, O_CK + 128 * i, 128))
for i in range(24):
    FM_BLOCKS.append(("g", i, O_G + 128 * i, 128))
NFM = len(FM_BLOCKS)
TM_BLOCKS = [("av", 0, O_AV)] + [("cv", i, O_CV + 512 * i) for i in range(3)]

C_DELTA = (1, 2, 8)
C_DIL = (1, 4, 16)
C_MASKS = [(g, dl) for g in range(3) for dl in range(-C_DELTA[g], C_DELTA[g] + 1)]
RING = 20


class Buf:
    __slots__ = ("name", "w", "r", "dsem")

    def __init__(self, name):
        self.name = name
        self.w = None
        self.r = {}
        self.dsem = None


class K:
    def __init__(self, nc, es):
        self.nc = nc
        self.eng = {"pe": nc.tensor, "act": nc.scalar, "dve": nc.vector, "pool": nc.gpsimd, "sp": nc.sync}
        self.sem = {}
        self.cnt = {}
        for e in ("pe", "act", "dve", "pool"):
            self.sem[e] = es.enter_context(nc.semaphore("e_" + e))
            self.cnt[e] = 0
        self.waited = {e: {} for e in self.eng}
        self.pool_sems = []
        self.free_hw = []
        self.free_sw = []
        for i in range(88):
            self.pool_sems.append([es.enter_context(nc.semaphore("d%d" % i)), 0])
            (self.free_hw if i < 62 else self.free_sw).append(i)
        self.semkey = {}
        self.phase_bufs = []

    def buf(self, name):
        b = Buf(name)
        self.phase_bufs.append(b)
        return b

    def _key(self, sem):
        return id(sem)

    def _wait(self, e, deps):
        for (sem, val, owner) in deps:
            k = id(sem)
            if self.waited[e].get(k, 0) >= val:
                continue
            self.eng[e].wait_ge(sem, val)
            self.waited[e][k] = val

    def _deps(self, e, reads, writes, partial=False):
        deps = []
        for b in reads:
            if b.w is not None:
                if not (b.w[2] == e and e == "pe"):
                    deps.append(b.w)
        same = (lambda owner: owner == e and e != "dma")
        for b in writes:
            if b.w is not None and not same(b.w[2]):
                if not (partial and b.w[2] == "dma"):
                    deps.append(b.w)
            for t in b.r.values():
                if not same(t[2]):
                    deps.append(t)
        return deps

    def _commit(self, tok, reads, writes):
        for b in reads:
            b.r[id(tok[0])] = tok
        for b in writes:
            b.w = tok
            b.r = {}

    def op(self, e, fn, reads=(), writes=()):
        self._wait(e, self._deps(e, reads, writes))
        ins = fn(self.eng[e])
        ins.then_inc(self.sem[e], 1)
        self.cnt[e] += 1
        tok = (self.sem[e], self.cnt[e], e)
        self._commit(tok, reads, writes)
        return tok

    def mm_group(self, fns, reads=(), writes=()):
        self._wait("pe", self._deps("pe", reads, writes))
        ins = None
        for f in fns:
            ins = f(self.nc.tensor)
        ins.then_inc(self.sem["pe"], 1)
        self.cnt["pe"] += 1
        tok = (self.sem["pe"], self.cnt["pe"], "pe")
        self._commit(tok, reads, writes)
        return tok

    def dma(self, q, pairs, reads=(), writes=(), partial=False):
        self._wait(q, self._deps("dma", reads, writes, partial=partial))
        b = reads[0] if reads else writes[0]
        kind = "sw" if q == "pool" else "hw"
        if b.dsem is None:
            b.dsem = {}
        if kind not in b.dsem:
            b.dsem[kind] = (self.free_sw if kind == "sw" else self.free_hw).pop()
        ent = self.pool_sems[b.dsem[kind]]
        for (o, i) in pairs:
            self.eng[q].dma_start(out=o, in_=i).then_inc(ent[0], 16)
            ent[1] += 16
        tok = (ent[0], ent[1], "dma")
        self._commit(tok, reads, writes)
        return tok

    def barrier(self):
        deps = [(self.sem[e], self.cnt[e], e) for e in self.sem if self.cnt[e] > 0]
        for ent in self.pool_sems:
            if ent[1] > 0:
                deps.append((ent[0], ent[1], "dma"))
        for e in self.eng:
            self._wait(e, deps)
        for b in self.phase_bufs:
            if b.dsem is not None:
                for kind, idx in b.dsem.items():
                    (self.free_sw if kind == "sw" else self.free_hw).append(idx)
                b.dsem = None
            b.w = None
            b.r = {}
        self.phase_bufs = []


def _host_consts():
    c = {}
    c["ident_f"] = np.eye(128, dtype=np.float32)
    pa = np.zeros((128, 128), np.float32)
    for hh in range(2):
        for i in range(8):
            pa[hh * 64 + i + 8, hh * 64 + i] = 1.0
            pa[hh * 64 + i, hh * 64 + i + 8] = 1.0
    pb = np.zeros((128, 128), np.float32)
    for i in range(16):
        pb[64 + i + 16, 64 + i] = 1.0
        pb[64 + i, 64 + i + 16] = 1.0
    pk = np.zeros((128, 128), np.float32)
    for i in range(16):
        pk[i + 16, i] = 1.0
        pk[i, i + 16] = 1.0
    c["perm"] = np.stack([pa, pb, pk], 0)
    fs = np.zeros((128, 6), np.float32)
    invp = (THETA ** (-np.arange(0, 16, 2, dtype=np.float32) / np.float32(16))).astype(np.float32)
    invm = (THETA ** (-np.arange(0, 32, 2, dtype=np.float32) / np.float32(32))).astype(np.float32)
    for hh in range(2):
        for i in range(8):
            fs[hh * 64 + i, 0] = invp[i]
            fs[hh * 64 + i, 1] = -1.0
            fs[hh * 64 + 8 + i, 0] = invp[i]
            fs[hh * 64 + 8 + i, 1] = 1.0
    for i in range(16):
        fs[64 + i, 2] = invm[i]
        fs[64 + i, 3] = -1.0
        fs[80 + i, 2] = invm[i]
        fs[80 + i, 3] = 1.0
        fs[i, 4] = invm[i]
        fs[i, 5] = -1.0
        fs[16 + i, 4] = invm[i]
        fs[16 + i, 5] = 1.0
    c["fs"] = fs
    kk = np.arange(128)[:, None]
    qq = np.arange(128)[None, :]
    mb = np.zeros((128, len(C_MASKS), 128), np.float32)
    for mi, (g, dl) in enumerate(C_MASKS):
        d = C_DIL[g]
        diff = dl * 128 + kk - qq
        ok = (diff % d == 0) & (np.abs(diff) <= 64 * d)
        mb[:, mi, :] = np.where(ok, 1.0, 0.0)
    c["maskb"] = mb
    sel = np.zeros((16, 16, 128), np.float32)
    for e in range(16):
        sel[e, e, :] = 1.0
    c["selc"] = sel
    return c


def build_program(depth=DEPTH, dbg=None):
    nc = bass.Bass("TRN2", target_bir_lowering=False)
    dbg = dbg or {}

    def din(name, shape, dt=F32):
        return nc.dram_tensor(name, list(shape), dt, kind="ExternalInput").ap()

    def dscr(name, shape, dt):
        kind = "ExternalOutput" if name in dbg.get("outs", ()) else "Internal"
        return nc.dram_tensor(name, list(shape), dt, kind=kind).ap()

    x_in = din("x", [S, D])
    pos_in = din("positions", [1, S], I32)
    ln_in_g = din("ln_in_g", [D])
    ln_in_b = din("ln_in_b", [D])
    w_in = din("w_in", [DEPTH, D, INW])
    b_gate = din("b_gate", [DEPTH, 3 * D])
    lam_in = din("lam_all", [DEPTH, 4, 64])
    diff_g = din("diff_norm_g", [DEPTH, 128])
    q_norm_g = din("mla_q_norm_g", [DEPTH, 384])
    kv_norm_g = din("mla_kv_norm_g", [DEPTH, 256])
    w_qb = din("w_mla_qb", [DEPTH, 384, 768])
    w_kvb = din("w_mla_kvb", [DEPTH, 256, 1024])
    w_br = din("w_branch", [DEPTH, 3, 512, D])
    w_out = din("w_out", [DEPTH, D, D])
    ln1_g = din("ln1_g", [DEPTH, D])
    ln1_b = din("ln1_b", [DEPTH, D])
    router_w = din("router_w", [D, 16])
    router_bias = din("router_bias_t", [NT * 16])
    w_eg = din("w_eg", [DEPTH, 17, D, 256])
    w_eu = din("w_eu", [DEPTH, 17, D, 256])
    w_ed = din("w_ed", [DEPTH, 17, 256, D])
    ln2_g = din("ln2_g", [DEPTH, D])
    ln2_b = din("ln2_b", [DEPTH, D])
    c_ident = din("c_ident_f", [128, 128])
    c_perm = din("c_perm", [3, 128, 128])
    c_fs = din("c_fs", [128, 6])
    c_maskb = din("c_maskb", [128, len(C_MASKS), 128])
    c_selc = din("c_selc", [16, 16, 128])
    out = nc.dram_tensor("out", [S, D], F32, kind="ExternalOutput").ap()

    xres = dscr("xres", [S, D], F32)
    x1res = dscr("x1res", [S, D], F32)
    xT = dscr("xT", [128, 8, S], BF16)
    x1T = dscr("x1T", [128, 8, S], BF16)
    tabs = dscr("tabs", [6, 128, S], F32)
    WS = []
    for l_ in range(DEPTH):
        WS.append(dict(
            w1=dscr("w1_%d" % l_, [NFM, 128, 8, 128], BF16), w1v=dscr("w1v_%d" % l_, [4, 128, 8, 512], BF16),
            wqb_s=dscr("wqb_s_%d" % l_, [128, 3, 768], BF16), wkvk_s=dscr("wkvk_s_%d" % l_, [128, 2, 512], BF16),
            wkvv_s=dscr("wkvv_s_%d" % l_, [128, 2, 512], BF16), wbr_s=dscr("wbr_s_%d" % l_, [128, 12, D], BF16),
            wout_s=dscr("wout_s_%d" % l_, [128, 8, D], BF16), weg_s=dscr("weg_s_%d" % l_, [17, 128, 8, 256], BF16),
            weu_s=dscr("weu_s_%d" % l_, [17, 128, 8, 256], BF16), wed_s=dscr("wed_s_%d" % l_, [128, 34, D], BF16)))
    qA = dscr("qA", [4, 128, S], BF16)
    kA = dscr("kA", [4, 128, S], BF16)
    vA = dscr("vA", [4, 128, NT, 129], BF16)
    qB = dscr("qB", [8, 96, S], BF16)
    kB = dscr("kB", [8, 96, S], BF16)
    vB = dscr("vB", [8, 128, NT, 65], BF16)
    qC = dscr("qC", [6, 128, S], BF16)
    kC = dscr("kC", [6, 128, S], BF16)
    vC = dscr("vC", [S, 12 * 129], BF16)
    gT = dscr("gT", [24, 128, S], BF16)
    oT = dscr("oT", [12, 128, S], BF16)
    gateT = dscr("gateT", [16, S], F32)

    stop_after = dbg.get("stop_after", None)

    with contextlib.ExitStack() as es:
        es.enter_context(nc.allow_non_contiguous_dma(reason="small strided parameter loads"))
        k = K(nc, es)
        uniq = [0]

        def sb(stack, name, shape, dt):
            uniq[0] += 1
            return stack.enter_context(nc.sbuf_tensor("%s_%d" % (name, uniq[0]), list(shape), dt))

        def ps(stack, name, shape, dt=F32):
            uniq[0] += 1
            return stack.enter_context(nc.psum_tensor("%s_%d" % (name, uniq[0]), list(shape), dt))

        ident_f = sb(es, "ident_f", [128, 128], F32)
        ident_b = sb(es, "ident_b", [128, 128], BF16)
        ones_f = sb(es, "ones_f", [128, 128], F32)
        perm_b = sb(es, "perm_b", [128, 3, 128], BF16)
        fs_t = sb(es, "fs_t", [128, 6], F32)
        eps5 = sb(es, "eps5", [128, 1], F32)
        eps6 = sb(es, "eps6", [128, 1], F32)
        pi_t = sb(es, "pi_t", [128, 1], F32)
        scores_all = sb(es, "scores_all", [128, NT, 16], F32)
        B_const = k.buf("const")
        B_scores = Buf("scores")
        perm_f = sb(es, "perm_f", [128, 3, 128], F32)
        k.dma("sp", [(ident_f[:], c_ident[:, :]), (fs_t[:], c_fs[:, :]),
                     (perm_f[:], c_perm.rearrange("a p n -> p a n"))], writes=[B_const])
        k.op("dve", lambda e: e.tensor_copy(out=ident_b[:], in_=ident_f[:]), reads=[B_const], writes=[B_const])
        k.op("dve", lambda e: e.tensor_copy(out=perm_b[:], in_=perm_f[:]), reads=[B_const], writes=[B_const])
        k.op("pool", lambda e: e.memset(ones_f[:], 1.0), writes=[B_const])
        k.op("pool", lambda e: e.memset(eps5[:], 1e-5), writes=[B_const])
        k.op("pool", lambda e: e.memset(eps6[:], 1e-6), writes=[B_const])
        k.op("pool", lambda e: e.memset(pi_t[:], math.pi), writes=[B_const])
        k.barrier()

        def finish_tile(st, pfx, rbuf, r_t, gam, bet, B_gb, tidx, res_dram, xT_stage, B_xTs, psT, B_psT,
                        router=None, final_out=None, defer_tr=False):
            ring = st["ring"][tidx % len(st["ring"])]
            stats, mv, B_st = ring
            k.op("dve", lambda e: e.bn_stats(out=stats[:, 0, :], in_=r_t[:, 0:512]), reads=[rbuf], writes=[B_st])
            k.op("dve", lambda e: e.bn_stats(out=stats[:, 1, :], in_=r_t[:, 512:1024]), reads=[rbuf], writes=[B_st])
            k.op("dve", lambda e: e.bn_aggr(out=mv[:, 0:2], in_=stats[:]), reads=[B_st], writes=[B_st])
            k.op("act", lambda e: e.activation(out=mv[:, 2:3], in_=mv[:, 1:2], func=AF.Sqrt, bias=eps5[:, 0:1], scale=1.0),
                 reads=[B_st], writes=[B_st])
            k.op("dve", lambda e: e.reciprocal(out=mv[:, 3:4], in_=mv[:, 2:3]), reads=[B_st], writes=[B_st])
            k.op("dve", lambda e: e.tensor_scalar(out=r_t[:], in0=r_t[:], scalar1=mv[:, 0:1], scalar2=mv[:, 3:4],
                                                   op0=ALU.subtract, op1=ALU.mult), reads=[B_st, rbuf], writes=[rbuf])
            k.op("pool", lambda e: e.tensor_tensor(out=r_t[:], in0=r_t[:], in1=gam[:], op=ALU.mult),
                 reads=[rbuf, B_gb], writes=[rbuf])
            k.op("pool", lambda e: e.tensor_tensor(out=r_t[:], in0=r_t[:], in1=bet[:], op=ALU.add),
                 reads=[rbuf, B_gb], writes=[rbuf])
            if final_out is not None:
                k.dma("sp", [(final_out[tidx * 128:(tidx + 1) * 128, :], r_t[:])], reads=[rbuf], writes=[st["B_out"]],
                      partial=True)
                return
            k.dma("sp", [(res_dram[tidx * 128:(tidx + 1) * 128, :], r_t[:])], reads=[rbuf], writes=[st["B_res"]],
                  partial=True)
            if defer_tr:
                return lambda: finish_tr(rbuf, r_t, tidx, xT_stage, B_xTs, psT, B_psT, router)
            finish_tr(rbuf, r_t, tidx, xT_stage, B_xTs, psT, B_psT, router)

        def finish_tr(rbuf, r_t, tidx, xT_stage, B_xTs, psT, B_psT, router):
            j = tidx % 4
            for half in range(2):
                k.mm_group([(lambda e, c=c: e.transpose(out=psT[:, (c % 4) * 128:(c % 4 + 1) * 128],
                                                         in_=r_t[:, c * 128:(c + 1) * 128], identity=ident_f[:]))
                            for c in range(half * 4, half * 4 + 4)], reads=[rbuf], writes=[B_psT])
                if router is not None:
                    xf = router["xf"][tidx % 2]
                    Bxf_ = router["B_xf"][tidx % 2]
                    k.op("dve", lambda e, half=half: e.tensor_copy(
                        out=xf[:, half * 4:half * 4 + 4, :],
                        in_=psT[:].rearrange("p (c t) -> p c t", c=4)), reads=[B_psT], writes=[Bxf_])
                    k.op("pool", lambda e, half=half: e.tensor_copy(
                        out=xT_stage[:, half * 4:half * 4 + 4, j * 128:(j + 1) * 128],
                        in_=xf[:, half * 4:half * 4 + 4, :]), reads=[Bxf_], writes=[B_xTs])
                else:
                    k.op("act", lambda e, half=half: e.activation(
                        out=xT_stage[:, half * 4:half * 4 + 4, j * 128:(j + 1) * 128],
                        in_=psT[:].rearrange("p (c t) -> p c t", c=4), func=AF.Copy), reads=[B_psT], writes=[B_xTs])
            if router is not None:
                xf = router["xf"][tidx % 2]
                Bxf_ = router["B_xf"][tidx % 2]
                k.mm_group([(lambda e, c=c: e.matmul(out=router["ps"][:, 0:16], lhsT=xf[:, c, :], rhs=router["w"][:, c, :],
                                                      start=(c == 0), stop=(c == 7))) for c in range(8)],
                           reads=[Bxf_, B_const], writes=[router["B_ps"]])
                k.op("act", lambda e: e.activation(out=scores_all[:, tidx, :], in_=router["ps"][:, 0:16], func=AF.Sigmoid),
                     reads=[router["B_ps"]], writes=[B_scores])

        def ln_setup(stack, pfx):
            st = {}
            st["ring"] = [(sb(stack, pfx + "stats%d" % i, [128, 2, 6], F32), sb(stack, pfx + "mv%d" % i, [128, 4], F32),
                           k.buf(pfx + "st%d" % i)) for i in range(3)]
            st["B_res"] = k.buf(pfx + "res")
            st["B_out"] = k.buf(pfx + "out")
            return st

        def load_gb(stack, pfx, g_ap, b_ap):
            gam = sb(stack, pfx + "gam", [128, D], F32)
            bet = sb(stack, pfx + "bet", [128, D], F32)
            B_gb = k.buf(pfx + "gb")
            k.dma("sp", [(gam[:], g_ap.partition_broadcast(128)), (bet[:], b_ap.partition_broadcast(128))], writes=[B_gb])
            return gam, bet, B_gb

        def phase_tables():
            with contextlib.ExitStack() as ph:
                posi = sb(ph, "posi", [128, 1024], I32)
                posf = sb(ph, "posf", [128, 1024], F32)
                t_ang = sb(ph, "t_ang", [128, 1024], F32)
                t_k = sb(ph, "t_k", [128, 1024], F32)
                t_n = sb(ph, "t_n", [128, 1024], F32)
                t_ni = sb(ph, "t_ni", [128, 1024], I32)
                t_r = sb(ph, "t_r", [128, 1024], F32)
                t_a = sb(ph, "t_a", [128, 1024], F32)
                t_o = sb(ph, "t_o", [128, 2, 1024], F32)
                B_pos = k.buf("pos"); B_w = k.buf("tw"); B_o = k.buf("to"); B_tab = k.buf("tabs")
                early0, late0 = wprep_jobs(0, ph)
                late_jobs[0] = late0
                for job in early0:
                    job()
                for tb in range(8):
                    sl = slice(tb * 1024, (tb + 1) * 1024)
                    k.dma("sp", [(posi[:], pos_in[0, sl].partition_broadcast(128))], writes=[B_pos])
                    k.op("dve", lambda e: e.tensor_copy(out=posf[:], in_=posi[:]), reads=[B_pos], writes=[B_w])
                    for s in range(3):
                        k.op("dve", lambda e: e.tensor_scalar(out=t_ang[:], in0=posf[:], scalar1=fs_t[:, 2 * s:2 * s + 1],
                                                               scalar2=None, op0=ALU.mult), reads=[B_w], writes=[B_w])
                        k.op("dve", lambda e: e.tensor_scalar(out=t_k[:], in0=t_ang[:], scalar1=1.0 / TWO_PI, scalar2=None,
                                                               op0=ALU.mult), reads=[B_w], writes=[B_w])
                        k.op("dve", lambda e: e.tensor_copy(out=t_ni[:], in_=t_k[:]), reads=[B_w], writes=[B_w])
                        k.op("dve", lambda e: e.tensor_copy(out=t_n[:], in_=t_ni[:]), reads=[B_w], writes=[B_w])
                        k.op("dve", lambda e: e.scalar_tensor_tensor(out=t_r[:], in0=t_n[:], scalar=-C1, in1=t_ang[:],
                                                                      op0=ALU.mult, op1=ALU.add), reads=[B_w], writes=[B_w])
                        k.op("dve", lambda e: e.scalar_tensor_tensor(out=t_r[:], in0=t_n[:], scalar=-C2, in1=t_r[:],
                                                                      op0=ALU.mult, op1=ALU.add), reads=[B_w], writes=[B_w])
                        k.op("dve", lambda e: e.tensor_scalar(out=t_a[:], in0=t_r[:], scalar1=0.0, scalar2=None,
                                                               op0=ALU.is_lt), reads=[B_w], writes=[B_w])
                        k.op("dve", lambda e: e.scalar_tensor_tensor(out=t_r[:], in0=t_a[:], scalar=TWO_PI, in1=t_r[:],
                                                                      op0=ALU.mult, op1=ALU.add), reads=[B_w], writes=[B_w])
                        k.op("dve", lambda e: e.tensor_scalar(out=t_n[:], in0=t_r[:], scalar1=math.pi / 2, scalar2=None,
                                                               op0=ALU.add), reads=[B_w], writes=[B_w])
                        k.op("dve", lambda e: e.tensor_scalar(out=t_a[:], in0=t_n[:], scalar1=TWO_PI, scalar2=None,
                                                               op0=ALU.is_ge), reads=[B_w], writes=[B_w])
                        k.op("dve", lambda e: e.scalar_tensor_tensor(out=t_a[:], in0=t_a[:], scalar=-TWO_PI, in1=t_n[:],
                                                                      op0=ALU.mult, op1=ALU.add), reads=[B_w], writes=[B_w])
                        k.op("dve", lambda e: e.tensor_scalar(out=t_a[:], in0=t_a[:], scalar1=-1.0, scalar2=math.pi,
                                                               op0=ALU.mult, op1=ALU.add), reads=[B_w], writes=[B_w])
                        k.op("dve", lambda e: e.tensor_scalar(out=t_a[:], in0=t_a[:], scalar1=math.pi, scalar2=-math.pi,
                                                               op0=ALU.min, op1=ALU.max), reads=[B_w], writes=[B_w])
                        k.op("act", lambda e: e.activation(out=t_o[:, 0, :], in_=t_a[:], func=AF.Sin),
                             reads=[B_w], writes=[B_o])
                        k.op("dve", lambda e: e.tensor_scalar(out=t_k[:], in0=t_r[:], scalar1=-1.0, scalar2=math.pi,
                                                               op0=ALU.mult, op1=ALU.add), reads=[B_w], writes=[B_w])
                        k.op("dve", lambda e: e.tensor_scalar(out=t_k[:], in0=t_k[:], scalar1=math.pi, scalar2=-math.pi,
                                                               op0=ALU.min, op1=ALU.max), reads=[B_w], writes=[B_w])
                        k.op("act", lambda e: e.activation(out=t_o[:, 1, :], in_=t_k[:], func=AF.Sin),
                             reads=[B_w, B_o], writes=[B_o])
                        k.op("dve", lambda e: e.tensor_scalar(out=t_o[:, 1, :], in0=t_o[:, 1, :],
                                                               scalar1=fs_t[:, 2 * s + 1:2 * s + 2], scalar2=None, op0=ALU.mult),
                             reads=[B_o], writes=[B_o])
                        k.dma("sp", [(tabs[2 * s:2 * s + 2, :, sl].rearrange("a p t -> p a t"), t_o[:])],
                              reads=[B_o], writes=[B_tab], partial=True)
                k.barrier()

        def phase_ln_in():
            with contextlib.ExitStack() as ph:
                st = ln_setup(ph, "l0")
                gam, bet, B_gb = load_gb(ph, "l0", ln_in_g, ln_in_b)
                rt = [sb(ph, "l0r%d" % i, [128, D], F32) for i in range(3)]
                Br = [k.buf("l0r%d" % i) for i in range(3)]
                xs = [sb(ph, "l0xs%d" % i, [128, 8, 512], BF16) for i in range(2)]
                Bxs = [k.buf("l0xs%d" % i) for i in range(2)]
                psT = ps(ph, "l0psT", [128, 512])
                B_psT = k.buf("l0psT")
                B_xT = k.buf("xT")
                for t in range(NT):
                    i = t % 3
                    k.dma("sp", [(rt[i][:], x_in[t * 128:(t + 1) * 128, :])], writes=[Br[i]])
                    sidx = (t // 4) % 2
                    finish_tile(st, "l0", Br[i], rt[i], gam, bet, B_gb, t, xres, xs[sidx], Bxs[sidx], psT, B_psT)
                    if t % 4 == 3:
                        k.dma("pool", [(xT[:, :, (t // 4) * 512:(t // 4 + 1) * 512], xs[sidx][:])], reads=[Bxs[sidx]],
                              writes=[B_xT], partial=True)
                k.barrier()

        B_wprep = [Buf("wprep%d" % i) for i in range(DEPTH)]
        late_jobs = {}

        def wprep_jobs(l, ph):
            W = WS[l]
            B_w = B_wprep[l]
            jobs = []
            pairs = []
            for bi, (kind, idx, col0, ncol) in enumerate(FM_BLOCKS):
                for c in range(8):
                    pairs.append((W["w1"][bi, :, c, 0:ncol], w_in[l, c * 128:(c + 1) * 128, col0:col0 + ncol]))
            for bi, (kind, idx, col0) in enumerate(TM_BLOCKS):
                for c in range(8):
                    pairs.append((W["w1v"][bi, :, c, :], w_in[l, c * 128:(c + 1) * 128, col0:col0 + 512]))
            for br in range(3):
                for c in range(4):
                    pairs.append((W["wbr_s"][:, br * 4 + c, :], w_br[l, br, c * 128:(c + 1) * 128, :]))
            for c in range(8):
                pairs.append((W["wout_s"][:, c, :], w_out[l, c * 128:(c + 1) * 128, :]))
            for e_ in range(17):
                for c in range(8):
                    pairs.append((W["weg_s"][e_, :, c, :], w_eg[l, e_, c * 128:(c + 1) * 128, :]))
                    pairs.append((W["weu_s"][e_, :, c, :], w_eu[l, e_, c * 128:(c + 1) * 128, :]))
                for hc in range(2):
                    pairs.append((W["wed_s"][:, e_ * 2 + hc, :], w_ed[l, e_, hc * 128:(hc + 1) * 128, :]))
            n_early = (NFM + 4) * 8
            early, late = [], []
            for i in range(0, len(pairs), 8):
                (early if i < n_early else late).append(
                    lambda i=i: k.dma("pool", pairs[i:i + 8], writes=[B_w], partial=True))

            def mla_job():
                wq_f = sb(ph, "wq_f", [128, 3, 768], F32)
                wk_f = sb(ph, "wk_f", [128, 2, 1024], F32)
                gq = sb(ph, "gq", [128, 3], F32)
                gk = sb(ph, "gk", [128, 2], F32)
                wq_b = sb(ph, "wq_b", [128, 3, 768], BF16)
                wkk_b = sb(ph, "wkk_b", [128, 2, 512], BF16)
                wkv_b = sb(ph, "wkv_b", [128, 2, 512], BF16)
                B_l = k.buf("wp_l"); B_o = k.buf("wp_o")
                k.dma("sp", [(wq_f[:], w_qb[l].rearrange("(c p) n -> p c n", p=128)),
                             (wk_f[:], w_kvb[l].rearrange("(c p) n -> p c n", p=128)),
                             (gq[:], q_norm_g[l].rearrange("(c p) -> p c", p=128)),
                             (gk[:], kv_norm_g[l].rearrange("(c p) -> p c", p=128))], writes=[B_l])
                for c in range(3):
                    k.op("dve", lambda e, c=c: e.tensor_scalar(out=wq_b[:, c, :], in0=wq_f[:, c, :], scalar1=gq[:, c:c + 1],
                                                               scalar2=None, op0=ALU.mult), reads=[B_l], writes=[B_o])
                for c in range(2):
                    src = wk_f[:, c, :].rearrange("p (h t e) -> p h t e", h=8, t=2)
                    k.op("dve", lambda e, c=c, src=src: e.tensor_scalar(
                        out=wkk_b[:, c, :].rearrange("p (h e) -> p h e", h=8), in0=src[:, :, 0, :],
                        scalar1=gk[:, c:c + 1], scalar2=None, op0=ALU.mult), reads=[B_l], writes=[B_o])
                    k.op("dve", lambda e, c=c, src=src: e.tensor_scalar(
                        out=wkv_b[:, c, :].rearrange("p (h e) -> p h e", h=8), in0=src[:, :, 1, :],
                        scalar1=gk[:, c:c + 1], scalar2=None, op0=ALU.mult), reads=[B_l], writes=[B_o])
                k.dma("pool", [(W["wqb_s"][:, :, :], wq_b[:]), (W["wkvk_s"][:, :, :], wkk_b[:]),
                             (W["wkvv_s"][:, :, :], wkv_b[:])], reads=[B_o], writes=[B_w], partial=True)

            early.append(mla_job)
            return early, late

        def phase_proj(l):
            with contextlib.ExitStack() as ph:
                wqb_t = sb(ph, "p1wqb", [128, 3, 768], BF16)
                wkk_t = sb(ph, "p1wkk", [128, 2, 512], BF16)
                wkv_t = sb(ph, "p1wkv", [128, 2, 512], BF16)
                bg_t = sb(ph, "p1bg", [128, 24], F32)
                B_res = k.buf("p1res")
                W = WS[l]
                k.dma("sp", [(wqb_t[:], W["wqb_s"][:, :, :]), (wkk_t[:], W["wkvk_s"][:, :, :]), (wkv_t[:], W["wkvv_s"][:, :, :]),
                             (bg_t[:], b_gate[l].rearrange("(j p) -> p j", p=128))], writes=[B_res])
                xb = [sb(ph, "p1xb%d" % i, [128, 8, TB], BF16) for i in range(2)]
                Bxb = [k.buf("p1xb%d" % i) for i in range(2)]
                tb_t = [sb(ph, "p1tab%d" % i, [128, 6, TB], F32) for i in range(2)]
                Btb = [k.buf("p1tab%d" % i) for i in range(2)]
                NW = 6
                wt = [sb(ph, "p1w%d" % i, [128, 8, 128], BF16) for i in range(NW)]
                Bwt = [k.buf("p1w%d" % i) for i in range(NW)]
                wv = [sb(ph, "p1wv%d" % i, [128, 8, 512], BF16) for i in range(2)]
                Bwv = [k.buf("p1wv%d" % i) for i in range(2)]
                NP = 4
                pp = [ps(ph, "p1ps%d" % i, [128, 512]) for i in range(NP)]
                Bpp = [k.buf("p1ps%d" % i) for i in range(NP)]
                pr = [ps(ph, "p1pr%d" % i, [128, 512]) for i in range(2)]
                Bpr = [k.buf("p1pr%d" % i) for i in range(2)]
                pq = ps(ph, "p1pq", [128, 512]); Bpq = k.buf("p1pq")
                pz = ps(ph, "p1pz", [128, 512]); Bpz = k.buf("p1pz")
                cT = sb(ph, "p1cT", [128, 5, TB], BF16); BcT = k.buf("p1cT")
                sq = [sb(ph, "p1sq%d" % i, [128, TB], F32) for i in range(2)]
                Bsq = [k.buf("p1sq%d" % i) for i in range(2)]
                rstd = sb(ph, "p1rstd", [128, 2, TB], F32); Brstd = k.buf("p1rstd")
                rtm = sb(ph, "p1rtm", [128, 4], F32); Brtm = k.buf("p1rtm")
                NR = 4
                rawf = [sb(ph, "p1rawf%d" % i, [128, TB], F32) for i in range(NR)]
                Brawf = [k.buf("p1rawf%d" % i) for i in range(NR)]
                rawb = [sb(ph, "p1rawb%d" % i, [128, TB], BF16) for i in range(NR)]
                t1 = [sb(ph, "p1t1%d" % i, [128, TB], F32) for i in range(NR)]
                t2 = [sb(ph, "p1t2%d" % i, [128, TB], F32) for i in range(NR)]
                Braw = [k.buf("p1raw%d" % i) for i in range(NR)]
                Bt1 = [k.buf("p1t1%d" % i) for i in range(NR)]
                Bt2 = [k.buf("p1t2%d" % i) for i in range(NR)]
                NS = 6
                stg = [sb(ph, "p1stg%d" % i, [128, TB], BF16) for i in range(NS)]
                Bstg = [k.buf("p1stg%d" % i) for i in range(NS)]
                vstA = sb(ph, "p1vstA", [128, 4, 4, 129], BF16); BvstA = k.buf("p1vstA")
                vstB = sb(ph, "p1vstB", [128, 4, 8, 65], BF16); BvstB = k.buf("p1vstB")
                vstC = sb(ph, "p1vstC", [128, 4, 12, 129], BF16); BvstC = k.buf("p1vstC")
                k.op("pool", lambda e: e.memset(vstA[:], 1.0), writes=[BvstA])
                k.op("pool", lambda e: e.memset(vstB[:], 1.0), writes=[BvstB])
                k.op("pool", lambda e: e.memset(vstC[:], 1.0), writes=[BvstC])
                B_qA = k.buf("qA"); B_kA = k.buf("kA"); B_vA = k.buf("vA"); B_qB = k.buf("qB"); B_kB = k.buf("kB")
                B_vB = k.buf("vB"); B_qC = k.buf("qC"); B_kC = k.buf("kC"); B_vC = k.buf("vC"); B_gT = k.buf("gT")
                cnt = {"w": 0, "p": 0, "r": 0, "s": 0, "wv": 0, "pr": 0, "sq": 0}

                def rope_out(pst, Bps, rows, tabset, tbi, rstd_ap, dst_ap, Bdst):
                    ri = cnt["r"] % NR; cnt["r"] += 1
                    si = cnt["s"] % NS; cnt["s"] += 1
                    pi_ = cnt["pr"] % 2; cnt["pr"] += 1
                    cosT = tb_t[tbi][0:rows, 2 * tabset, :]
                    sinT = tb_t[tbi][0:rows, 2 * tabset + 1, :]
                    if rstd_ap is None:
                        k.op("act", lambda e: e.activation(out=rawb[ri][0:rows, :], in_=pst[0:rows, :], func=AF.Copy),
                             reads=[Bps], writes=[Braw[ri]])
                        k.op("dve", lambda e: e.tensor_tensor(out=t1[ri][0:rows, :], in0=pst[0:rows, :], in1=cosT,
                                                               op=ALU.mult), reads=[Bps, Btb[tbi], Braw[ri]], writes=[Bt1[ri]])
                    else:
                        k.op("dve", lambda e: e.tensor_tensor(out=rawf[ri][0:rows, :], in0=pst[0:rows, :], in1=rstd_ap,
                                                               op=ALU.mult), reads=[Bps, Brstd], writes=[Brawf[ri]])
                        k.op("act", lambda e: e.activation(out=rawb[ri][0:rows, :], in_=rawf[ri][0:rows, :], func=AF.Copy),
                             reads=[Brawf[ri]], writes=[Braw[ri]])
                        k.op("dve", lambda e: e.tensor_tensor(out=t1[ri][0:rows, :], in0=rawf[ri][0:rows, :], in1=cosT,
                                                               op=ALU.mult), reads=[Brawf[ri], Btb[tbi]], writes=[Bt1[ri]])
                    def part2():
                        k.mm_group([lambda e: e.matmul(out=pr[pi_][0:rows, :], lhsT=perm_b[0:rows, tabset, 0:rows],
                                                       rhs=rawb[ri][0:rows, :], start=True, stop=True)],
                                   reads=[Braw[ri]], writes=[Bpr[pi_]])
                        k.op("dve", lambda e: e.tensor_tensor(out=t2[ri][0:rows, :], in0=pr[pi_][0:rows, :], in1=sinT,
                                                               op=ALU.mult), reads=[Bpr[pi_], Btb[tbi]], writes=[Bt2[ri]])
                        k.op("pool", lambda e: e.tensor_tensor(out=stg[si][0:rows, :], in0=t1[ri][0:rows, :],
                                                                in1=t2[ri][0:rows, :], op=ALU.add),
                             reads=[Bt1[ri], Bt2[ri]], writes=[Bstg[si]])
                        k.dma("pool", [(dst_ap, stg[si][0:rows, :])], reads=[Bstg[si]], writes=[Bdst], partial=True)

                    pend.append(part2)

                pend = []

                def flush():
                    while pend:
                        pend.pop(0)()

                for tb in range(NTB):
                    tbi = tb % 2
                    tsl = slice(tb * TB, (tb + 1) * TB)
                    k.dma("sp", [(xb[tbi][:], xT[:, :, tsl])], writes=[Bxb[tbi]])
                    k.dma("sp", [(tb_t[tbi][:], tabs[:, :, tsl].rearrange("a p t -> p a t"))], writes=[Btb[tbi]])
                    for bi, (kind, idx, col0, ncol) in enumerate(FM_BLOCKS):
                        wi = cnt["w"] % NW; cnt["w"] += 1
                        pi = cnt["p"] % NP; cnt["p"] += 1
                        k.dma("sp", [(wt[wi][:], W["w1"][bi])], writes=[Bwt[wi]])
                        k.mm_group([(lambda e, c=c: e.matmul(out=pp[pi][0:ncol, :], lhsT=wt[wi][:, c, 0:ncol],
                                                              rhs=xb[tbi][:, c, :], start=(c == 0), stop=(c == 7)))
                                    for c in range(8)], reads=[Bwt[wi], Bxb[tbi]], writes=[Bpp[pi]])
                        flush()
                        if kind in ("aq", "ak", "cq", "ck"):
                            dst = {"aq": qA, "ak": kA, "cq": qC, "ck": kC}[kind]
                            Bd = {"aq": B_qA, "ak": B_kA, "cq": B_qC, "ck": B_kC}[kind]
                            rope_out(pp[pi], Bpp[pi], 128, 0, tbi, None, dst[idx, :, tsl], Bd)
                        elif kind in ("bcq", "bckv"):
                            ci = idx if kind == "bcq" else 3 + idx
                            k.op("act", lambda e: e.activation(out=cT[:, ci, :], in_=pp[pi][:], func=AF.Copy),
                                 reads=[Bpp[pi]], writes=[BcT])
                            sqi = cnt["sq"] % 2; cnt["sq"] += 1
                            k.op("act", lambda e: e.activation(out=sq[sqi][:], in_=pp[pi][:], func=AF.Square),
                                 reads=[Bpp[pi]], writes=[Bsq[sqi]])
                            first = idx == 0
                            last = (kind == "bcq" and idx == 2) or (kind == "bckv" and idx == 1)
                            k.mm_group([lambda e: e.matmul(out=pz[:], lhsT=ones_f[:], rhs=sq[sqi][:], start=first, stop=last)],
                                       reads=[Bsq[sqi], B_const], writes=[Bpz])
                            if kind == "bckv":
                                k.mm_group([(lambda e, j=j: e.matmul(out=pq[:, 256 + j:256 + j + 1],
                                                                      lhsT=sq[sqi][:, j * 128:(j + 1) * 128], rhs=ones_f[:, 0:1],
                                                                      start=(first and j == 0), stop=last,
                                                                      skip_group_check=True)) for j in range(4)],
                                           reads=[Bsq[sqi], B_const], writes=[Bpq])
                            if last:
                                ri_ = 0 if kind == "bcq" else 1
                                nfe = 384.0 if kind == "bcq" else 256.0
                                k.op("act", lambda e: e.activation(out=rstd[:, ri_, :], in_=pz[:], func=AF.Sqrt,
                                                                    bias=eps6[:, 0:1], scale=1.0 / nfe),
                                     reads=[Bpz], writes=[Brstd])
                                k.op("dve", lambda e: e.reciprocal(out=rstd[:, ri_, :], in_=rstd[:, ri_, :]),
                                     reads=[Brstd], writes=[Brstd])
                                if kind == "bckv":
                                    k.op("act", lambda e: e.activation(out=rtm[:], in_=pq[:, 256:260], func=AF.Sqrt,
                                                                        bias=eps6[:, 0:1], scale=1.0 / nfe),
                                         reads=[Bpq], writes=[Brtm])
                                    k.op("dve", lambda e: e.reciprocal(out=rtm[:], in_=rtm[:]), reads=[Brtm], writes=[Brtm])
                        elif kind == "bkr":
                            for h in range(8):
                                pass
                            ri = cnt["r"] % NR
                            si_peek = cnt["s"] % NS
                            rope_out(pp[pi], Bpp[pi], 32, 2, tbi, None, kB[0, 64:96, tsl], B_kB)
                            pend.append(lambda si_peek=si_peek, tsl=tsl: k.dma(
                                "pool", [(kB[h, 64:96, tsl], stg[si_peek][0:32, :]) for h in range(1, 8)],
                                reads=[Bstg[si_peek]], writes=[B_kB], partial=True))
                        elif kind == "g":
                            si = cnt["s"] % NS; cnt["s"] += 1
                            k.op("act", lambda e: e.activation(out=stg[si][:], in_=pp[pi][:], func=AF.Sigmoid,
                                                                bias=bg_t[:, idx:idx + 1], scale=1.0),
                                 reads=[Bpp[pi], B_res], writes=[Bstg[si]])
                            k.dma("pool", [(gT[idx, :, tsl], stg[si][:])], reads=[Bstg[si]], writes=[B_gT], partial=True)
                        if kind == "bckv" and idx == 1:
                            for h in range(8):
                                pi2 = cnt["p"] % NP; cnt["p"] += 1
                                k.mm_group([(lambda e, c=c: e.matmul(out=pp[pi2][0:96, :], lhsT=wqb_t[:, c, h * 96:(h + 1) * 96],
                                                                      rhs=cT[:, c, :], start=(c == 0), stop=(c == 2)))
                                            for c in range(3)], reads=[BcT, B_res], writes=[Bpp[pi2]])
                                flush()
                                rope_out(pp[pi2], Bpp[pi2], 96, 1, tbi, rstd[0:96, 0, :], qB[h, :, tsl], B_qB)
                            for h in range(8):
                                pi2 = cnt["p"] % NP; cnt["p"] += 1
                                si = cnt["s"] % NS; cnt["s"] += 1
                                k.mm_group([(lambda e, c=c: e.matmul(out=pp[pi2][0:64, :], lhsT=wkk_t[:, c, h * 64:(h + 1) * 64],
                                                                      rhs=cT[:, 3 + c, :], start=(c == 0), stop=(c == 1)))
                                            for c in range(2)], reads=[BcT, B_res], writes=[Bpp[pi2]])
                                k.op("dve", lambda e: e.tensor_tensor(out=stg[si][0:64, :], in0=pp[pi2][0:64, :],
                                                                       in1=rstd[0:64, 1, :], op=ALU.mult),
                                     reads=[Bpp[pi2], Brstd], writes=[Bstg[si]])
                                k.dma("pool", [(kB[h, 0:64, tsl], stg[si][0:64, :])], reads=[Bstg[si]], writes=[B_kB],
                                      partial=True)
                            for j in range(4):
                                pi2 = cnt["p"] % NP; cnt["p"] += 1
                                k.mm_group([(lambda e, c=c: e.matmul(out=pp[pi2][:, :], lhsT=cT[:, 3 + c, j * 128:(j + 1) * 128],
                                                                      rhs=wkv_t[:, c, :], start=(c == 0), stop=(c == 1)))
                                            for c in range(2)], reads=[BcT, B_res], writes=[Bpp[pi2]])
                                k.op("dve", lambda e: e.tensor_scalar(
                                    out=vstB[:, j, :, 0:64], in0=pp[pi2][:].rearrange("p (h e) -> p h e", h=8),
                                    scalar1=rtm[:, j:j + 1], scalar2=None, op0=ALU.mult),
                                     reads=[Bpp[pi2], Brtm], writes=[BvstB])
                            k.dma("pool", [(vB[h, :, tb * 4:(tb + 1) * 4, :], vstB[:, :, h, :]) for h in range(8)],
                                  reads=[BvstB], writes=[B_vB], partial=True)
                    flush()
                    for bi, (kind, idx, col0) in enumerate(TM_BLOCKS):
                        wvi = cnt["wv"] % 2; cnt["wv"] += 1
                        k.dma("sp", [(wv[wvi][:], W["w1v"][bi])], writes=[Bwv[wvi]])
                        for j in range(4):
                            pi2 = cnt["p"] % NP; cnt["p"] += 1
                            k.mm_group([(lambda e, c=c: e.matmul(out=pp[pi2][:, :], lhsT=xb[tbi][:, c, j * 128:(j + 1) * 128],
                                                                  rhs=wv[wvi][:, c, :], start=(c == 0), stop=(c == 7)))
                                        for c in range(8)], reads=[Bwv[wvi], Bxb[tbi]], writes=[Bpp[pi2]])
                            if kind == "av":
                                k.op("act", lambda e: e.activation(out=vstA[:, j, :, 0:128],
                                                                    in_=pp[pi2][:].rearrange("p (h e) -> p h e", h=4),
                                                                    func=AF.Copy), reads=[Bpp[pi2]], writes=[BvstA])
                            else:
                                k.op("act", lambda e: e.activation(out=vstC[:, j, idx * 4:(idx + 1) * 4, 0:128],
                                                                    in_=pp[pi2][:].rearrange("p (h e) -> p h e", h=4),
                                                                    func=AF.Copy), reads=[Bpp[pi2]], writes=[BvstC])
                        if kind == "av":
                            k.dma("pool", [(vA[h, :, tb * 4:(tb + 1) * 4, :], vstA[:, :, h, :]) for h in range(4)],
                                  reads=[BvstA], writes=[B_vA], partial=True)
                        elif idx == 2:
                            k.dma("pool", [(vC[(tb * 4 + j) * 128:(tb * 4 + j + 1) * 128, :],
                                            vstC[:, j, :, :].rearrange("p h e -> p (h e)")) for j in range(4)],
                                  reads=[BvstC], writes=[B_vC], partial=True)
                k.barrier()

        def lam_setup(ph, l, psm, Bpsm):
            lam_init = 0.8 - 0.6 * math.exp(-0.3 * l)
            lv = sb(ph, "lamv", [1, 4, 64], F32)
            lp = sb(ph, "lamp", [1, 2, 64], F32)
            ls = sb(ph, "lams", [1, 4], F32)
            neglam = sb(ph, "neglam", [128, 1], F32)
            gfac = sb(ph, "gfac", [128, 128], F32)
            B_l = k.buf("lam")
            k.dma("sp", [(lv[:], lam_in[l:l + 1, :, :]), (gfac[:], diff_g[l].partition_broadcast(128))], writes=[B_l])
            k.op("dve", lambda e: e.tensor_tensor(out=lp[:, 0, :], in0=lv[:, 0, :], in1=lv[:, 1, :], op=ALU.mult),
                 reads=[B_l], writes=[B_l])
            k.op("dve", lambda e: e.tensor_tensor(out=lp[:, 1, :], in0=lv[:, 2, :], in1=lv[:, 3, :], op=ALU.mult),
                 reads=[B_l], writes=[B_l])
            k.op("dve", lambda e: e.tensor_reduce(out=ls[:, 0:2], in_=lp[:], axis=AX.X, op=ALU.add), reads=[B_l], writes=[B_l])
            k.op("act", lambda e: e.activation(out=ls[:, 0:2], in_=ls[:, 0:2], func=AF.Exp), reads=[B_l], writes=[B_l])
            k.op("dve", lambda e: e.tensor_tensor(out=ls[:, 2:3], in0=ls[:, 1:2], in1=ls[:, 0:1], op=ALU.subtract),
                 reads=[B_l], writes=[B_l])
            k.op("dve", lambda e: e.tensor_scalar(out=ls[:, 3:4], in0=ls[:, 2:3], scalar1=-lam_init, scalar2=None, op0=ALU.add),
                 reads=[B_l], writes=[B_l])
            k.mm_group([lambda e: e.matmul(out=psm[:, 0:1], lhsT=ones_f[0:1, :], rhs=ls[0:1, 3:4], start=True, stop=True)],
                       reads=[B_l, B_const], writes=[Bpsm])
            k.op("dve", lambda e: e.tensor_copy(out=neglam[:], in_=psm[:, 0:1]), reads=[Bpsm], writes=[B_l])
            k.op("dve", lambda e: e.tensor_scalar(out=gfac[:], in0=gfac[:], scalar1=1.0 - lam_init, scalar2=None, op0=ALU.mult),
                 reads=[B_l], writes=[B_l])
            return neglam, gfac, B_l

        def phase_attn_dense(l):
            with contextlib.ExitStack() as ph:
                neglam_gf = {}
                with contextlib.ExitStack() as ph0:
                    pM = ps(ph0, "aM", [128, 512]); BpM = k.buf("aM")
                    neglam, gfac, B_l = lam_setup(ph, l, pM, BpM)
                    k.barrier()
                B_l = k.buf("lamc")
                NSR = 3
                pS = [ps(ph, "aS%d" % i, [128, 1024]) for i in range(NSR)]
                BpS = [k.buf("aS%d" % i) for i in range(NSR)]
                pAcc = ps(ph, "aAcc", [128, 1024]); BpAcc = k.buf("aAcc")
                qk = [(sb(ph, "aq%d" % i, [128, S], BF16), sb(ph, "ak%d" % i, [128, S], BF16),
                       sb(ph, "av%d" % i, [128, NT, 129], BF16)) for i in range(2)]
                Bqk = [k.buf("aqkv%d" % i) for i in range(2)]
                NE = 4
                E = [sb(ph, "aE%d" % i, [128, 1024], BF16) for i in range(NE)]
                BE = [k.buf("aE%d" % i) for i in range(NE)]
                accs = [sb(ph, "aaccs%d" % i, [128, 4, 129], F32) for i in range(2)]
                Baccs = [k.buf("aaccs%d" % i) for i in range(2)]
                rz = [sb(ph, "arz%d" % i, [128, 4], F32) for i in range(2)]
                Brz = [k.buf("arz%d" % i) for i in range(2)]
                oA = [sb(ph, "aoA%d" % i, [128, 4, 128], F32) for i in range(2)]
                BoA = [k.buf("aoA%d" % i) for i in range(2)]
                acomb = sb(ph, "acomb", [128, 4, 128], F32); Bac = k.buf("acomb")
                asq = sb(ph, "asq", [128, 4, 128], F32)
                ass = sb(ph, "ass", [128, 12], F32); Bass_ = k.buf("ass")
                ob = [sb(ph, "aob%d" % i, [128, 4, 128], BF16) for i in range(2)]
                Bob = [k.buf("aob%d" % i) for i in range(2)]
                ostg = [sb(ph, "aostg%d" % i, [128, 512], BF16) for i in range(2)]
                Bostg = [k.buf("aostg%d" % i) for i in range(2)]
                B_oT = k.buf("oT")
                scale_b = 96.0 ** -0.5

                heads = []
                for h in range(4):
                    heads.append(("A", h))
                for h in range(8):
                    heads.append(("B", h))

                def load_head(hi):
                    kind, h = heads[hi]
                    qb, kb, vb = qk[hi % 2]
                    if kind == "A":
                        k.dma("sp", [(qb[:], qA[h]), (kb[:], kA[h]), (vb[:], vA[h])], writes=[Bqk[hi % 2]])
                    else:
                        k.dma("sp", [(qb[0:96, :], qB[h]), (kb[0:96, :], kB[h]), (vb[:, :, 0:65], vB[h])],
                              writes=[Bqk[hi % 2]])

                items = []
                for hi, (kind, h) in enumerate(heads):
                    for qg in range(2 * NTB if kind == "A" else NTB):
                        for kt2 in range(NT // 2):
                            items.append((hi, kind, h, qg, 0, kt2))
                n = len(items)
                LA = 2
                delayed = {}

                def defer(idx, fn):
                    delayed.setdefault(idx, []).append(fn)

                def ops_of(it):
                    hi, kind, h, qg, m, kt2 = it
                    qb, kb, vb = qk[hi % 2]
                    if kind == "A":
                        return (qb[m * 64:(m + 1) * 64, :], kb[m * 64:(m + 1) * 64, :], vb, 129, 0.125, 256)
                    return (qb[0:96, :], kb[0:96, :], vb, 65, scale_b, 128)

                def emit_qk(i):
                    hi, kind, h, qg, m, kt2 = items[i]
                    if qg == 0 and m == 0 and kt2 == 0 and hi == 0:
                        load_head(0)
                    qT, kT, vt, dv1, scale, accw = ops_of(items[i])
                    si = i % NSR
                    ei = i % NE
                    Bin = Bqk[hi % 2]
                    if kind == "A":
                        qb, kb, vb = qk[hi % 2]
                        k.mm_group([(lambda e, j=j, m=m: e.matmul(
                            out=pS[si][:, m * 512 + j * 256:m * 512 + (j + 1) * 256],
                            lhsT=kb[m * 64:(m + 1) * 64, (kt2 * 2 + j) * 128:(kt2 * 2 + j + 1) * 128],
                            rhs=qb[m * 64:(m + 1) * 64, qg * 256:(qg + 1) * 256], start=True, stop=True))
                                    for j in range(2) for m in range(2)], reads=[Bin], writes=[BpS[si]])
                    else:
                        k.mm_group([(lambda e, j=j: e.matmul(out=pS[si][:, j * 512:(j + 1) * 512],
                                                              lhsT=kT[:, (kt2 * 2 + j) * 128:(kt2 * 2 + j + 1) * 128],
                                                              rhs=qT[:, qg * 512:(qg + 1) * 512], start=True, stop=True))
                                    for j in range(2)], reads=[Bin], writes=[BpS[si]])
                    k.op("act", lambda e: e.activation(out=E[ei][:], in_=pS[si][:], func=AF.Exp, scale=scale),
                         reads=[BpS[si]], writes=[BE[ei]])

                def emit_pv(i):
                    hi, kind, h, qg, m, kt2 = items[i]
                    if qg == 0 and m == 0 and kt2 == 0 and hi + 1 < len(heads):
                        load_head(hi + 1)
                    qT, kT, vt, dv1, scale, accw = ops_of(items[i])
                    ei = i % NE
                    Bin = Bqk[hi % 2]
                    fns = []
                    if kind == "A":
                        for j in range(2):
                            kt = kt2 * 2 + j
                            for m in range(2):
                                for js in range(2):
                                    a_ = m * 2 + js
                                    fns.append(lambda e, j=j, m=m, js=js, kt=kt, a_=a_: e.matmul(
                                        out=pAcc[:, a_ * 256:a_ * 256 + 129],
                                        lhsT=E[ei][:, m * 512 + j * 256 + js * 128:m * 512 + j * 256 + (js + 1) * 128],
                                        rhs=vt[:, kt, 0:129], start=(kt == 0 and a_ % 2 == 0), stop=(kt == NT - 1),
                                        skip_group_check=True))
                    for j in range(2 if kind == "B" else 0):
                        kt = kt2 * 2 + j
                        for js in range(4):
                            fns.append(lambda e, j=j, js=js, kt=kt: e.matmul(
                                out=pAcc[:, js * accw:js * accw + dv1],
                                lhsT=E[ei][:, j * 512 + js * 128:j * 512 + (js + 1) * 128],
                                rhs=vt[:, kt, 0:dv1], start=(kt == 0 and (js * accw) % 512 == 0), stop=(kt == NT - 1),
                                skip_group_check=True))
                    k.mm_group(fns, reads=[BE[ei], Bin], writes=[BpAcc])
                    if kt2 == NT // 2 - 1:
                        post(i)

                gcount = [0]

                def post(i):
                    hi, kind, h, qg, m, kt2 = items[i]
                    g = gcount[0]; gcount[0] += 1
                    ai = g % 2
                    if kind == "A":
                        oi = g % 2
                        k.op("dve", lambda e: e.tensor_copy(
                            out=accs[ai][:], in_=pAcc[:].rearrange("p (j w) -> p j w", w=256)[:, :, 0:129]),
                             reads=[BpAcc], writes=[Baccs[ai]])
                        k.op("dve", lambda e: e.reciprocal(out=rz[ai][:], in_=accs[ai][:, :, 128]),
                             reads=[Baccs[ai]], writes=[Brz[ai]])
                        k.op("pool", lambda e: e.tensor_tensor(
                            out=oA[0][:], in0=accs[ai][:, :, 0:128],
                            in1=rz[ai][:].unsqueeze(2).to_broadcast([128, 4, 128]), op=ALU.mult),
                             reads=[Baccs[ai], Brz[ai]], writes=[BoA[0]])

                        def p1():
                            k.op("dve", lambda e: e.scalar_tensor_tensor(out=acomb[:, 0:2, :], in0=oA[0][:, 2:4, :],
                                                                          scalar=neglam[:, 0:1], in1=oA[0][:, 0:2, :],
                                                                          op0=ALU.mult, op1=ALU.add),
                                 reads=[BoA[0]], writes=[Bac])
                            k.op("pool", lambda e: e.tensor_tensor(out=asq[:, 0:2, :], in0=acomb[:, 0:2, :],
                                                                    in1=acomb[:, 0:2, :], op=ALU.mult),
                                 reads=[Bac], writes=[Bass_])
                            k.op("dve", lambda e: e.tensor_reduce(out=ass[:, 0:2], in_=asq[:, 0:2, :], axis=AX.X, op=ALU.add),
                                 reads=[Bass_], writes=[Bass_])

                        def p2():
                            k.op("act", lambda e: e.activation(out=ass[:, 4:6], in_=ass[:, 0:2], func=AF.Ln,
                                                                bias=eps6[:, 0:1], scale=1.0 / 128.0),
                                 reads=[Bass_], writes=[Bass_])

                        def p3():
                            k.op("act", lambda e: e.activation(out=ass[:, 8:10], in_=ass[:, 4:6], func=AF.Exp, scale=-0.5),
                                 reads=[Bass_], writes=[Bass_])

                        def p4():
                            for js in range(2):
                                k.op("dve", lambda e, js=js: e.scalar_tensor_tensor(
                                    out=ob[oi][:, js, :], in0=acomb[:, js, :], scalar=ass[:, 8 + js:9 + js], in1=gfac[:],
                                    op0=ALU.mult, op1=ALU.mult), reads=[Bac, Bass_], writes=[Bob[oi]])

                        def p5():
                            si = (i + 8 + LA - 1) % NSR
                            pTrb = pS[si][:, 0:512].bitcast(BF16)
                            k.mm_group([(lambda e, js=js: e.transpose(out=pTrb[:, js * 128:(js + 1) * 128],
                                                                       in_=ob[oi][:, js, :], identity=ident_b[:]))
                                        for js in range(2)], reads=[Bob[oi], B_const], writes=[BpS[si]])
                            k.op("dve", lambda e: e.tensor_copy(out=ostg[oi][:, 0:256], in_=pTrb[:, 0:256]), reads=[BpS[si]],
                                 writes=[Bostg[oi]])
                            k.dma("pool", [(oT[h, :, qg * 256:(qg + 1) * 256], ostg[oi][:, 0:256])], reads=[Bostg[oi]],
                                  writes=[B_oT], partial=True)

                        defer(i + 1, p1); defer(i + 3, p2); defer(i + 4, p3); defer(i + 6, p4); defer(i + 8, p5)
                    else:
                        oi = g % 2
                        k.op("dve", lambda e: e.tensor_copy(
                            out=accs[ai][:, :, 0:65], in_=pAcc[:, 0:512].rearrange("p (j w) -> p j w", w=128)[:, :, 0:65]),
                             reads=[BpAcc], writes=[Baccs[ai]])
                        k.op("dve", lambda e: e.reciprocal(out=rz[ai][:], in_=accs[ai][:, :, 64]),
                             reads=[Baccs[ai]], writes=[Brz[ai]])
                        k.op("pool", lambda e: e.tensor_tensor(
                            out=ob[oi][:, :, 0:64], in0=accs[ai][:, :, 0:64],
                            in1=rz[ai][:].unsqueeze(2).to_broadcast([128, 4, 64]), op=ALU.mult),
                             reads=[Baccs[ai], Brz[ai]], writes=[Bob[oi]])

                        def p5():
                            si = (i + 4 + LA - 1) % NSR
                            pTrb = pS[si][:, 0:512].bitcast(BF16)
                            k.mm_group([(lambda e, js=js: e.transpose(out=pTrb[0:64, js * 128:(js + 1) * 128],
                                                                       in_=ob[oi][:, js, 0:64], identity=ident_b[:]))
                                        for js in range(4)], reads=[Bob[oi], B_const], writes=[BpS[si]])
                            k.op("dve", lambda e: e.tensor_copy(out=ostg[oi][0:64, :], in_=pTrb[0:64, 0:512]),
                                 reads=[BpS[si]], writes=[Bostg[oi]])
                            k.dma("pool", [(oT[4 + h // 2, (h % 2) * 64:(h % 2) * 64 + 64, qg * 512:(qg + 1) * 512],
                                            ostg[oi][0:64, :])], reads=[Bostg[oi]], writes=[B_oT], partial=True)

                        defer(i + 4, p5)

                wjobs = list(late_jobs.pop(l, []))
                if l + 1 < depth:
                    e1, l1 = wprep_jobs(l + 1, ph)
                    wjobs += e1 + l1
                for idx in range(n + LA + 10):
                    j = idx - LA
                    if wjobs and idx % 40 == 20:
                        wjobs.pop(0)()
                    if idx < n:
                        emit_qk(idx)
                    if 0 <= j < n:
                        emit_pv(j)
                    if j in delayed:
                        for fn in delayed.pop(j):
                            fn()
                assert not delayed
                while wjobs:
                    wjobs.pop(0)()
                k.barrier()

        def phase_attn_dil(l):
            with contextlib.ExitStack() as ph:
                NSB = 3
                pS = [ps(ph, "cS%d" % i, [128, 512]) for i in range(NSB)]
                BpS = [k.buf("cS%d" % i) for i in range(NSB)]
                pAcc = [ps(ph, "cAcc%d" % i, [128, 1024]) for i in range(2)]
                BpAcc = [k.buf("cAcc%d" % i) for i in range(2)]
                pTr = ps(ph, "cTr", [128, 512]); BpTr = k.buf("cTr")
                pTrb = pTr[:].bitcast(BF16)
                maskf = sb(ph, "cmaskf", [128, len(C_MASKS), 128], F32)
                maskb = sb(ph, "cmaskb", [128, len(C_MASKS), 128], BF16)
                B_m = k.buf("cmask")
                k.dma("sp", [(maskf[:], c_maskb[:, :, :])], writes=[B_m])
                k.op("dve", lambda e: e.tensor_copy(out=maskb[:], in_=maskf[:]), reads=[B_m], writes=[B_m])
                kr = [sb(ph, "ckr%d" % i, [128, 6, 128], BF16) for i in range(RING)]
                vr = [sb(ph, "cvr%d" % i, [128, 12, 129], BF16) for i in range(RING)]
                Bkv = [k.buf("ckv%d" % i) for i in range(RING)]
                NQ = 3
                qr = [sb(ph, "cqr%d" % i, [128, 6, 128], BF16) for i in range(NQ)]
                Bq = [k.buf("cq%d" % i) for i in range(NQ)]
                NE = 4
                E = [sb(ph, "cE%d" % i, [128, 512], BF16) for i in range(NE)]
                BE = [k.buf("cE%d" % i) for i in range(NE)]
                rz = [sb(ph, "crz%d" % i, [128, 4], F32) for i in range(2)]
                Brz = [k.buf("crz%d" % i) for i in range(2)]
                ob = [sb(ph, "cob%d" % i, [128, 4, 128], BF16) for i in range(2)]
                Bob = [k.buf("cob%d" % i) for i in range(2)]
                ostg = [sb(ph, "costg%d" % i, [128, 4, 512], BF16) for i in range(2)]
                Bostg = [k.buf("costg%d" % i) for i in range(2)]
                B_oT = k.buf("oTc")

                def load_kv(kt):
                    sl = kt % RING
                    k.dma("sp", [(kr[sl][:], kC[:, :, kt * 128:(kt + 1) * 128].rearrange("j p t -> p j t")),
                                 (vr[sl][:].rearrange("p h e -> p (h e)"), vC[kt * 128:(kt + 1) * 128, :])],
                          writes=[Bkv[sl]])

                items = []
                for qt in range(NT):
                    for c in range(4):
                        tiles = []
                        for g in range(3):
                            for dl in range(-C_DELTA[g], C_DELTA[g] + 1):
                                kt = qt + dl
                                if 0 <= kt < NT:
                                    tiles.append((g, dl, kt))
                        ntile = len(tiles)
                        for b0 in range(0, ntile, 4):
                            items.append((qt, c, b0, tiles[b0:b0 + 4], ntile, b0 + 4 >= ntile))
                n = len(items)
                LA = 2
                delayed = {}

                def emit_qk(i):
                    qt, c, b0, grp, ntile, lastg = items[i]
                    if c == 0 and b0 == 0:
                        if qt == 0:
                            for kt in range(0, 9):
                                load_kv(kt)
                        elif qt + 8 < NT:
                            load_kv(qt + 8)
                        k.dma("sp", [(qr[qt % NQ][:], qC[:, :, qt * 128:(qt + 1) * 128].rearrange("j p t -> p j t"))],
                              writes=[Bq[qt % NQ]])
                    qi = qt % NQ
                    si = i % NSB
                    ei = i % NE
                    fns = []
                    rd = [Bq[qi], B_m, B_const]
                    for s_, (g, dl, kt) in enumerate(grp):
                        f = g * 4 + c
                        j, hf = f // 2, f % 2
                        mi = C_MASKS.index((g, dl))
                        sl = kt % RING
                        rd.append(Bkv[sl])
                        fns.append(lambda e, s_=s_, j=j, hf=hf, sl=sl: e.matmul(
                            out=pS[si][:, s_ * 128:(s_ + 1) * 128], lhsT=kr[sl][hf * 64:(hf + 1) * 64, j, :],
                            rhs=qr[qi][hf * 64:(hf + 1) * 64, j, :], start=True, stop=True))
                    k.mm_group(fns, reads=rd, writes=[BpS[si]])
                    nn = len(grp)
                    k.op("act", lambda e: e.activation(out=E[ei][:, 0:nn * 128], in_=pS[si][:, 0:nn * 128], func=AF.Exp,
                                                        scale=0.125), reads=[BpS[si]], writes=[BE[ei]])
                    mis = [C_MASKS.index((g, dl)) for (g, dl, kt) in grp]
                    r0 = 0
                    while r0 < nn:
                        r1 = r0 + 1
                        while r1 < nn and mis[r1] == mis[r1 - 1] + 1:
                            r1 += 1
                        meng = "dve" if (i % 2 == 0) else "pool"
                        k.op(meng, lambda e, r0=r0, r1=r1: e.tensor_tensor(
                            out=E[ei][:, r0 * 128:r1 * 128], in0=E[ei][:, r0 * 128:r1 * 128],
                            in1=maskb[:, mis[r0]:mis[r0] + (r1 - r0), :].rearrange("p m t -> p (m t)"), op=ALU.mult),
                             reads=[BE[ei], B_m], writes=[BE[ei]])
                        r0 = r1

                def emit_pv(i):
                    qt, c, b0, grp, ntile, lastg = items[i]
                    ei = i % NE
                    ai = qt % 2
                    fns = []
                    rd = [BE[ei]]
                    for s_, (g, dl, kt) in enumerate(grp):
                        f = g * 4 + c
                        sl = kt % RING
                        rd.append(Bkv[sl])
                        gi = b0 + s_
                        fns.append(lambda e, s_=s_, f=f, sl=sl, gi=gi: e.matmul(
                            out=pAcc[ai][:, c * 256:c * 256 + 129], lhsT=E[ei][:, s_ * 128:(s_ + 1) * 128],
                            rhs=vr[sl][:, f, :], start=(gi == 0), stop=(gi == ntile - 1)))
                    k.mm_group(fns, reads=rd, writes=[BpAcc[ai]])
                    if c == 3 and lastg:
                        oi = qt % 2
                        k.op("dve", lambda e: e.reciprocal(
                            out=rz[oi][:], in_=pAcc[ai][:].rearrange("p (j w) -> p j w", w=256)[:, :, 128]),
                             reads=[BpAcc[ai]], writes=[Brz[oi]])
                        for cc in range(4):
                            k.op("dve", lambda e, cc=cc: e.tensor_scalar(
                                out=ob[oi][:, cc, :], in0=pAcc[ai][:, cc * 256:cc * 256 + 128], scalar1=rz[oi][:, cc:cc + 1],
                                scalar2=None, op0=ALU.mult), reads=[BpAcc[ai], Brz[oi]], writes=[Bob[oi]])

                        def p5():
                            k.mm_group([(lambda e, cc=cc: e.transpose(out=pTrb[:, cc * 128:(cc + 1) * 128], in_=ob[oi][:, cc, :],
                                                                       identity=ident_b[:])) for cc in range(4)],
                                       reads=[Bob[oi], B_const], writes=[BpTr])
                            gi_ = (qt // 4) % 2
                            k.op("dve", lambda e: e.tensor_copy(
                                out=ostg[gi_][:, :, (qt % 4) * 128:(qt % 4 + 1) * 128],
                                in_=pTrb[:, 0:512].rearrange("p (c t) -> p c t", c=4)), reads=[BpTr], writes=[Bostg[gi_]])
                            if qt % 4 == 3:
                                qg = qt // 4
                                k.dma("pool", [(oT[8 + cc, :, qg * 512:(qg + 1) * 512], ostg[gi_][:, cc, :]) for cc in range(4)],
                                      reads=[Bostg[gi_]], writes=[B_oT], partial=True)

                        delayed.setdefault(i + 6, []).append(p5)

                for idx in range(n + LA + 8):
                    j = idx - LA
                    if idx < n:
                        emit_qk(idx)
                    if 0 <= j < n:
                        emit_pv(j)
                    if j in delayed:
                        for fn in delayed.pop(j):
                            fn()
                assert not delayed
                k.barrier()

        def phase_merge(l):
            with contextlib.ExitStack() as ph:
                st = ln_setup(ph, "l1")
                gam, bet, B_gb = load_gb(ph, "l1", ln1_g[l], ln1_b[l])
                wbr_t = sb(ph, "m_wbr", [128, 12, D], BF16)
                wout_t = sb(ph, "m_wout", [128, 8, D], BF16)
                rw_t = sb(ph, "m_rw", [128, 8, 16], F32)
                B_w = k.buf("m_w")
                W = WS[l]
                k.dma("sp", [(wbr_t[:], W["wbr_s"][:, :, :]), (wout_t[:], W["wout_s"][:, :, :]),
                             (rw_t[:], router_w.rearrange("(c p) e -> p c e", p=128))], writes=[B_w])
                ot = [sb(ph, "m_ot%d" % i, [128, 12, TB], BF16) for i in range(2)]
                gt = [sb(ph, "m_gt%d" % i, [128, 24, TB], BF16) for i in range(2)]
                Bin = [k.buf("m_in%d" % i) for i in range(2)]
                NPB = 3
                pb = [ps(ph, "m_p%d" % i, [128, 512]) for i in range(NPB)]
                Bpb = [k.buf("m_p%d" % i) for i in range(NPB)]
                NPM = 3
                pmx = [ps(ph, "m_mix%d" % i, [128, 512]) for i in range(NPM)]
                Bpmx = [k.buf("m_mix%d" % i) for i in range(NPM)]
                psT = ps(ph, "m_psT", [128, 512]); B_psT = k.buf("m_psT")
                prt = ps(ph, "m_prt", [128, 512]); Bprt = k.buf("m_prt")
                mm_ = [[sb(ph, "m_m%d_%d" % (s_, i), [128, TB], F32) for i in range(3)] for s_ in range(2)]
                Bmm = [[k.buf("m_m%d_%d" % (s_, i)) for i in range(3)] for s_ in range(2)]
                yT = [sb(ph, "m_yT%d" % i, [128, 8, TB], BF16) for i in range(2)]
                ByT = [k.buf("m_yT%d" % i) for i in range(2)]
                NRT = 3
                rt = [sb(ph, "m_r%d" % i, [128, D], F32) for i in range(NRT)]
                Brt = [k.buf("m_r%d" % i) for i in range(NRT)]
                xs = [sb(ph, "m_xs%d" % i, [128, 8, 512], BF16) for i in range(2)]
                Bxs = [k.buf("m_xs%d" % i) for i in range(2)]
                xf = [sb(ph, "m_xf%d" % i, [128, 8, 128], F32) for i in range(2)]
                B_xf = [k.buf("m_xf%d" % i) for i in range(2)]
                router = {"xf": xf, "B_xf": B_xf, "ps": prt, "B_ps": Bprt, "w": rw_t}
                B_x1T = k.buf("x1T")
                cnt = {"p": 0, "m": 0}

                def load_in(tb):
                    bi = tb % 2
                    tsl = slice(tb * TB, (tb + 1) * TB)
                    k.dma("sp", [(ot[bi][:], oT[:, :, tsl].rearrange("j p t -> p j t")),
                                 (gt[bi][:], gT[:, :, tsl].rearrange("j p t -> p j t"))], writes=[Bin[bi]])

                def stage_a(tb, dc):
                    bi = tb % 2
                    ms = dc % 2
                    for br in range(3):
                        pi = cnt["p"] % NPB; cnt["p"] += 1
                        k.mm_group([(lambda e, c=c: e.matmul(out=pb[pi][:], lhsT=wbr_t[:, br * 4 + c, dc * 128:(dc + 1) * 128],
                                                              rhs=ot[bi][:, br * 4 + c, :], start=(c == 0), stop=(c == 3)))
                                    for c in range(4)], reads=[B_w, Bin[bi]], writes=[Bpb[pi]])
                        k.op("dve", lambda e, br=br, pi=pi: e.tensor_tensor(
                            out=mm_[ms][br][:], in0=pb[pi][:], in1=gt[bi][:, br * 8 + dc, :], op=ALU.mult),
                             reads=[Bpb[pi], Bin[bi]], writes=[Bmm[ms][br]])
                    k.op("pool", lambda e: e.tensor_tensor(out=mm_[ms][0][:], in0=mm_[ms][0][:], in1=mm_[ms][1][:], op=ALU.add),
                         reads=[Bmm[ms][0], Bmm[ms][1]], writes=[Bmm[ms][0]])
                    k.op("pool", lambda e: e.tensor_tensor(out=yT[bi][:, dc, :], in0=mm_[ms][0][:], in1=mm_[ms][2][:],
                                                            op=ALU.add), reads=[Bmm[ms][0], Bmm[ms][2]], writes=[ByT[bi]])

                def stage_b1(tb, j):
                    bi = tb % 2
                    t = tb * 4 + j
                    ri = t % NRT
                    k.dma("sp", [(rt[ri][:], xres[t * 128:(t + 1) * 128, :])], writes=[Brt[ri]])
                    for hf in range(2):
                        pm = cnt["m"] % NPM; cnt["m"] += 1
                        k.mm_group([(lambda e, dc=dc: e.matmul(out=pmx[pm][:], lhsT=yT[bi][:, dc, j * 128:(j + 1) * 128],
                                                                rhs=wout_t[:, dc, hf * 512:(hf + 1) * 512],
                                                                start=(dc == 0), stop=(dc == 7))) for dc in range(8)],
                                   reads=[ByT[bi], B_w], writes=[Bpmx[pm]])
                        k.op("dve", lambda e, hf=hf, pm=pm: e.scalar_tensor_tensor(
                            out=rt[ri][:, hf * 512:(hf + 1) * 512], in0=rt[ri][:, hf * 512:(hf + 1) * 512], scalar=ALPHA,
                            in1=pmx[pm][:], op0=ALU.mult, op1=ALU.add), reads=[Brt[ri], Bpmx[pm]], writes=[Brt[ri]])
                    sidx = tb % 2
                    return finish_tile(st, "l1", Brt[ri], rt[ri], gam, bet, B_gb, t, x1res, xs[sidx], Bxs[sidx], psT, B_psT,
                                       router=router, defer_tr=True)

                load_in(0)
                for dc in range(8):
                    stage_a(0, dc)
                for tb in range(NTB):
                    nxt = tb + 1 < NTB
                    if nxt:
                        load_in(tb + 1)
                    trs = []
                    for j in range(4):
                        trs.append(stage_b1(tb, j))
                        if nxt:
                            stage_a(tb + 1, 2 * j)
                            stage_a(tb + 1, 2 * j + 1)
                        if j >= 1:
                            trs[j - 1]()
                    trs[3]()
                    tsl = slice(tb * TB, (tb + 1) * TB)
                    k.dma("pool", [(x1T[:, :, tsl], xs[tb % 2][:])], reads=[Bxs[tb % 2]], writes=[B_x1T], partial=True)
                k.barrier()

        def phase_moe(l, last):
            with contextlib.ExitStack() as ph:
                with contextlib.ExitStack() as rs:
                    NN = NT * 16
                    bias_t = sb(rs, "r_bias", [128, NN], F32)
                    biased = sb(rs, "r_biased", [128, NN], F32)
                    p6 = sb(rs, "r_p6", [128, NT * 4, 6], F32)
                    gs = sb(rs, "r_gs", [128, NT, 4], F32)
                    gmax = sb(rs, "r_gmax", [128, NT], F32)
                    gmask = sb(rs, "r_gmask", [128, NT, 4], F32)
                    emask = sb(rs, "r_emask", [128, NN], F32)
                    tneg = sb(rs, "r_tneg", [128, NN], F32)
                    masked = sb(rs, "r_masked", [128, NN], F32)
                    m1 = sb(rs, "r_m1", [128, NT], F32)
                    sel1 = sb(rs, "r_sel1", [128, NN], F32)
                    sel2 = sb(rs, "r_sel2", [128, NN], F32)
                    gate = sb(rs, "r_gate", [128, NT, 16], F32)
                    gts = sb(rs, "r_gts", [16, 512], F32)
                    pg = ps(rs, "r_pg", [128, 512]); Bpg = k.buf("r_pg")
                    B_r = k.buf("r")
                    B_gs = k.buf("r_gts"); B_gateT = k.buf("gateT")
                    k.dma("sp", [(bias_t[:], router_bias.partition_broadcast(128))], writes=[B_r])
                    sc = scores_all[:].rearrange("p t e -> p (t e)")
                    v3 = lambda a: a[:].rearrange("p (t e) -> p t e", e=16)
                    v4 = lambda a: a[:].rearrange("p (g e) -> p g e", e=4)
                    R = [B_r]

                    def dv(fn, extra=()):
                        k.op("dve", fn, reads=[B_r] + list(extra), writes=[B_r])

                    dv(lambda e: e.tensor_tensor(out=biased[:], in0=sc, in1=bias_t[:], op=ALU.add), [B_scores])
                    b4 = v4(biased)
                    pairs = [(0, 1), (0, 2), (0, 3), (1, 2), (1, 3), (2, 3)]
                    for pi_, (a_, b_) in enumerate(pairs):
                        dv(lambda e, pi_=pi_, a_=a_, b_=b_: e.tensor_tensor(out=p6[:, :, pi_], in0=b4[:, :, a_],
                                                                           in1=b4[:, :, b_], op=ALU.add))
                    dv(lambda e: e.tensor_reduce(out=gs[:].rearrange("p t g -> p (t g)"), in_=p6[:], axis=AX.X, op=ALU.max))
                    dv(lambda e: e.tensor_reduce(out=gmax[:], in_=gs[:], axis=AX.X, op=ALU.max))
                    dv(lambda e: e.tensor_tensor(out=gmask[:], in0=gs[:], in1=gmax[:].unsqueeze(2).to_broadcast([128, NT, 4]),
                                                 op=ALU.is_equal))
                    dv(lambda e: e.tensor_copy(out=v4(emask), in_=gmask[:].rearrange("p t g -> p (t g)").unsqueeze(2)
                                               .to_broadcast([128, NT * 4, 4])))
                    dv(lambda e: e.tensor_scalar(out=tneg[:], in0=emask[:], scalar1=1e9, scalar2=-1e9, op0=ALU.mult,
                                                 op1=ALU.add))
                    dv(lambda e: e.tensor_tensor(out=masked[:], in0=biased[:], in1=emask[:], op=ALU.mult))
                    dv(lambda e: e.tensor_tensor(out=masked[:], in0=masked[:], in1=tneg[:], op=ALU.add))
                    dv(lambda e: e.tensor_reduce(out=m1[:], in_=v3(masked), axis=AX.X, op=ALU.max))
                    dv(lambda e: e.tensor_tensor(out=v3(sel1), in0=v3(masked), in1=m1[:].unsqueeze(2).to_broadcast([128, NT, 16]),
                                                 op=ALU.is_equal))
                    dv(lambda e: e.scalar_tensor_tensor(out=masked[:], in0=sel1[:], scalar=-1e9, in1=masked[:], op0=ALU.mult,
                                                        op1=ALU.add))
                    dv(lambda e: e.tensor_reduce(out=m1[:], in_=v3(masked), axis=AX.X, op=ALU.max))
                    dv(lambda e: e.tensor_tensor(out=v3(sel2), in0=v3(masked), in1=m1[:].unsqueeze(2).to_broadcast([128, NT, 16]),
                                                 op=ALU.is_equal))
                    dv(lambda e: e.tensor_tensor(out=sel1[:], in0=sel1[:], in1=sel2[:], op=ALU.add))
                    dv(lambda e: e.tensor_tensor(out=sel1[:], in0=sel1[:], in1=sc, op=ALU.mult), [B_scores])
                    dv(lambda e: e.tensor_reduce(out=m1[:], in_=v3(sel1), axis=AX.X, op=ALU.add))
                    dv(lambda e: e.reciprocal(out=m1[:], in_=m1[:]))
                    dv(lambda e: e.tensor_tensor(out=gate[:], in0=v3(sel1), in1=m1[:].unsqueeze(2).to_broadcast([128, NT, 16]),
                                                 op=ALU.mult))
                    for t4 in range(NT // 4):
                        k.mm_group([(lambda e, j=j: e.transpose(out=pg[0:16, j * 128:(j + 1) * 128], in_=gate[:, t4 * 4 + j, :],
                                                                 identity=ident_f[:])) for j in range(4)],
                                   reads=[B_r, B_const], writes=[Bpg])
                        k.op("dve", lambda e: e.tensor_copy(out=gts[:], in_=pg[0:16, :]), reads=[Bpg], writes=[B_gs])
                        k.dma("sp", [(gateT[:, t4 * 512:(t4 + 1) * 512], gts[:])], reads=[B_gs], writes=[B_gateT], partial=True)
                    k.barrier()
                st = ln_setup(ph, "l2")
                gam, bet, B_gb = load_gb(ph, "l2", ln2_g[l], ln2_b[l])
                wd_t = sb(ph, "e_wd", [128, 34, D], BF16)
                selc = sb(ph, "e_selc", [16, 16, 128], F32)
                B_w = k.buf("e_w")
                W = WS[l]
                k.dma("sp", [(wd_t[:], W["wed_s"][:, :, :]), (selc[:], c_selc[:, :, :])], writes=[B_w])
                xb = [sb(ph, "e_xb%d" % i, [128, 8, TB], BF16) for i in range(2)]
                gtb = [sb(ph, "e_gtb%d" % i, [16, TB], F32) for i in range(2)]
                Bxb = [k.buf("e_xb%d" % i) for i in range(2)]
                NW = 3
                wg = [sb(ph, "e_wg%d" % i, [128, 8, 256], BF16) for i in range(NW)]
                wu = [sb(ph, "e_wu%d" % i, [128, 8, 256], BF16) for i in range(NW)]
                Bwgu = [k.buf("e_wgu%d" % i) for i in range(NW)]
                pgu = [ps(ph, "e_pgu%d" % i, [128, 1024]) for i in range(2)]
                Bpgu = [k.buf("e_pgu%d" % i) for i in range(2)]
                pgb = ps(ph, "e_pgb", [128, 512]); Bpgb = k.buf("e_pgb")
                pdn = ps(ph, "e_pdn", [128, 1024]); Bpdn = k.buf("e_pdn")
                psT = ps(ph, "e_psT", [128, 512]); B_psT = k.buf("e_psT")
                gbc = [sb(ph, "e_gbc%d" % i, [128, TB], F32) for i in range(2)]
                Bgbc = [k.buf("e_gbc%d" % i) for i in range(2)]
                sg = [sb(ph, "e_sg%d" % i, [128, TB], F32) for i in range(2)]
                Bsg = [k.buf("e_sg%d" % i) for i in range(2)]
                h1 = [sb(ph, "e_h1%d" % i, [128, TB], F32) for i in range(2)]
                Bh1 = [k.buf("e_h1%d" % i) for i in range(2)]
                hg = sb(ph, "e_hg", [128, 34, TB], BF16); Bhg = k.buf("e_hg")
                xr = [sb(ph, "e_x%d" % i, [128, D], F32) for i in range(2)]
                Bxr = [k.buf("e_x%d" % i) for i in range(2)]
                rt, Brt = xr, Bxr
                xs = [sb(ph, "e_xs%d" % i, [128, 8, 512], BF16) for i in range(2)]
                Bxs = [k.buf("e_xs%d" % i) for i in range(2)]
                B_xT = k.buf("xTn")
                wc = 0; pc = 0; hc_ = 0
                for tb in range(NTB):
                    bi = tb % 2
                    tsl = slice(tb * TB, (tb + 1) * TB)
                    k.dma("sp", [(xb[bi][:], x1T[:, :, tsl]), (gtb[bi][:], gateT[:, tsl])], writes=[Bxb[bi]])
                    for e_ in range(17):
                        wi = wc % NW; wc += 1
                        k.dma("sp", [(wg[wi][:], W["weg_s"][e_]), (wu[wi][:], W["weu_s"][e_])], writes=[Bwgu[wi]])
                        gi = e_ % 2
                        if e_ < 16:
                            k.mm_group([lambda e: e.matmul(out=pgb[:], lhsT=selc[:, e_, :], rhs=gtb[bi][:], start=True, stop=True)],
                                       reads=[B_w, Bxb[bi]], writes=[Bpgb])
                            k.op("act", lambda e: e.activation(out=gbc[gi][:], in_=pgb[:], func=AF.Copy), reads=[Bpgb],
                                 writes=[Bgbc[gi]])
                        for hf in range(2):
                            pi = pc % 2; pc += 1
                            hi = hc_ % 2; hc_ += 1
                            fns = []
                            for c in range(8):
                                fns.append(lambda e, c=c: e.matmul(out=pgu[pi][:, 0:512], lhsT=wg[wi][:, c, hf * 128:(hf + 1) * 128],
                                                                   rhs=xb[bi][:, c, :], start=(c == 0), stop=(c == 7)))
                            for c in range(8):
                                fns.append(lambda e, c=c: e.matmul(out=pgu[pi][:, 512:1024],
                                                                   lhsT=wu[wi][:, c, hf * 128:(hf + 1) * 128],
                                                                   rhs=xb[bi][:, c, :], start=(c == 0), stop=(c == 7)))
                            k.mm_group(fns, reads=[Bwgu[wi], Bxb[bi]], writes=[Bpgu[pi]])
                            k.op("act", lambda e: e.activation(out=sg[hi][:], in_=pgu[pi][:, 0:512], func=AF.Silu),
                                 reads=[Bpgu[pi]], writes=[Bsg[hi]])
                            ch = e_ * 2 + hf
                            if e_ < 16:
                                k.op("dve", lambda e: e.tensor_tensor(out=h1[hi][:], in0=pgu[pi][:, 512:1024], in1=sg[hi][:],
                                                                       op=ALU.mult), reads=[Bpgu[pi], Bsg[hi]], writes=[Bh1[hi]])
                                k.op("pool", lambda e: e.tensor_tensor(out=hg[:, ch, :], in0=h1[hi][:], in1=gbc[gi][:],
                                                                        op=ALU.mult), reads=[Bh1[hi], Bgbc[gi]], writes=[Bhg])
                            else:
                                k.op("dve", lambda e: e.tensor_tensor(out=hg[:, ch, :], in0=pgu[pi][:, 512:1024], in1=sg[hi][:],
                                                                       op=ALU.mult), reads=[Bpgu[pi], Bsg[hi]], writes=[Bhg])
                    for j in range(4):
                        t = tb * 4 + j
                        ri = t % 2
                        k.dma("sp", [(xr[ri][:], x1res[t * 128:(t + 1) * 128, :])], writes=[Bxr[ri]])
                        for hf in range(2):
                            k.mm_group([(lambda e, ch=ch: e.matmul(out=pdn[:, hf * 512:(hf + 1) * 512],
                                                                    lhsT=hg[:, ch, j * 128:(j + 1) * 128],
                                                                    rhs=wd_t[:, ch, hf * 512:(hf + 1) * 512],
                                                                    start=(ch == 0), stop=(ch == 33))) for ch in range(34)],
                                       reads=[Bhg, B_w], writes=[Bpdn])
                        k.op("dve", lambda e: e.scalar_tensor_tensor(out=rt[ri][:], in0=xr[ri][:], scalar=ALPHA, in1=pdn[:],
                                                                      op0=ALU.mult, op1=ALU.add),
                             reads=[Bxr[ri], Bpdn], writes=[Brt[ri]])
                        sidx = tb % 2
                        finish_tile(st, "l2", Brt[ri], rt[ri], gam, bet, B_gb, t, xres, xs[sidx], Bxs[sidx], psT, B_psT,
                                    final_out=(out if last else None))
                    if not last:
                        k.dma("pool", [(xT[:, :, tsl], xs[tb % 2][:])], reads=[Bxs[tb % 2]], writes=[B_xT], partial=True)
                k.barrier()

        phases = [("tables", phase_tables), ("ln_in", phase_ln_in)]
        for l in range(depth):
            phases += [("proj%d" % l, lambda l=l: phase_proj(l)),
                       ("dense%d" % l, lambda l=l: phase_attn_dense(l)), ("dil%d" % l, lambda l=l: phase_attn_dil(l)),
                       ("merge%d" % l, lambda l=l: phase_merge(l)),
                       ("moe%d" % l, lambda l=l: phase_moe(l, l == depth - 1))]
        skip = dbg.get("skip", ())
        for name, fn in phases:
            if name in skip:
                continue
            fn()
            if stop_after == name:
                break
        k.barrier()
    return nc


def make_in_maps(inputs, ncores=8):
    c = _host_consts()
    f = lambda a: np.ascontiguousarray(np.asarray(a, dtype=np.float32))
    shared = {
        "ln_in_g": f(inputs["ln_in_g"]), "ln_in_b": f(inputs["ln_in_b"]),
        "w_in": f(inputs["w_in"]), "b_gate": f(inputs["b_gate"]),
        "lam_all": np.ascontiguousarray(np.stack([f(inputs["lam_q1"]), f(inputs["lam_k1"]), f(inputs["lam_q2"]),
                                                  f(inputs["lam_k2"])], axis=1)),
        "diff_norm_g": f(inputs["diff_norm_g"]), "mla_q_norm_g": f(inputs["mla_q_norm_g"]),
        "mla_kv_norm_g": f(inputs["mla_kv_norm_g"]), "w_mla_qb": f(inputs["w_mla_qb"]),
        "w_mla_kvb": f(inputs["w_mla_kvb"]),
        "w_branch": np.ascontiguousarray(np.stack([f(inputs["w_branch_a"]), f(inputs["w_branch_b"]),
                                                   f(inputs["w_branch_c"])], axis=1)),
        "w_out": f(inputs["w_out"]), "ln1_g": f(inputs["ln1_g"]), "ln1_b": f(inputs["ln1_b"]),
        "router_w": f(inputs["router_w"]),
        "router_bias_t": np.ascontiguousarray(np.tile(f(inputs["router_bias"]), NT)),
        "w_eg": np.ascontiguousarray(np.concatenate([f(inputs["w_exp_gate"]), f(inputs["w_sh_gate"])[:, None]], axis=1)),
        "w_eu": np.ascontiguousarray(np.concatenate([f(inputs["w_exp_up"]), f(inputs["w_sh_up"])[:, None]], axis=1)),
        "w_ed": np.ascontiguousarray(np.concatenate([f(inputs["w_exp_down"]), f(inputs["w_sh_down"])[:, None]], axis=1)),
        "ln2_g": f(inputs["ln2_g"]), "ln2_b": f(inputs["ln2_b"]),
        "c_ident_f": c["ident_f"], "c_perm": c["perm"], "c_fs": c["fs"], "c_maskb": c["maskb"], "c_selc": c["selc"],
    }
    x = np.asarray(inputs["x"], dtype=np.float32)
    pos = np.asarray(inputs["positions"]).astype(np.int32)
    maps = []
    for b in range(ncores):
        m = dict(shared)
        m["x"] = np.ascontiguousarray(x[b])
        m["positions"] = np.ascontiguousarray(pos[b:b + 1])
        maps.append(m)
    return maps


def kernel(**inputs):
    nc = build_program()
    maps = make_in_maps(inputs, 8)
    res = run_bass_kernel_spmd(nc, maps, core_ids=list(range(8)))
    return np.stack([np.asarray(r["out"], dtype=np.float32) for r in res.results], axis=0)
```
